# Optimizing a Trainium2 kernel written in Bass

```python
import jax, jax.numpy as jnp
from jax import lax
import numpy as np

D_MODEL = 1024
BATCH = 16
SEQ = 2048
DEPTH = 4

CHUNK = 64
RWKV_HEAD_DIM = 64
RWKV_WIDTH = D_MODEL
RWKV_HEADS = RWKV_WIDTH // RWKV_HEAD_DIM
CONV_WIDTH = D_MODEL
CONV_KERNEL = 31
DECAY_LORA = D_MODEL // 16
A_LORA = D_MODEL // 16
V_LORA = D_MODEL // 32
G_LORA = D_MODEL // 8
D_FF = -(-8 * D_MODEL // (3 * 256)) * 256
N_IN_COLS = 3 * RWKV_WIDTH + 2 * CONV_WIDTH + 2 * D_MODEL
RMS_EPS = 1e-6
LN_EPS = 1e-5
GN_EPS = 64e-5

kernel_name = 'rwkv7_conformer_gated_hybrid'


def rms_norm(x, g):
    xf = x.astype(jnp.float32)
    y = xf * lax.rsqrt(jnp.mean(xf * xf, axis=-1, keepdims=True) + RMS_EPS)
    return (y * g.astype(jnp.float32)).astype(x.dtype)


def layer_norm(x, w, b):
    xf = x.astype(jnp.float32)
    mu = jnp.mean(xf, axis=-1, keepdims=True)
    var = jnp.mean(jnp.square(xf - mu), axis=-1, keepdims=True)
    y = (xf - mu) * lax.rsqrt(var + LN_EPS)
    return (y * w.astype(jnp.float32) + b.astype(jnp.float32)).astype(x.dtype)


def token_shift(x):
    return jnp.pad(x[:, :-1], ((0, 0), (1, 0), (0, 0)))


def lerp_shift(x, mu):
    return x + (token_shift(x) - x) * mu


def wkv7(r, w, k, v, a, b):
    B, T, H, N = r.shape
    seq = tuple(jnp.moveaxis(t.astype(jnp.float32), 1, 0) for t in (r, w, k, v, a, b))

    def step(S, inp):
        r_t, w_t, k_t, v_t, a_t, b_t = inp
        sa = jnp.einsum('bhvk,bhk->bhv', S, a_t)
        S = S * w_t[:, :, None, :] + sa[..., None] * b_t[:, :, None, :] + v_t[..., None] * k_t[:, :, None, :]
        y = jnp.einsum('bhvk,bhk->bhv', S, r_t)
        return S, y

    S0 = jnp.zeros((B, H, N, N), jnp.float32)
    _, ys = lax.scan(step, S0, seq)
    return jnp.moveaxis(ys, 0, 1)


def rwkv7_time_mix(h, p_r, p_k, p_v, v_first, vres, mu_rkv, mu_wag, w0, w1, w2, a0, a1, a2,
                   g1, g2, kk_scale, ka_scale, r_k, gn_w, gn_b, w_o):
    B, T, _ = h.shape
    xx = token_shift(h) - h
    xw = h + xx * mu_wag[0]
    xa = h + xx * mu_wag[1]
    xg = h + xx * mu_wag[2]
    r = lerp_shift(p_r, mu_rkv[0])
    k = lerp_shift(p_k, mu_rkv[1])
    v = lerp_shift(p_v, mu_rkv[2])
    w_log = -jax.nn.softplus(-(w0 + jnp.tanh(xw @ w1) @ w2).astype(jnp.float32)) - 0.5
    decay = jnp.exp(-jnp.exp(w_log))
    if vres is None:
        v_first = v
    else:
        mu_v, v0, v1, v2 = vres
        xv = h + xx * mu_v
        v = v + (v_first - v) * jax.nn.sigmoid(v0 + (xv @ v1) @ v2)
    a = jax.nn.sigmoid(a0 + (xa @ a1) @ a2)
    g = jax.nn.sigmoid(xg @ g1) @ g2

    def heads(t):
        return t.reshape(B, T, RWKV_HEADS, RWKV_HEAD_DIM)

    kk = heads(k * kk_scale).astype(jnp.float32)
    kk = kk / jnp.maximum(jnp.sqrt(jnp.sum(kk * kk, axis=-1, keepdims=True)), 1e-12)
    k = k * (1.0 + (a - 1.0) * ka_scale)
    rh, kh, vh, ah = heads(r), heads(k), heads(v), heads(a)
    y = wkv7(rh, heads(decay), kh, vh, -kk, kk * ah.astype(jnp.float32))
    mu = jnp.mean(y, axis=-1, keepdims=True)
    var = jnp.mean(jnp.square(y - mu), axis=-1, keepdims=True)
    y = ((y - mu) * lax.rsqrt(var + GN_EPS)).reshape(B, T, RWKV_WIDTH)
    y = y * gn_w.astype(jnp.float32) + gn_b.astype(jnp.float32)
    bonus = (jnp.sum((rh * kh * r_k).astype(jnp.float32), axis=-1, keepdims=True) * vh.astype(jnp.float32))
    y = ((y + bonus.reshape(B, T, RWKV_WIDTH)) * g.astype(jnp.float32)).astype(h.dtype)
    return y @ w_o, v_first


def conformer_conv(p_c, dw, dw_b, ln_w, ln_b, w_o):
    u, gate = jnp.split(p_c, 2, axis=-1)
    c = u * jax.nn.sigmoid(gate)
    c = lax.conv_general_dilated(c, dw[:, None, :].astype(c.dtype), window_strides=(1,),
                                 padding=[(CONV_KERNEL - 1, 0)],
                                 dimension_numbers=('NWC', 'WIO', 'NWC'),
                                 feature_group_count=CONV_WIDTH) + dw_b
    c = jax.nn.silu(layer_norm(c, ln_w, ln_b))
    return c @ w_o


def setup_inputs(seed: int = 0) -> dict:
    key = jax.random.key(seed)
    ks = iter(jax.random.split(key, 48))

    def nrm(shape, scale):
        return jax.random.normal(next(ks), shape, jnp.float32) * scale

    def unif(shape, lo, hi):
        return jax.random.uniform(next(ks), shape, jnp.float32, lo, hi)

    L, D, RW, CW = DEPTH, D_MODEL, RWKV_WIDTH, CONV_WIDTH
    Lv = DEPTH - 1
    return {
        'x': nrm((BATCH, SEQ, D), 1.0),
        'pre_mix_norm': 1.0 + nrm((L, D), 0.05),
        'post_mix_norm': 1.0 + nrm((L, D), 0.05),
        'pre_ffn_norm': 1.0 + nrm((L, D), 0.05),
        'post_ffn_norm': 1.0 + nrm((L, D), 0.05),
        'w_in': nrm((L, D, N_IN_COLS), D ** -0.5),
        'mu_rkv': unif((L, 3, RW), 0.0, 1.0),
        'mu_wag': unif((L, 3, D), 0.0, 1.0),
        'decay_w0': unif((L, RW), -6.0, -1.0),
        'decay_w1': nrm((L, D, DECAY_LORA), D ** -0.5),
        'decay_w2': nrm((L, DECAY_LORA, RW), 0.5 * DECAY_LORA ** -0.5),
        'a_0': nrm((L, RW), 0.5),
        'a_1': nrm((L, D, A_LORA), D ** -0.5),
        'a_2': nrm((L, A_LORA, RW), 0.5 * A_LORA ** -0.5),
        'g_1': nrm((L, D, G_LORA), D ** -0.5),
        'g_2': nrm((L, G_LORA, RW), G_LORA ** -0.5),
        'vres_mu': unif((Lv, D), 0.0, 1.0),
        'vres_0': nrm((Lv, RW), 0.5),
        'vres_1': nrm((Lv, D, V_LORA), D ** -0.5),
        'vres_2': nrm((Lv, V_LORA, RW), 0.5 * V_LORA ** -0.5),
        'k_k': 0.85 + nrm((L, RW), 0.05),
        'k_a': 1.0 + nrm((L, RW), 0.05),
        'r_k': nrm((L, RWKV_HEADS, RWKV_HEAD_DIM), 0.1),
        'gn_w': 1.0 + nrm((L, RW), 0.05),
        'gn_b': nrm((L, RW), 0.01),
        'w_rwkv_out': nrm((L, RW, D), RW ** -0.5),
        'conv_dw': nrm((L, CONV_KERNEL, CW), CONV_KERNEL ** -0.5),
        'conv_b': nrm((L, CW), 0.01),
        'conv_ln_w': 1.0 + nrm((L, CW), 0.05),
        'conv_ln_b': nrm((L, CW), 0.01),
        'w_conv_out': nrm((L, CW, D), CW ** -0.5),
        'w_out': nrm((L, D, D), D ** -0.5),
        'ffn_w_gate': nrm((L, D, D_FF), D ** -0.5),
        'ffn_w_up': nrm((L, D, D_FF), D ** -0.5),
        'ffn_w_down': nrm((L, D_FF, D), D_FF ** -0.5),
    }


def reference(x, pre_mix_norm, post_mix_norm, pre_ffn_norm, post_ffn_norm, w_in, mu_rkv, mu_wag,
              decay_w0, decay_w1, decay_w2, a_0, a_1, a_2, g_1, g_2, vres_mu, vres_0, vres_1, vres_2,
              k_k, k_a, r_k, gn_w, gn_b, w_rwkv_out, conv_dw, conv_b, conv_ln_w, conv_ln_b, w_conv_out,
              w_out, ffn_w_gate, ffn_w_up, ffn_w_down):
    splits = [RWKV_WIDTH, 2 * RWKV_WIDTH, 3 * RWKV_WIDTH,
              3 * RWKV_WIDTH + 2 * CONV_WIDTH, 3 * RWKV_WIDTH + 2 * CONV_WIDTH + D_MODEL]
    v_first = None
    for i in range(DEPTH):
        h = rms_norm(x, pre_mix_norm[i])
        proj = h @ w_in[i]
        p_r, p_k, p_v, p_c, z_rwkv, z_conv = jnp.split(proj, splits, axis=-1)
        vres = None if i == 0 else (vres_mu[i - 1], vres_0[i - 1], vres_1[i - 1], vres_2[i - 1])
        y_rwkv, v_first = rwkv7_time_mix(
            h, p_r, p_k, p_v, v_first, vres, mu_rkv[i], mu_wag[i],
            decay_w0[i], decay_w1[i], decay_w2[i], a_0[i], a_1[i], a_2[i], g_1[i], g_2[i],
            k_k[i], k_a[i], r_k[i], gn_w[i], gn_b[i], w_rwkv_out[i])
        y_conv = conformer_conv(p_c, conv_dw[i], conv_b[i], conv_ln_w[i], conv_ln_b[i], w_conv_out[i])
        merged = jax.nn.sigmoid(z_rwkv) * y_rwkv + jax.nn.sigmoid(z_conv) * y_conv
        x = x + rms_norm(merged @ w_out[i], post_mix_norm[i])
        h = rms_norm(x, pre_ffn_norm[i])
        f = (jax.nn.silu(h @ ffn_w_gate[i]) * (h @ ffn_w_up[i])) @ ffn_w_down[i]
        x = x + rms_norm(f, post_ffn_norm[i])
    return x
```

```python
import numpy as np
import ml_dtypes
from contextlib import ExitStack
import concourse.bass as bass
import concourse.mybir as mybir
from concourse.bass_utils import run_bass_kernel_spmd

F32 = mybir.dt.float32
BF16 = mybir.dt.bfloat16
AF = mybir.ActivationFunctionType
ALU = mybir.AluOpType
AX = mybir.AxisListType

D = 1024
H = 16
NFF = 22
NT = 128
NS = NT // 128
NVEC = 19
(V_GPRE, V_GPOST, V_GFPRE, V_GFPOST, V_MUR, V_MUK, V_MUV, V_MUW, V_MUA, V_MUG, V_MUVR, V_KK, V_KA, V_RK,
 V_GNW, V_GNB, V_CB, V_LNW, V_LNB) = range(NVEC)


class Buf:
    __slots__ = ("name", "w", "r")

    def __init__(self, name=""):
        self.name = name
        self.w = None
        self.r = []


class Plan:
    ENGS = ("tensor", "vector", "scalar", "gpsimd", "sync")

    def __init__(self):
        self.streams = {e: [] for e in self.ENGS}
        self.cnt = {e: 0 for e in self.ENGS}
        self.waited = {e: {} for e in self.ENGS}
        self.dma_cnt = {}

    def _need(self, eng, ev, waits):
        if ev is None:
            return
        k, v = ev
        if k == eng and v > self.cnt[eng]:
            return
        if self.waited[eng].get(k, 0) >= v:
            return
        if waits.get(k, 0) < v:
            waits[k] = v

    def op(self, eng, fn, reads=(), writes=(), sig=True, dma_sem=None):
        waits = {}
        for b in reads:
            self._need(eng, b.w, waits)
        for b in writes:
            self._need(eng, b.w, waits)
            for ev in b.r:
                self._need(eng, ev, waits)
        wl = []
        for k, v in waits.items():
            self.waited[eng][k] = v
            wl.append((k, v))
        if dma_sem is not None:
            self.dma_cnt[dma_sem] = self.dma_cnt.get(dma_sem, 0) + 16
            ev = (dma_sem, self.dma_cnt[dma_sem])
            self.streams[eng].append((wl, fn, dma_sem, 16))
        elif sig:
            self.cnt[eng] += 1
            ev = (eng, self.cnt[eng])
            self.streams[eng].append((wl, fn, eng, 1))
        else:
            self.streams[eng].append((wl, fn, None, 0))
            ev = (eng, self.cnt[eng] + 1)
        for b in reads:
            if len(b.r) > 8:
                b.r = [e for e in b.r if not (e[0] == ev[0] and e[1] <= ev[1])]
            b.r.append(ev)
        for b in writes:
            b.w = ev
            b.r = []

    def barrier(self):
        evs = [(e, self.cnt[e]) for e in self.ENGS if self.cnt[e] > 0]
        evs += [(k, v) for k, v in self.dma_cnt.items()]
        for e in self.ENGS:
            wl = []
            for (k, v) in evs:
                if k == e:
                    continue
                if self.waited[e].get(k, 0) < v:
                    self.waited[e][k] = v
                    wl.append((k, v))
            if wl:
                self.streams[e].append((wl, None, None, 0))

    def emit(self, nc, sems, final_waits):
        with nc.Block() as block:
            def mk(engname):
                def body(e):
                    for (wl, fn, sk, inc) in self.streams[engname]:
                        for (k, v) in wl:
                            e.wait_ge(sems[k], v)
                        if fn is None:
                            continue
                        ins = fn(e)
                        if sk is not None:
                            ins.then_inc(sems[sk], inc)
                    for (k, v) in final_waits.get(engname, []):
                        e.wait_ge(sems[k], v)
                return body
            block.tensor(mk("tensor"))
            block.vector(mk("vector"))
            block.scalar(mk("scalar"))
            block.gpsimd(mk("gpsimd"))
            block.sync(mk("sync"))


class StopBuild(Exception):
    pass


def build(T, nseq, layers, dbg=False, kstop=0):
    def chk(n):
        if kstop == n:
            raise StopBuild()
    nc = bass.Bass("TRN2", target_bir_lowering=False)
    P = Plan()
    NL = len(layers)
    dr = {}

    def din(name, shape, dt=F32):
        dr[name] = nc.dram_tensor(name, list(shape), dt, kind="ExternalInput").ap()
        return dr[name]

    x_in = din("x", [nseq, T, D])
    ident_d = din("ident", [128, 128], BF16)
    tri_d = din("tri3", [128, 3, 128])
    mskL_d = din("mskL", [128, 512])
    mskT_d = din("mskT", [128, 512])
    id4_d = din("ident4", [128, 512])
    WN = {}
    for l in layers:
        WN[l] = dict(
            tok=din(f"wtok{l}", [128, 8, 3072]), fm=din(f"wfm{l}", [128, 32, 8, 128]),
            l1=din(f"wl1{l}", [128, 8, 288]), l2=din(f"wl2{l}", [128, 4, 1024]),
            ro=din(f"wro{l}", [128, 8, 8, 128]), co=din(f"wco{l}", [128, 8, 8, 128]),
            wo=din(f"wo{l}", [128, 8, 1024]), g=din(f"wg{l}", [128, NFF, 8, 128]),
            u=din(f"wu{l}", [128, NFF, 8, 128]), d=din(f"wd{l}", [128, NFF, 1024]),
            vec=din(f"vec{l}", [NVEC, D]), vecT=din(f"vecT{l}", [128, 8, NVEC]), dw=din(f"dwT{l}", [128, 8, 31]))
    out = nc.dram_tensor("out", [nseq, T, D], F32, kind="ExternalOutput").ap()
    if 0 in layers and NL == 1:
        vf_d = nc.dram_tensor("vf", [nseq, T, D], F32, kind="ExternalOutput").ap()
    elif 0 in layers:
        vf_d = nc.dram_tensor("vf", [nseq, T, D], F32).ap()
    else:
        vf_d = din("vf", [nseq, T, D])
    SC = {}
    for l in layers:
        SC[l] = dict(
            tok=nc.dram_tensor(f"s_tok{l}", [128, 12, 16, 256], BF16).ap(),
            fm=nc.dram_tensor(f"s_fm{l}", [128, 32, 8, 128], BF16).ap(),
            l1=nc.dram_tensor(f"s_l1{l}", [128, 8, 2, 288], BF16).ap(),
            l2=nc.dram_tensor(f"s_l2{l}", [128, 4, 1024], BF16).ap(),
            ro=nc.dram_tensor(f"s_ro{l}", [128, 8, 8, 128], BF16).ap(),
            co=nc.dram_tensor(f"s_co{l}", [128, 8, 8, 128], BF16).ap(),
            wo=nc.dram_tensor(f"s_wo{l}", [128, 8, 1024], BF16).ap(),
            g=nc.dram_tensor(f"s_g{l}", [128, NFF, 8, 128], BF16).ap(),
            u=nc.dram_tensor(f"s_u{l}", [128, NFF, 8, 128], BF16).ap(),
            d=nc.dram_tensor(f"s_d{l}", [128, NFF, 1024], BF16).ap())

    with ExitStack() as st:
        def sb(name, shape, dt=F32):
            return st.enter_context(nc.sbuf_tensor(name, list(shape), dt))
        B = {}

        def nb(name):
            B[name] = Buf(name)
            return B[name]

        def act(out_, in_, func, R, W, **kw):
            P.op("scalar", lambda e: e.activation(out=out_, in_=in_, func=func, **kw), R, W)

        def ts(eng, out_, in0, s1, s2, op0, op1, R, W):
            if s2 is None:
                P.op(eng, lambda e: e.tensor_scalar(out=out_, in0=in0, scalar1=s1, scalar2=None, op0=op0), R, W)
            else:
                P.op(eng, lambda e: e.tensor_scalar(out=out_, in0=in0, scalar1=s1, scalar2=s2, op0=op0, op1=op1), R, W)

        def tt(eng, out_, in0, in1, op, R, W):
            P.op(eng, lambda e: e.tensor_tensor(out=out_, in0=in0, in1=in1, op=op), R, W)

        def stt(out_, in0, scalar, in1, op0, op1, R, W):
            P.op("vector", lambda e: e.scalar_tensor_tensor(out=out_, in0=in0, scalar=scalar, in1=in1, op0=op0, op1=op1), R, W)

        def cp(eng, out_, in_, R, W):
            P.op(eng, lambda e: e.tensor_copy(out=out_, in_=in_), R, W)

        def mset(eng, ap, val, W):
            P.op(eng, lambda e: e.memset(ap, val), (), W)

        def mm(out_, lhsT, rhs, start, stop, R, W, sig):
            P.op("tensor", lambda e: e.matmul(out_, lhsT, rhs, start=start, stop=stop), R, W, sig=sig)

        def tr(out_, in_, idn, R, W, sig):
            P.op("tensor", lambda e: e.transpose(out_, in_, idn), R, W, sig=sig)

        dma_rr = [0]

        def dma(out_, in_, R, W, sem, eng="sync"):
            P.op(eng, lambda e: e.dma_start(out=out_, in_=in_), R, W, dma_sem=sem)

        dumps = []

        def dump(name, ap_, shape, dt_, bufs):
            if not dbg:
                return
            t_ = nc.dram_tensor(name, list(shape), dt_, kind="ExternalOutput").ap()
            dma(t_, ap_, bufs, (), "dbg_" + name)
            dumps.append("dbg_" + name)

        IDENT = sb("IDENT", [128, 128], BF16); nb("IDENT")
        TRI = sb("TRI", [128, 3, 128]); nb("TRI")
        MSKL = sb("MSKL", [128, 512]); nb("MSKL")
        MSKT = sb("MSKT", [128, 512]); nb("MSKT")
        ID4 = sb("ID4", [128, 512]); nb("ID4")
        ONES32 = sb("ONES32", [128, 128]); nb("ONES32")
        NH = sb("NH", [128, NT]); nb("NH")
        dma(IDENT[:], ident_d, (), [B["IDENT"]], "c0")
        dma(TRI[:], tri_d, (), [B["TRI"]], "c1")
        dma(MSKL[:], mskL_d, (), [B["MSKL"]], "c2")
        dma(MSKT[:], mskT_d, (), [B["MSKT"]], "c3")
        dma(ID4[:], id4_d, (), [B["ID4"]], "c4")
        mset("gpsimd", ONES32[:], 1.0, [B["ONES32"]])
        mset("gpsimd", NH[:], -0.5, [B["NH"]])

        with ExitStack() as pst:
            def psb(name, shape, dt=F32):
                return pst.enter_context(nc.sbuf_tensor(name, list(shape), dt))
            STG = [psb(f"STG{i}", [128, 4096]) for i in range(2)]
            STB = [psb(f"STB{i}", [128, 4608], BF16) for i in range(2)]
            for i in range(2):
                nb(f"STG{i}"); nb(f"STB{i}")
            VB = psb("VB", [128, 3, 1024]); nb("VB")
            VB1 = psb("VB1", [128, 3, 1024]); nb("VB1")
            VT = psb("VT", [128, 8, NVEC]); nb("VT")
            VT1 = psb("VT1", [128, 8, 4]); nb("VT1")
            pk = [0]

            def plain(dst2d, src2d, n):
                for c0 in range(0, n, 4096):
                    c1 = min(n, c0 + 4096)
                    i = pk[0] % 2; pk[0] += 1
                    dma(STG[i][:, 0:c1 - c0], src2d[:, c0:c1], (), [B[f"STG{i}"]], f"pi{i}")
                    if i == 0:
                        cp("vector", STB[i][:, 0:c1 - c0], STG[i][:, 0:c1 - c0], [B[f"STG{i}"]], [B[f"STB{i}"]])
                    else:
                        act(STB[i][:, 0:c1 - c0], STG[i][:, 0:c1 - c0], AF.Copy, [B[f"STG{i}"]], [B[f"STB{i}"]])
                    dma(dst2d[:, c0:c1], STB[i][:, 0:c1 - c0], [B[f"STB{i}"]], (), f"po{i}", eng="gpsimd")

            for l in layers:
                W_, S_ = WN[l], SC[l]
                for j in range(3):
                    dma(VB[:, j, :], W_["vec"][V_MUR + j, :].partition_broadcast(128), (), [B["VB"]], "pv0")
                dma(VT[:], W_["vecT"], (), [B["VT"]], "pv1")
                ts("vector", VB1[:], VB[:], -1.0, 1.0, ALU.mult, ALU.add, [B["VB"]], [B["VB1"]])
                ts("vector", VT1[:], VT[:, :, V_MUW:V_MUW + 4], -1.0, 1.0, ALU.mult, ALU.add, [B["VT"]], [B["VT1"]])
                for j in range(12):
                    i = pk[0] % 2; pk[0] += 1
                    wi = j // 4
                    dma(STG[i][:, 0:2048].rearrange("p (k c) -> p k c", k=8), W_["tok"][:, :, j * 256:(j + 1) * 256],
                        (), [B[f"STG{i}"]], f"pi{i}")
                    for s in range(2):
                        vb = (VB1 if s == 0 else VB)
                        for kc in range(8):
                            tt("vector" if kc % 2 == 0 else "gpsimd",
                               STB[i][:, (kc * 2 + s) * 256:(kc * 2 + s + 1) * 256],
                               STG[i][:, kc * 256:(kc + 1) * 256], vb[:, wi, (j % 4) * 256:(j % 4 + 1) * 256], ALU.mult,
                               [B[f"STG{i}"], B["VB"], B["VB1"]], [B[f"STB{i}"]])
                    dma(S_["tok"][:, j, :, :], STB[i][:, 0:4096].rearrange("p (k c) -> p k c", k=16),
                        [B[f"STB{i}"]], (), f"po{i}", eng="gpsimd")
                i = pk[0] % 2; pk[0] += 1
                dma(STG[i][:, 0:2304].rearrange("p (k c) -> p k c", k=8), W_["l1"], (), [B[f"STG{i}"]], f"pi{i}")
                for (c0, c1, mi) in ((0, 64, 0), (64, 128, 1), (128, 256, 2), (256, 288, 3)):
                    for kc in range(8):
                        for s in range(2):
                            sc_ = (VT1[:, kc, mi:mi + 1] if s == 0 else VT[:, kc, V_MUW + mi:V_MUW + mi + 1])
                            ts("vector", STB[i][:, (kc * 2 + s) * 288 + c0:(kc * 2 + s) * 288 + c1],
                               STG[i][:, kc * 288 + c0:kc * 288 + c1], sc_, None, ALU.mult, None,
                               [B[f"STG{i}"], B["VT"], B["VT1"]], [B[f"STB{i}"]])
                dma(S_["l1"].rearrange("p k s c -> p (k s c)"), STB[i][:, 0:4608],
                    [B[f"STB{i}"]], (), f"po{i}", eng="gpsimd")
                plain(S_["l2"].rearrange("p a c -> p (a c)"), W_["l2"].rearrange("p a c -> p (a c)"), 4096)
                plain(S_["fm"].rearrange("p a k c -> p (a k c)"), W_["fm"].rearrange("p a k c -> p (a k c)"), 32768)
                plain(S_["ro"].rearrange("p a k c -> p (a k c)"), W_["ro"].rearrange("p a k c -> p (a k c)"), 8192)
                plain(S_["co"].rearrange("p a k c -> p (a k c)"), W_["co"].rearrange("p a k c -> p (a k c)"), 8192)
                plain(S_["wo"].rearrange("p k c -> p (k c)"), W_["wo"].rearrange("p k c -> p (k c)"), 8192)
                plain(S_["g"].rearrange("p a k c -> p (a k c)"), W_["g"].rearrange("p a k c -> p (a k c)"), NFF * 1024)
                plain(S_["u"].rearrange("p a k c -> p (a k c)"), W_["u"].rearrange("p a k c -> p (a k c)"), NFF * 1024)
                plain(S_["d"].rearrange("p k c -> p (k c)"), W_["d"].rearrange("p k c -> p (k c)"), NFF * 1024)
            P.barrier()
        RING = [sb(f"RING{i}", [128, 4608], BF16) for i in range(3)]
        for i in range(3):
            nb(f"RING{i}")
        rk = [0]

        def slab(src2d, n):
            i = rk[0] % 3; rk[0] += 1
            dma(RING[i][:, 0:n], src2d, (), [B[f"RING{i}"]], f"rg{i}")
            return RING[i], B[f"RING{i}"]

        PS = [st.enter_context(nc.psum_tensor(f"PSB{i}", [128, 512], F32)) for i in range(7)]
        PT = st.enter_context(nc.psum_tensor("PTB", [128, 1024], BF16)); nb("PT")
        for i in range(7):
            nb(f"PS{i}")
        pk2 = [0]

        def bank():
            i = pk2[0] % 7; pk2[0] += 1
            return PS[i], B[f"PS{i}"]

        def T_(name, shape, dt=F32):
            nb(name)
            return sb(name, shape, dt)
        X = T_("X", [128, NS, D])
        XNT = T_("XNT", [128, 8, NT + 1], BF16)
        CARRY = T_("CARRY", [128, 8, 1], BF16)
        L2BUF = T_("L2BUF", [128, 4096], BF16)
        CB = T_("CB", [128, 8, NT + 30])
        ACC = T_("ACC", [128, 8, NT])
        HT = T_("HT", [128, NFF, NT], BF16)
        CCT = T_("CCT", [128, 8, NT], BF16)
        YT = T_("YT", [128, 8, NT], BF16)
        MRG = T_("MRG", [128, 8, NT], BF16)
        XNB = T_("XNB", [128, D], BF16)
        SS = T_("SS", [128, 4]); MS = T_("MS", [128, 4]); RSTD = T_("RSTD", [128, 4])
        LW1 = T_("LW1", [65, NT], BF16); LA1 = T_("LA1", [65, NT], BF16); LV1 = T_("LV1", [33, NT], BF16)
        LG1 = T_("LG1", [128, NT], BF16)
        TMPF = [T_(f"TMPF{i}", [128, NT]) for i in range(2)]
        MEAN = T_("MEAN", [128, NT]); VAR = T_("VAR", [128, NT]); RS = T_("RS", [128, NT])
        PV = T_("PV", [128, 8, NVEC])
        HLN = T_("HLN", [128, 8, 2])
        DW = T_("DW", [128, 8, 31])
        GPOST = T_("GPOST", [128, D]); GFPOST = T_("GFPOST", [128, D])
        KKT = T_("KKT", [128, D]); KAT = T_("KAT", [128, D]); RKT = T_("RKT", [128, D])
        GNW = T_("GNW", [128, D]); GNB = T_("GNB", [128, D])
        A = [T_(f"A{i}", [128, D]) for i in range(7)]
        EB = [T_(f"EB{i}", [128, D], BF16) for i in range(4)]
        OB = [T_(f"OB{i}", [128, D], BF16) for i in range(2)]
        JUNK = OB[1]; B["JUNK"] = B["OB1"]
        KH = T_("KH", [128, D], BF16); BH = T_("BH", [128, D], BF16); VBF = T_("VBF", [128, D], BF16)
        TAR = T_("TAR", [128, 8, 2, 128], BF16)
        TBT = T_("TBT", [128, 8, 128], BF16); TKT = T_("TKT", [128, 8, 128], BF16)
        SM = T_("SM", [128, 16, 4])
        S32 = T_("S32", [128, 8, 64]); SBF = T_("SBF", [128, 8, 64], BF16); PC = T_("PC", [128, 8])
        XB = T_("XB", [128, 16, 64], BF16); UB = T_("UB", [128, 16, 64], BF16)
        R0 = [T_(f"R0_{g}", [128, 512], BF16) for g in range(1)]
        QRG = [[T_(f"QRG{g}_{i}", [128, 3, 512], BF16) for i in range(2)] for g in range(1)]
        MB = T_("MB", [128, 16, 2, 128], BF16)
        MK = T_("MK", [128, 16, 2, 128], BF16)
        G7 = T_("G7", [128, 16, 128], BF16)
        TARm = [T_(f"TARm{i}", [128, 8, 2, 128], BF16) for i in range(2)]
        TBTm = [T_(f"TBTm{i}", [128, 8, 128], BF16) for i in range(2)]
        SBFm = [T_(f"SBFm{i}", [128, 8, 64], BF16) for i in range(2)]
        for i_ in range(2):
            for (tn, tl) in (("TARm", TARm), ("TBTm", TBTm), ("SBFm", SBFm)):
                mset("vector", tl[i_][:], 0.0, [B[f"{tn}{i_}"]])

        mset("vector", LW1[64:65, :], 1.0, [B["LW1"]])
        mset("vector", LA1[64:65, :], 1.0, [B["LA1"]])
        mset("vector", LV1[32:33, :], 1.0, [B["LV1"]])

        def rmsnorm_to(dst, coff, gcol):
            for s_ in range(NS):
                act(JUNK[:], X[:, s_, :], AF.Square, [B["X"]], [B["JUNK"], B["SS"]], accum_out=SS[:, 0:1])
                ts("vector", MS[:, 0:1], SS[:, 0:1], 1.0 / D, 1e-6, ALU.mult, ALU.add, [B["SS"]], [B["MS"]])
                tt("gpsimd", RSTD[:, 0:1], MS[:, 0:1], NH[:, 0:1], ALU.pow, [B["MS"], B["NH"]], [B["RSTD"]])
                act(XNB[:], X[:, s_, :], AF.Copy, [B["X"], B["RSTD"]], [B["XNB"]], scale=RSTD[:, 0:1])
                for kc in range(8):
                    tr(PT[:, kc * 128:(kc + 1) * 128], XNB[:, kc * 128:(kc + 1) * 128], IDENT[:],
                       [B["XNB"], B["IDENT"]], [B["PT"]], sig=(kc == 7))
                for kc in range(8):
                    ts("vector" if kc % 2 else "gpsimd" if False else "vector",
                       dst[:, kc, coff + s_ * 128:coff + (s_ + 1) * 128], PT[:, kc * 128:(kc + 1) * 128],
                       PV[:, kc, gcol:gcol + 1], None, ALU.mult, None, [B["PT"], B["PV"]], [B[dst_name[id(dst)]]])

        dst_name = {id(XNT): "XNT"}

        def sigm_from_tanh(eng, ap, R, W):
            ts(eng, ap, ap, 0.5, 0.5, ALU.mult, ALU.add, R, W)

        try:
            chk(1)
            for l in layers:
                W_, S_ = WN[l], SC[l]
                has_v = (l != 0)
                dma(PV[:], W_["vecT"], (), [B["PV"]], "lp0")
                dma(DW[:], W_["dw"], (), [B["DW"]], "lp1")
                for (tile_, bn, row) in ((GPOST, "GPOST", V_GPOST), (GFPOST, "GFPOST", V_GFPOST), (KKT, "KKT", V_KK),
                                         (KAT, "KAT", V_KA), (RKT, "RKT", V_RK), (GNW, "GNW", V_GNW), (GNB, "GNB", V_GNB)):
                    dma(tile_[:], W_["vec"][row, :].partition_broadcast(128), (), [B[bn]], "lp_" + bn)
                ts("vector", HLN[:], PV[:, :, V_LNW:V_LNW + 2], 0.5, None, ALU.mult, None, [B["PV"]], [B["HLN"]])
                dma(L2BUF[:], S_["l2"].rearrange("p a c -> p (a c)"), (), [B["L2BUF"]], "lp2")
                for seq in range(nseq):
                    mset("vector", S32[:], 0.0, [B["S32"]])
                    mset("vector", SBF[:], 0.0, [B["SBF"]])
                    mset("gpsimd", CB[:, :, 0:30], 0.0, [B["CB"]])
                    for ti in range(T // NT):
                        t0 = ti * NT
                        xsrc = (x_in if l == layers[0] else out)[seq, t0:t0 + NT, :].rearrange("(s p) d -> p s d", p=128)
                        dma(X[:], xsrc, (), [B["X"]], "dx")
                        if ti == 0:
                            mset("vector", XNT[:, :, 0:1], 0.0, [B["XNT"]])
                        else:
                            cp("vector", XNT[:, :, 0:1], CARRY[:], [B["CARRY"]], [B["XNT"]])
                        rmsnorm_to(XNT, 1, V_GPRE)
                        cp("vector", CARRY[:], XNT[:, :, NT:NT + 1], [B["XNT"]], [B["CARRY"]])
                        chk(2)
                        L1, BL1 = slab(S_["l1"].rearrange("p k s c -> p (k s c)"), 4608)
                        L1v = L1[:, 0:4608].rearrange("p (k s c) -> p k s c", k=8, s=2)
                        for (c0, c1, dstt, dn, fn_) in ((0, 64, LW1, "LW1", AF.Tanh), (64, 128, LA1, "LA1", AF.Copy),
                                                       (128, 256, LG1, "LG1", AF.Tanh), (256, 288, LV1, "LV1", AF.Copy)):
                            if c0 == 256 and not has_v:
                                continue
                            M_ = c1 - c0
                            pb, bpb = bank()
                            for kc in range(8):
                                for s2 in range(2):
                                    mm(pb[0:M_, 0:NT], L1v[:, kc, s2, c0:c1], XNT[:, kc, 1 - s2:1 - s2 + NT],
                                       kc == 0 and s2 == 0, kc == 7 and s2 == 1, [BL1, B["XNT"]], [bpb], sig=(kc == 7 and s2 == 1))
                            if dn == "LG1":
                                act(LG1[:], pb[0:128, 0:NT], AF.Tanh, [bpb], [B["LG1"]], scale=0.5)
                                sigm_from_tanh("gpsimd", LG1[:], [B["LG1"]], [B["LG1"]])
                            else:
                                act(dstt[0:M_, :], pb[0:M_, 0:NT], fn_, [bpb], [B[dn]])
                        BL2 = B["L2BUF"]
                        L2v = L2BUF[:, 0:4096].rearrange("p (a c) -> p a c", a=4)
                        chk(3)
                        for q in range(2):
                            FU, BFU = slab(S_["fm"][:, q * 4:(q + 1) * 4].rearrange("p a k c -> p (a k c)"), 4096)
                            FG, BFG = slab(S_["fm"][:, 8 + q * 4:8 + (q + 1) * 4].rearrange("p a k c -> p (a k c)"), 4096)
                            FUv = FU[:, 0:4096].rearrange("p (a k c) -> p a k c", a=4, k=8)
                            FGv = FG[:, 0:4096].rearrange("p (a k c) -> p a k c", a=4, k=8)
                            for o4 in range(4):
                                oc = q * 4 + o4
                                bu, bbu = bank(); bg, bbg = bank()
                                for kc in range(8):
                                    mm(bu[:, 0:NT], FUv[:, o4, kc, :], XNT[:, kc, 1:1 + NT], kc == 0, kc == 7, [BFU, B["XNT"]], [bbu], sig=(kc == 7))
                                for kc in range(8):
                                    mm(bg[:, 0:NT], FGv[:, o4, kc, :], XNT[:, kc, 1:1 + NT], kc == 0, kc == 7, [BFG, B["XNT"]], [bbg], sig=(kc == 7))
                                tf = TMPF[oc % 2]; btf = B[f"TMPF{oc % 2}"]
                                act(tf[:], bg[:, 0:NT], AF.Tanh, [bbg], [btf], scale=0.5)
                                sigm_from_tanh("gpsimd", tf[:], [btf], [btf])
                                tt("vector", CB[:, oc, 30:30 + NT], bu[:, 0:NT], tf[:], ALU.mult, [bbu, btf], [B["CB"]])
                        for oc in range(8):
                            ts("vector", ACC[:, oc, :], CB[:, oc, 0:NT], DW[:, oc, 0:1], PV[:, oc, V_CB:V_CB + 1], ALU.mult, ALU.add,
                               [B["CB"], B["DW"], B["PV"]], [B["ACC"]])
                            for j in range(1, 31):
                                stt(ACC[:, oc, :], CB[:, oc, j:j + NT], DW[:, oc, j:j + 1], ACC[:, oc, :], ALU.mult, ALU.add,
                                    [B["CB"], B["DW"], B["ACC"]], [B["ACC"]])
                        cp("gpsimd", CB[:, :, 0:30], CB[:, :, NT:NT + 30], [B["CB"]], [B["CB"]])
                        s1, bs1 = bank(); s2b, bs2 = bank()
                        for oc in range(8):
                            mm(s1[:, 0:NT], ONES32[:], ACC[:, oc, :], oc == 0, oc == 7, [B["ONES32"], B["ACC"]], [bs1], sig=(oc == 7))
                        for oc in range(8):
                            tf = TMPF[oc % 2]; btf = B[f"TMPF{oc % 2}"]
                            act(tf[:], ACC[:, oc, :], AF.Square, [B["ACC"]], [btf])
                            mm(s2b[:, 0:NT], ONES32[:], tf[:], oc == 0, oc == 7, [B["ONES32"], btf], [bs2], sig=True)
                        act(MEAN[:], s1[:, 0:NT], AF.Copy, [bs1], [B["MEAN"]], scale=1.0 / D)
                        act(VAR[:], s2b[:, 0:NT], AF.Copy, [bs2], [B["VAR"]], scale=1.0 / D)
                        tt("gpsimd", RS[:], MEAN[:], MEAN[:], ALU.mult, [B["MEAN"]], [B["RS"]])
                        tt("gpsimd", VAR[:], VAR[:], RS[:], ALU.subtract, [B["VAR"], B["RS"]], [B["VAR"]])
                        ts("gpsimd", VAR[:], VAR[:], 1e-5, None, ALU.add, None, [B["VAR"]], [B["VAR"]])
                        tt("gpsimd", RS[:], VAR[:], NH[:], ALU.pow, [B["VAR"], B["NH"]], [B["RS"]])
                        for oc in range(8):
                            t1 = TMPF[0]; t2 = TMPF[1]
                            tt("vector", t1[:], ACC[:, oc, :], MEAN[:], ALU.subtract, [B["ACC"], B["MEAN"]], [B["TMPF0"]])
                            tt("gpsimd", t1[:], t1[:], RS[:], ALU.mult, [B["TMPF0"], B["RS"]], [B["TMPF0"]])
                            act(t2[:], t1[:], AF.Tanh, [B["TMPF0"], B["HLN"]], [B["TMPF1"]], scale=HLN[:, oc, 0:1], bias=HLN[:, oc, 1:2])
                            ts("vector", t1[:], t1[:], PV[:, oc, V_LNW:V_LNW + 1], PV[:, oc, V_LNB:V_LNB + 1], ALU.mult, ALU.add,
                               [B["TMPF0"], B["PV"]], [B["TMPF0"]])
                            sigm_from_tanh("gpsimd", t2[:], [B["TMPF1"]], [B["TMPF1"]])
                            tt("vector", CCT[:, oc, :], t1[:], t2[:], ALU.mult, [B["TMPF0"], B["TMPF1"]], [B["CCT"]])

                        chk(4)
                        r32, k32, v32, asg, lw, t1, t2 = A
                        bR, bK, bV, bAS, bLW, bT1, bT2 = [B[f"A{i}"] for i in range(7)]
                        for j in range(12):
                            TK, BTK = slab(S_["tok"][:, j].rearrange("p k c -> p (k c)"), 4096)
                            TKv = TK[:, 0:4096].rearrange("p (k c) -> p k c", k=16)
                            pb, bpb = bank()
                            for kc in range(8):
                                for s2 in range(2):
                                    mm(pb[:, 0:256], XNT[:, kc, 1 - s2:1 - s2 + NT], TKv[:, kc * 2 + s2, :],
                                       kc == 0 and s2 == 0, kc == 7 and s2 == 1, [BTK, B["XNT"]], [bpb], sig=(kc == 7 and s2 == 1))
                            dstA = A[j // 4]
                            act(dstA[:, (j % 4) * 256:(j % 4 + 1) * 256], pb[:, 0:256], AF.Copy, [bpb], [B[f"A{j // 4}"]])
                        def lora2(src, K_, a_idx):
                            res = []
                            for hf in range(2):
                                pb, bpb = bank()
                                mm(pb[:, 0:512], src[0:K_, :], L2v[0:K_, a_idx, hf * 512:(hf + 1) * 512], True, True,
                                   [B["LW1"], B["LA1"], B["LV1"], B["LG1"], BL2], [bpb], sig=True)
                                res.append((pb, bpb))
                            return res
                        if l == 0:
                            dma(vf_d[seq, t0:t0 + NT, :], v32[:], [bV], (), "vfo")
                        else:
                            VF = t2
                            dma(VF[:], vf_d[seq, t0:t0 + NT, :], (), [bT2], "vfi")
                            rr = lora2(LV1, 33, 2)
                            for hf, (pb, bpb) in enumerate(rr):
                                act(t1[:, hf * 512:(hf + 1) * 512], pb[:, 0:512], AF.Tanh, [bpb], [bT1], scale=0.5)
                            sigm_from_tanh("gpsimd", t1[:], [bT1], [bT1])
                            tt("gpsimd", VF[:], VF[:], v32[:], ALU.subtract, [bT2, bV], [bT2])
                            tt("gpsimd", VF[:], VF[:], t1[:], ALU.mult, [bT2, bT1], [bT2])
                            tt("gpsimd", v32[:], v32[:], VF[:], ALU.add, [bV, bT2], [bV])
                        rr = lora2(LA1, 65, 1)
                        for hf, (pb, bpb) in enumerate(rr):
                            act(asg[:, hf * 512:(hf + 1) * 512], pb[:, 0:512], AF.Tanh, [bpb], [bAS], scale=0.5)
                        sigm_from_tanh("gpsimd", asg[:], [bAS], [bAS])
                        rr = lora2(LW1, 65, 0)
                        for hf, (pb, bpb) in enumerate(rr):
                            act(lw[:, hf * 512:(hf + 1) * 512], pb[:, 0:512], AF.Tanh, [bpb], [bLW], scale=0.5)
                        ts("gpsimd", lw[:], lw[:], -0.30326533, -0.30326533, ALU.mult, ALU.add, [bLW], [bLW])
                        chk(5)
                        for (ti_, dsts) in ((0, ((EB[0], "EB0", 1.0), (EB[1], "EB1", -1.0))), (1, ((EB[2], "EB2", 1.0),)), (2, ((EB[3], "EB3", 1.0),))):
                            for hf in range(2):
                                pb, bpb = bank()
                                mm(pb[:, 0:512], TRI[:, ti_, :], lw[:, hf * 512:(hf + 1) * 512], True, True, [B["TRI"], bLW], [bpb], sig=True)
                                for (dd, dn, sc_) in dsts:
                                    act(dd[:, hf * 512:(hf + 1) * 512], pb[:, 0:512], AF.Exp, [bpb], [B[dn]], scale=sc_)
                        pb, bpb = bank()
                        for h in range(H):
                            po = (h % 2) * 64
                            mm(pb[po:po + 64, h // 2:h // 2 + 1], lw[:, h * 64:(h + 1) * 64], ONES32[:, 0:1], True, True,
                               [bLW, B["ONES32"]], [bpb], sig=(h == H - 1))
                        act(PC[:], pb[:, 0:8], AF.Exp, [bpb], [B["PC"]])
                        tt("gpsimd", t1[:], k32[:], KKT[:], ALU.mult, [bK, B["KKT"]], [bT1])
                        tt("gpsimd", t2[:], t1[:], t1[:], ALU.mult, [bT1], [bT2])
                        P.op("vector", lambda e: e.tensor_reduce(out=SM[:, :, 0], in_=t2[:].rearrange("p (h c) -> p h c", h=H), axis=AX.X, op=ALU.add), [bT2], [B["SM"]])
                        ts("vector", SM[:, :, 0], SM[:, :, 0], 1e-24, None, ALU.max, None, [B["SM"]], [B["SM"]])
                        tt("gpsimd", SM[:, :, 1], SM[:, :, 0], NH[:, 0:H], ALU.pow, [B["SM"], B["NH"]], [B["SM"]])
                        for h in range(H):
                            ts("vector", t1[:, h * 64:(h + 1) * 64], t1[:, h * 64:(h + 1) * 64], SM[:, h, 1:2], None, ALU.mult, None, [bT1, B["SM"]], [bT1])
                        kk = t1
                        stt(t2[:], asg[:], -1.0, KAT[:], ALU.add, ALU.mult, [bAS, B["KAT"]], [bT2])
                        stt(k32[:], t2[:], 1.0, k32[:], ALU.add, ALU.mult, [bT2, bK], [bK])
                        tt("gpsimd", t2[:], r32[:], k32[:], ALU.mult, [bR, bK], [bT2])
                        tt("gpsimd", t2[:], t2[:], RKT[:], ALU.mult, [bT2, B["RKT"]], [bT2])
                        P.op("vector", lambda e: e.tensor_reduce(out=SM[:, :, 2], in_=t2[:].rearrange("p (h c) -> p h c", h=H), axis=AX.X, op=ALU.add), [bT2], [B["SM"]])
                        tt("gpsimd", asg[:], asg[:], kk[:], ALU.mult, [bAS, bT1], [bAS])
                        bvec = asg
                        tt("vector", KH[:], k32[:], EB[3][:], ALU.mult, [bK, B["EB3"]], [B["KH"]])
                        tt("gpsimd", BH[:], bvec[:], EB[3][:], ALU.mult, [bAS, B["EB3"]], [B["BH"]])
                        cp("gpsimd", VBF[:], v32[:], [bV], [B["VBF"]])
                        def trans_to(srcf, bsrc, eb, ebn, neg, dst_fn, dname, oi):
                            ob = OB[oi]; bob = B[f"OB{oi}"]
                            if neg:
                                stt(ob[:], srcf[:], -1.0, eb[:], ALU.mult, ALU.mult, [bsrc, B[ebn]], [bob])
                            else:
                                tt("vector", ob[:], srcf[:], eb[:], ALU.mult, [bsrc, B[ebn]], [bob])
                            for kc in range(8):
                                tr(PT[:, kc * 128:(kc + 1) * 128], ob[:, kc * 128:(kc + 1) * 128], IDENT[:], [bob, B["IDENT"]], [B["PT"]], sig=(kc == 7))
                            cp("vector", dst_fn, PT[:].rearrange("p (k t) -> p k t", k=8), [B["PT"]], [B[dname]])
                        trans_to(r32, bR, EB[0], "EB0", False, TAR[:, :, 1, :], "TAR", 0)
                        trans_to(kk, bT1, EB[2], "EB2", True, TAR[:, :, 0, :], "TAR", 1)
                        trans_to(bvec, bAS, EB[1], "EB1", False, TBT[:], "TBT", 0)
                        trans_to(k32, bK, EB[1], "EB1", False, TKT[:], "TKT", 1)
                        for i_ in range(2):
                            ps_ = slice(i_ * 64, (i_ + 1) * 64)
                            cp("vector", TARm[i_][ps_], TAR[ps_], [B["TAR"]], [B[f"TARm{i_}"]])
                            cp("gpsimd", TBTm[i_][ps_], TBT[ps_], [B["TBT"]], [B[f"TBTm{i_}"]])
                            cp("gpsimd", SBFm[i_][ps_], SBF[ps_], [B["SBF"]], [B[f"SBFm{i_}"]])
                        chk(6)
                        for g4 in range(4):
                            gi = 0
                            pl, bpl = bank()
                            for hh in range(4):
                                h = g4 * 4 + hh; kc = h // 2; po = (h % 2) * 64
                                mm(pl[:, hh * 128:(hh + 1) * 128], TAR[:, kc, 0, :], TBTm[h % 2][:, kc, :], True, True,
                                   [B["TAR"], B[f"TBTm{h % 2}"]], [bpl], sig=(hh == 3))
                            tt("vector", R0[gi][:], pl[:, 0:512], MSKL[:], ALU.mult, [bpl, B["MSKL"]], [B[f"R0_{gi}"]])
                            for (lt, ltn, dstm, dmn) in ((TBT, "TBT", MB, "MB"), (TKT, "TKT", MK, "MK")):
                                for h2 in range(2):
                                    pb, bpb = bank()
                                    for hh in range(2):
                                        h = g4 * 4 + h2 * 2 + hh; kc = h // 2; po = (h % 2) * 64
                                        mm(pb[:, hh * 256:(hh + 1) * 256], lt[:, kc, :], TARm[h % 2][:, kc, :, :].rearrange("p a t -> p (a t)"),
                                           True, True, [B[ltn], B[f"TARm{h % 2}"]], [bpb], sig=(hh == 1))
                                    h0 = g4 * 4 + h2 * 2
                                    tt("vector", dstm[:, h0:h0 + 2, :, :].rearrange("p h a t -> p (h a t)"), pb[:, 0:512], MSKT[:], ALU.mult,
                                       [bpb, B["MSKT"]], [B[dmn]])
                            cur = QRG[gi][0]; nxt = QRG[gi][1]; bcur = B[f"QRG{gi}_0"]; bnxt = B[f"QRG{gi}_1"]
                            cp("vector", cur[:, 0, :].rearrange("p (h t) -> p h t", h=4), MB[:, g4 * 4:g4 * 4 + 4, 0, :], [B["MB"]], [bcur])
                            cp("gpsimd", cur[:, 1, :], R0[gi][:], [B[f"R0_{gi}"]], [bcur])
                            tt("gpsimd", cur[:, 2, :], cur[:, 0, :], ID4[:], ALU.add, [bcur, B["ID4"]], [bcur])
                            for jj in range(1, 7):
                                pq, bpq = bank(); pr, bpr = bank(); pg, bpg = bank()
                                for hh in range(4):
                                    cs_ = slice(hh * 128, (hh + 1) * 128)
                                    if jj < 6:
                                        mm(pq[:, cs_], cur[:, 1, cs_], cur[:, 0, cs_], True, True, [bcur], [bpq], sig=(hh == 3))
                                for hh in range(4):
                                    cs_ = slice(hh * 128, (hh + 1) * 128)
                                    mm(pr[:, cs_], cur[:, 0, cs_], cur[:, 1, cs_], True, True, [bcur], [bpr], sig=(hh == 3))
                                if jj < 6:
                                    act(nxt[:, 0, :], pq[:, 0:512], AF.Copy, [bpq], [bnxt])
                                cp("vector", nxt[:, 1, :], pr[:, 0:512], [bpr], [bnxt])
                                for hh in range(4):
                                    cs_ = slice(hh * 128, (hh + 1) * 128)
                                    mm(pg[:, cs_], nxt[:, 1, cs_], cur[:, 2, cs_], True, True, [bnxt, bcur], [bpg], sig=(hh == 3))
                                if jj < 6:
                                    tt("vector", nxt[:, 2, :], pg[:, 0:512], cur[:, 2, :], ALU.add, [bpg, bcur], [bnxt])
                                else:
                                    tt("vector", G7[:, g4 * 4:g4 * 4 + 4, :].rearrange("p h t -> p (h t)"), pg[:, 0:512], cur[:, 2, :], ALU.add,
                                       [bpg, bcur], [B["G7"]])
                                cur, nxt = nxt, cur; bcur, bnxt = bnxt, bcur
                        chk(7)
                        for h8 in range(2):
                            px, bpx = bank()
                            for hh in range(8):
                                h = h8 * 8 + hh; kc = h // 2; po = (h % 2) * 64
                                mm(px[:, hh * 64:(hh + 1) * 64], TAR[:, kc, 0, :], SBFm[h % 2][:, kc, :], True, False, [B["TAR"], B[f"SBFm{h % 2}"]], [bpx], sig=False)
                                mm(px[:, hh * 64:(hh + 1) * 64], MK[:, h, 0, :], VBF[:, h * 64:(h + 1) * 64], False, True, [B["MK"], B["VBF"]], [bpx], sig=(hh == 7))
                            cp("vector", XB[:, h8 * 8:(h8 + 1) * 8, :].rearrange("p h c -> p (h c)"), px[:, 0:512], [bpx], [B["XB"]])
                        for h8 in range(2):
                            pu, bpu = bank()
                            for hh in range(8):
                                h = h8 * 8 + hh
                                mm(pu[:, hh * 64:(hh + 1) * 64], G7[:, h, :], XB[:, h, :], True, True, [B["G7"], B["XB"]], [bpu], sig=(hh == 7))
                            cp("vector", UB[:, h8 * 8:(h8 + 1) * 8, :].rearrange("p h c -> p (h c)"), pu[:, 0:512], [bpu], [B["UB"]])
                        ybanks = []
                        for h8 in range(2):
                            py, bpy = bank()
                            for hh in range(8):
                                h = h8 * 8 + hh; kc = h // 2; po = (h % 2) * 64
                                o_ = py[:, hh * 64:(hh + 1) * 64]
                                mm(o_, TAR[:, kc, 1, :], SBFm[h % 2][:, kc, :], True, False, [B["TAR"], B[f"SBFm{h % 2}"]], [bpy], sig=False)
                                mm(o_, MB[:, h, 1, :], UB[:, h, :], False, False, [B["MB"], B["UB"]], [bpy], sig=False)
                                mm(o_, MK[:, h, 1, :], VBF[:, h * 64:(h + 1) * 64], False, True, [B["MK"], B["VBF"]], [bpy], sig=(hh == 7))
                            ybanks.append((py, bpy))
                        pss, bpss = bank()
                        for h in range(H):
                            kc = h // 2; po = (h % 2) * 64
                            o_ = pss[po:po + 64, kc * 64:(kc + 1) * 64]
                            mm(o_, BH[:, h * 64:(h + 1) * 64], UB[:, h, :], True, False, [B["BH"], B["UB"]], [bpss], sig=False)
                            mm(o_, KH[:, h * 64:(h + 1) * 64], VBF[:, h * 64:(h + 1) * 64], False, True, [B["KH"], B["VBF"]], [bpss], sig=(h == H - 1))
                        for kc in range(8):
                            stt(S32[:, kc, :], S32[:, kc, :], PC[:, kc:kc + 1], pss[:, kc * 64:(kc + 1) * 64], ALU.mult, ALU.add,
                                [B["S32"], B["PC"], bpss], [B["S32"]])
                        y32 = r32
                        for h8, (py, bpy) in enumerate(ybanks):
                            act(y32[:, h8 * 512:(h8 + 1) * 512], py[:, 0:512], AF.Copy, [bpy], [bR])
                        cp("gpsimd", SBF[:], S32[:], [B["S32"]], [B["SBF"]])
                        P.op("vector", lambda e: e.tensor_reduce(out=SM[:, :, 0], in_=y32[:].rearrange("p (h c) -> p h c", h=H), axis=AX.X, op=ALU.add), [bR], [B["SM"]])
                        tt("gpsimd", t2[:], y32[:], y32[:], ALU.mult, [bR], [bT2])
                        P.op("vector", lambda e: e.tensor_reduce(out=SM[:, :, 1], in_=t2[:].rearrange("p (h c) -> p h c", h=H), axis=AX.X, op=ALU.add), [bT2], [B["SM"]])
                        ts("vector", SM[:, :, 0], SM[:, :, 0], 1.0 / 64, None, ALU.mult, None, [B["SM"]], [B["SM"]])
                        tt("vector", SM[:, :, 3], SM[:, :, 0], SM[:, :, 0], ALU.mult, [B["SM"]], [B["SM"]])
                        stt(SM[:, :, 1], SM[:, :, 1], 1.0 / 64, SM[:, :, 3], ALU.mult, ALU.subtract, [B["SM"]], [B["SM"]])
                        ts("vector", SM[:, :, 1], SM[:, :, 1], 64e-5, None, ALU.add, None, [B["SM"]], [B["SM"]])
                        tt("gpsimd", SM[:, :, 3], SM[:, :, 1], NH[:, 0:H], ALU.pow, [B["SM"], B["NH"]], [B["SM"]])
                        for h in range(H):
                            hs = slice(h * 64, (h + 1) * 64)
                            ts("vector", y32[:, hs], y32[:, hs], SM[:, h, 0:1], SM[:, h, 3:4], ALU.subtract, ALU.mult, [bR, B["SM"]], [bR])
                        tt("gpsimd", y32[:], y32[:], GNW[:], ALU.mult, [bR, B["GNW"]], [bR])
                        tt("gpsimd", y32[:], y32[:], GNB[:], ALU.add, [bR, B["GNB"]], [bR])
                        for h in range(H):
                            hs = slice(h * 64, (h + 1) * 64)
                            stt(y32[:, hs], v32[:, hs], SM[:, h, 2:3], y32[:, hs], ALU.mult, ALU.add, [bV, B["SM"], bR], [bR])
                        rr = lora2(LG1, 128, 3)
                        for hf, (pb, bpb) in enumerate(rr):
                            tt("vector", OB[0][:, hf * 512:(hf + 1) * 512], pb[:, 0:512], y32[:, hf * 512:(hf + 1) * 512], ALU.mult, [bpb, bR], [B["OB0"]])
                        for kc in range(8):
                            tr(PT[:, kc * 128:(kc + 1) * 128], OB[0][:, kc * 128:(kc + 1) * 128], IDENT[:], [B["OB0"], B["IDENT"]], [B["PT"]], sig=(kc == 7))
                        cp("vector", YT[:], PT[:].rearrange("p (k t) -> p k t", k=8), [B["PT"]], [B["YT"]])

                        chk(8)
                        def fm_proj(slab_src_fn, rhs_t, brhs, oc):
                            pass
                        for q in range(2):
                            RO_, BRO = slab(S_["ro"][:, q * 4:(q + 1) * 4].rearrange("p a k c -> p (a k c)"), 4096)
                            ZR_, BZR = slab(S_["fm"][:, 16 + q * 4:16 + (q + 1) * 4].rearrange("p a k c -> p (a k c)"), 4096)
                            for o4 in range(4):
                                oc = q * 4 + o4
                                py_, bpy_ = bank(); pz_, bpz_ = bank()
                                ROv = RO_[:, 0:4096].rearrange("p (a k c) -> p a k c", a=4, k=8)
                                ZRv = ZR_[:, 0:4096].rearrange("p (a k c) -> p a k c", a=4, k=8)
                                for kc in range(8):
                                    mm(py_[:, 0:NT], ROv[:, o4, kc, :], YT[:, kc, :], kc == 0, kc == 7, [BRO, B["YT"]], [bpy_], sig=(kc == 7))
                                for kc in range(8):
                                    mm(pz_[:, 0:NT], ZRv[:, o4, kc, :], XNT[:, kc, 1:1 + NT], kc == 0, kc == 7, [BZR, B["XNT"]], [bpz_], sig=(kc == 7))
                                tf = TMPF[0]
                                act(tf[:], pz_[:, 0:NT], AF.Tanh, [bpz_], [B["TMPF0"]], scale=0.5)
                                sigm_from_tanh("gpsimd", tf[:], [B["TMPF0"]], [B["TMPF0"]])
                                tt("vector", ACC[:, oc, :], py_[:, 0:NT], tf[:], ALU.mult, [bpy_, B["TMPF0"]], [B["ACC"]])
                        for q in range(2):
                            CO_, BCO = slab(S_["co"][:, q * 4:(q + 1) * 4].rearrange("p a k c -> p (a k c)"), 4096)
                            ZC_, BZC = slab(S_["fm"][:, 24 + q * 4:24 + (q + 1) * 4].rearrange("p a k c -> p (a k c)"), 4096)
                            for o4 in range(4):
                                oc = q * 4 + o4
                                py_, bpy_ = bank(); pz_, bpz_ = bank()
                                COv = CO_[:, 0:4096].rearrange("p (a k c) -> p a k c", a=4, k=8)
                                ZCv = ZC_[:, 0:4096].rearrange("p (a k c) -> p a k c", a=4, k=8)
                                for kc in range(8):
                                    mm(py_[:, 0:NT], COv[:, o4, kc, :], CCT[:, kc, :], kc == 0, kc == 7, [BCO, B["CCT"]], [bpy_], sig=(kc == 7))
                                for kc in range(8):
                                    mm(pz_[:, 0:NT], ZCv[:, o4, kc, :], XNT[:, kc, 1:1 + NT], kc == 0, kc == 7, [BZC, B["XNT"]], [bpz_], sig=(kc == 7))
                                tf = TMPF[1]
                                act(tf[:], pz_[:, 0:NT], AF.Tanh, [bpz_], [B["TMPF1"]], scale=0.5)
                                sigm_from_tanh("gpsimd", tf[:], [B["TMPF1"]], [B["TMPF1"]])
                                tt("vector", tf[:], py_[:, 0:NT], tf[:], ALU.mult, [bpy_, B["TMPF1"]], [B["TMPF1"]])
                                tt("gpsimd", MRG[:, oc, :], tf[:], ACC[:, oc, :], ALU.add, [B["TMPF1"], B["ACC"]], [B["MRG"]])

                        def out_norm_resid(pbs, gtile, gname):
                            for hf, (pb, bpb) in enumerate(pbs):
                                act(JUNK[:, 0:512], pb[:, 0:512], AF.Square, [bpb], [B["JUNK"], B["SS"]], accum_out=SS[:, hf:hf + 1])
                            tt("vector", MS[:, 0:1], SS[:, 0:1], SS[:, 1:2], ALU.add, [B["SS"]], [B["MS"]])
                            ts("vector", MS[:, 0:1], MS[:, 0:1], 1.0 / D, 1e-6, ALU.mult, ALU.add, [B["MS"]], [B["MS"]])
                            tt("gpsimd", RSTD[:, 0:1], MS[:, 0:1], NH[:, 0:1], ALU.pow, [B["MS"], B["NH"]], [B["RSTD"]])
                            for hf, (pb, bpb) in enumerate(pbs):
                                hs = slice(hf * 512, (hf + 1) * 512)
                                stt(A[5][:, hs], pb[:, 0:512], RSTD[:, 0:1], gtile[:, hs], ALU.mult, ALU.mult, [bpb, B["RSTD"], B[gname]], [B["A5"]])
                            tt("gpsimd", X[:, 0, :], X[:, 0, :], A[5][:], ALU.add, [B["X"], B["A5"]], [B["X"]])

                        WO_, BWO = [], []
                        pbs = []
                        for hf in range(2):
                            w_, bw_ = slab(S_["wo"][:, :, hf * 512:(hf + 1) * 512], 4096)
                            wv = w_[:, 0:4096].rearrange("p (k c) -> p k c", k=8)
                            pb, bpb = bank()
                            for kc in range(8):
                                mm(pb[:, 0:512], MRG[:, kc, :], wv[:, kc, :], kc == 0, kc == 7, [B["MRG"], bw_], [bpb], sig=(kc == 7))
                            pbs.append((pb, bpb))
                        out_norm_resid(pbs, GPOST, "GPOST")
                        if ti == 0 and seq == 0 and l == layers[0]:
                            dump("d_x1", X[:, 0, :], [128, D], F32, [B["X"]])
                            dump("d_yt", YT[:], [128, 8, NT], BF16, [B["YT"]])
                            dump("d_cct", CCT[:], [128, 8, NT], BF16, [B["CCT"]])
                            dump("d_mrg", MRG[:], [128, 8, NT], BF16, [B["MRG"]])
                            dump("d_xnt", XNT[:], [128, 8, NT + 1], BF16, [B["XNT"]])
                            dump("d_v", A[2][:], [128, D], F32, [B["A2"]])
                            dump("d_k", A[1][:], [128, D], F32, [B["A1"]])
                            dump("d_y", A[0][:], [128, D], F32, [B["A0"]])
                            dump("d_b", A[3][:], [128, D], F32, [B["A3"]])
                            dump("d_lw", A[4][:], [128, D], F32, [B["A4"]])
                        rmsnorm_to(XNT, 1, V_GFPRE)
                        for q in range(6):
                            n4 = 4 if q < 5 else 2
                            G_, BG_ = slab(S_["g"][:, q * 4:q * 4 + n4].rearrange("p a k c -> p (a k c)"), n4 * 1024)
                            U_, BU_ = slab(S_["u"][:, q * 4:q * 4 + n4].rearrange("p a k c -> p (a k c)"), n4 * 1024)
                            Gv = G_[:, 0:n4 * 1024].rearrange("p (a k c) -> p a k c", a=n4, k=8)
                            Uv = U_[:, 0:n4 * 1024].rearrange("p (a k c) -> p a k c", a=n4, k=8)
                            for o4 in range(n4):
                                oc = q * 4 + o4
                                pg_, bpg_ = bank(); pu_, bpu_ = bank()
                                for kc in range(8):
                                    mm(pg_[:, 0:NT], Gv[:, o4, kc, :], XNT[:, kc, 1:1 + NT], kc == 0, kc == 7, [BG_, B["XNT"]], [bpg_], sig=(kc == 7))
                                for kc in range(8):
                                    mm(pu_[:, 0:NT], Uv[:, o4, kc, :], XNT[:, kc, 1:1 + NT], kc == 0, kc == 7, [BU_, B["XNT"]], [bpu_], sig=(kc == 7))
                                tf = TMPF[oc % 2]; btf = B[f"TMPF{oc % 2}"]
                                act(tf[:], pg_[:, 0:NT], AF.Tanh, [bpg_], [btf], scale=0.5)
                                sigm_from_tanh("gpsimd", tf[:], [btf], [btf])
                                tt("vector", tf[:], pg_[:, 0:NT], tf[:], ALU.mult, [bpg_, btf], [btf])
                                tt("vector", HT[:, oc, :], pu_[:, 0:NT], tf[:], ALU.mult, [bpu_, btf], [B["HT"]])
                        pbs = []
                        for hf in range(2):
                            pb, bpb = bank()
                            for q in range(3):
                                n8 = 8 if q < 2 else 6
                                w_, bw_ = slab(S_["d"][:, q * 8:q * 8 + n8, hf * 512:(hf + 1) * 512], n8 * 512)
                                wv = w_[:, 0:n8 * 512].rearrange("p (k c) -> p k c", k=n8)
                                for k8 in range(n8):
                                    kc = q * 8 + k8
                                    mm(pb[:, 0:512], HT[:, kc, :], wv[:, k8, :], kc == 0, kc == NFF - 1, [B["HT"], bw_], [bpb], sig=(k8 == n8 - 1))
                            pbs.append((pb, bpb))
                        out_norm_resid(pbs, GFPOST, "GFPOST")
                        dma(out[seq, t0:t0 + NT, :], X[:, 0, :], [B["X"]], (), "dxo")
        except StopBuild:
            pass
        sems = {}
        for k in list(Plan.ENGS) + list(P.dma_cnt.keys()):
            sems[k] = st.enter_context(nc.semaphore("zq_" + k + "_sm"))
        P.emit(nc, sems, {"sync": ([("dxo", P.dma_cnt["dxo"])] if "dxo" in P.dma_cnt else []) + ([("vfo", P.dma_cnt["vfo"])] if "vfo" in P.dma_cnt else []) + [(d_, 16) for d_ in dumps]})
    return nc, P


def _fm_layout(W):
    n = W.shape[1] // 128
    return np.ascontiguousarray(W.reshape(8, 128, n, 128).transpose(1, 2, 0, 3))


def _tok_layout(W):
    K = W.shape[0] // 128
    return np.ascontiguousarray(W.reshape(K, 128, W.shape[1]).transpose(1, 0, 2))


def host_consts():
    s = np.arange(128)[:, None]
    t = np.arange(128)[None, :]
    c = {}
    c["ident"] = np.eye(128).astype(ml_dtypes.bfloat16)
    c["tri3"] = np.ascontiguousarray(np.stack([(s <= t), (s < t), (s > t)], axis=1).astype(np.float32))
    c["mskL"] = np.ascontiguousarray(np.tile((s > t).astype(np.float32), (1, 4)))
    mT = np.concatenate([(t > s), (t >= s)], axis=1).astype(np.float32)
    c["mskT"] = np.ascontiguousarray(np.tile(mT, (1, 2)))
    c["ident4"] = np.ascontiguousarray(np.tile(np.eye(128, dtype=np.float32), (1, 4)))
    return c


def host_layer(inp, l):
    f = lambda a: np.asarray(a, dtype=np.float32)
    w_in = f(inp["w_in"][l])
    d = {}
    d[f"wtok{l}"] = _tok_layout(w_in[:, 0:3072])
    d[f"wfm{l}"] = _fm_layout(w_in[:, 3072:7168])
    v1 = f(inp["vres_1"][l - 1]) if l > 0 else np.zeros((D, 32), np.float32)
    d[f"wl1{l}"] = _tok_layout(np.concatenate([f(inp["decay_w1"][l]), f(inp["a_1"][l]), f(inp["g_1"][l]), v1], axis=1))
    l2 = np.zeros((128, 4, D), np.float32)
    l2[0:64, 0] = f(inp["decay_w2"][l]); l2[64, 0] = f(inp["decay_w0"][l])
    l2[0:64, 1] = f(inp["a_2"][l]); l2[64, 1] = f(inp["a_0"][l])
    if l > 0:
        l2[0:32, 2] = f(inp["vres_2"][l - 1]); l2[32, 2] = f(inp["vres_0"][l - 1])
    l2[0:128, 3] = f(inp["g_2"][l])
    d[f"wl2{l}"] = l2
    d[f"wro{l}"] = _fm_layout(f(inp["w_rwkv_out"][l]))
    d[f"wco{l}"] = _fm_layout(f(inp["w_conv_out"][l]))
    d[f"wo{l}"] = _tok_layout(f(inp["w_out"][l]))
    d[f"wg{l}"] = _fm_layout(f(inp["ffn_w_gate"][l]))
    d[f"wu{l}"] = _fm_layout(f(inp["ffn_w_up"][l]))
    d[f"wd{l}"] = _tok_layout(f(inp["ffn_w_down"][l]))
    vres_mu = f(inp["vres_mu"][l - 1]) if l > 0 else np.zeros(D, np.float32)
    rows = [inp["pre_mix_norm"][l], inp["post_mix_norm"][l], inp["pre_ffn_norm"][l], inp["post_ffn_norm"][l],
            inp["mu_rkv"][l][0], inp["mu_rkv"][l][1], inp["mu_rkv"][l][2],
            inp["mu_wag"][l][0], inp["mu_wag"][l][1], inp["mu_wag"][l][2], vres_mu,
            inp["k_k"][l], inp["k_a"][l], np.asarray(inp["r_k"][l]).reshape(-1), inp["gn_w"][l], inp["gn_b"][l],
            inp["conv_b"][l], inp["conv_ln_w"][l], inp["conv_ln_b"][l]]
    d[f"vec{l}"] = np.ascontiguousarray(np.stack([f(r) for r in rows], axis=0))
    d[f"vecT{l}"] = np.ascontiguousarray(d[f"vec{l}"].reshape(NVEC, 8, 128).transpose(2, 1, 0))
    d[f"dwT{l}"] = np.ascontiguousarray(f(inp["conv_dw"][l]).reshape(31, 8, 128).transpose(2, 1, 0))
    return d


_PROG = {}


def _get_prog(T, nseq, layers):
    key = (T, nseq, tuple(0 if l == 0 else 1 for l in layers), len(layers))
    if key not in _PROG:
        canon = [0] if layers == [0] else ([1] if len(layers) == 1 else list(layers))
        _PROG[key] = build(T, nseq, canon)[0]
    return _PROG[key]


def kernel(**inputs):
    x = np.asarray(inputs["x"], dtype=np.float32)
    Bt, T, _ = x.shape
    consts = host_consts()
    vf = None
    cur = x
    for l in range(4):
        lay = host_layer(inputs, l)
        canon = 0 if l == 0 else 1
        lay = {k[:-1] + str(canon): v for k, v in lay.items()}
        outs = [None] * Bt
        vfs = [None] * Bt
        for half in range(Bt // 8):
            nc = _get_prog(T, 1, [l])
            in_maps = []
            for c in range(8):
                b = half * 8 + c
                m = {"x": np.ascontiguousarray(cur[b:b + 1])}
                m.update(consts)
                m.update(lay)
                if l > 0:
                    m["vf"] = np.ascontiguousarray(vf[b:b + 1])
                in_maps.append(m)
            res = run_bass_kernel_spmd(nc, in_maps, core_ids=list(range(8)))
            for c in range(8):
                b = half * 8 + c
                outs[b] = res.results[c]["out"]
                if l == 0:
                    vfs[b] = res.results[c]["vf"]
        cur = np.concatenate(outs, axis=0)
        if l == 0:
            vf = np.concatenate(vfs, axis=0)
    return cur.astype(np.float32)
```

```python
import numpy as np
import ml_dtypes
from contextlib import ExitStack
import concourse.bass as bass
import concourse.mybir as mybir
from concourse.bass_utils import run_bass_kernel_spmd

F32 = mybir.dt.float32
BF16 = mybir.dt.bfloat16
AF = mybir.ActivationFunctionType
ALU = mybir.AluOpType
AX = mybir.AxisListType

D = 1024
H = 16
NFF = 22
NT = 128
NS = NT // 128
NVEC = 19
(V_GPRE, V_GPOST, V_GFPRE, V_GFPOST, V_MUR, V_MUK, V_MUV, V_MUW, V_MUA, V_MUG, V_MUVR, V_KK, V_KA, V_RK,
 V_GNW, V_GNB, V_CB, V_LNW, V_LNB) = range(NVEC)


class Buf:
    __slots__ = ("name", "w", "r")

    def __init__(self, name=""):
        self.name = name
        self.w = None
        self.r = []


class Plan:
    ENGS = ("tensor", "vector", "scalar", "gpsimd", "sync")

    def __init__(self):
        self.streams = {e: [] for e in self.ENGS}
        self.cnt = {e: 0 for e in self.ENGS}
        self.waited = {e: {} for e in self.ENGS}
        self.dma_cnt = {}

    def _need(self, eng, ev, waits):
        if ev is None:
            return
        k, v = ev
        if k == eng and v > self.cnt[eng]:
            return
        if self.waited[eng].get(k, 0) >= v:
            return
        if waits.get(k, 0) < v:
            waits[k] = v

    def op(self, eng, fn, reads=(), writes=(), sig=True, dma_sem=None):
        waits = {}
        for b in reads:
            self._need(eng, b.w, waits)
        for b in writes:
            self._need(eng, b.w, waits)
            for ev in b.r:
                self._need(eng, ev, waits)
        wl = []
        for k, v in waits.items():
            self.waited[eng][k] = v
            wl.append((k, v))
        if dma_sem is not None:
            self.dma_cnt[dma_sem] = self.dma_cnt.get(dma_sem, 0) + 16
            ev = (dma_sem, self.dma_cnt[dma_sem])
            self.streams[eng].append((wl, fn, dma_sem, 16))
        elif sig:
            self.cnt[eng] += 1
            ev = (eng, self.cnt[eng])
            self.streams[eng].append((wl, fn, eng, 1))
        else:
            self.streams[eng].append((wl, fn, None, 0))
            ev = (eng, self.cnt[eng] + 1)
        for b in reads:
            if len(b.r) > 8:
                b.r = [e for e in b.r if not (e[0] == ev[0] and e[1] <= ev[1])]
            b.r.append(ev)
        for b in writes:
            b.w = ev
            b.r = []

    def barrier(self):
        evs = [(e, self.cnt[e]) for e in self.ENGS if self.cnt[e] > 0]
        evs += [(k, v) for k, v in self.dma_cnt.items()]
        for e in self.ENGS:
            wl = []
            for (k, v) in evs:
                if k == e:
                    continue
                if self.waited[e].get(k, 0) < v:
                    self.waited[e][k] = v
                    wl.append((k, v))
            if wl:
                self.streams[e].append((wl, None, None, 0))

    def emit(self, nc, sems, final_waits):
        with nc.Block() as block:
            def mk(engname):
                def body(e):
                    for (wl, fn, sk, inc) in self.streams[engname]:
                        for (k, v) in wl:
                            e.wait_ge(sems[k], v)
                        if fn is None:
                            continue
                        ins = fn(e)
                        if sk is not None:
                            ins.then_inc(sems[sk], inc)
                    for (k, v) in final_waits.get(engname, []):
                        e.wait_ge(sems[k], v)
                return body
            block.tensor(mk("tensor"))
            block.vector(mk("vector"))
            block.scalar(mk("scalar"))
            block.gpsimd(mk("gpsimd"))
            block.sync(mk("sync"))


class StopBuild(Exception):
    pass


def build(T, nseq, layers, dbg=False, kstop=0):
    def chk(n):
        if kstop == n:
            raise StopBuild()
    nc = bass.Bass("TRN2", target_bir_lowering=False)
    P = Plan()
    NL = len(layers)
    dr = {}

    def din(name, shape, dt=F32):
        dr[name] = nc.dram_tensor(name, list(shape), dt, kind="ExternalInput").ap()
        return dr[name]

    x_in = din("x", [nseq, T, D])
    ident_d = din("ident", [128, 128], BF16)
    tri_d = din("tri3", [128, 3, 128])
    mskL_d = din("mskL", [128, 512])
    mskT_d = din("mskT", [128, 512])
    id4_d = din("ident4", [128, 512])
    WN = {}
    for l in layers:
        WN[l] = dict(
            tok=din(f"wtok{l}", [128, 8, 3072]), fm=din(f"wfm{l}", [128, 32, 8, 128]),
            l1=din(f"wl1{l}", [128, 8, 288]), l2=din(f"wl2{l}", [128, 4, 1024]),
            ro=din(f"wro{l}", [128, 8, 8, 128]), co=din(f"wco{l}", [128, 8, 8, 128]),
            wo=din(f"wo{l}", [128, 8, 1024]), g=din(f"wg{l}", [128, NFF, 8, 128]),
            u=din(f"wu{l}", [128, NFF, 8, 128]), d=din(f"wd{l}", [128, NFF, 1024]),
            vec=din(f"vec{l}", [NVEC, D]), vecT=din(f"vecT{l}", [128, 8, NVEC]), dw=din(f"dwT{l}", [128, 8, 31]))
    out = nc.dram_tensor("out", [nseq, T, D], F32, kind="ExternalOutput").ap()
    if 0 in layers and NL == 1:
        vf_d = nc.dram_tensor("vf", [nseq, T, D], F32, kind="ExternalOutput").ap()
    elif 0 in layers:
        vf_d = nc.dram_tensor("vf", [nseq, T, D], F32).ap()
    else:
        vf_d = din("vf", [nseq, T, D])
    SC = {}
    for l in layers:
        SC[l] = dict(
            tok=nc.dram_tensor(f"s_tok{l}", [128, 12, 16, 256], BF16).ap(),
            fm=nc.dram_tensor(f"s_fm{l}", [128, 32, 8, 128], BF16).ap(),
            l1=nc.dram_tensor(f"s_l1{l}", [128, 8, 2, 288], BF16).ap(),
            l2=nc.dram_tensor(f"s_l2{l}", [128, 4, 1024], BF16).ap(),
            ro=nc.dram_tensor(f"s_ro{l}", [128, 8, 8, 128], BF16).ap(),
            co=nc.dram_tensor(f"s_co{l}", [128, 8, 8, 128], BF16).ap(),
            wo=nc.dram_tensor(f"s_wo{l}", [128, 8, 1024], BF16).ap(),
            g=nc.dram_tensor(f"s_g{l}", [128, NFF, 8, 128], BF16).ap(),
            u=nc.dram_tensor(f"s_u{l}", [128, NFF, 8, 128], BF16).ap(),
            d=nc.dram_tensor(f"s_d{l}", [128, NFF, 1024], BF16).ap())

    with ExitStack() as st:
        def sb(name, shape, dt=F32):
            return st.enter_context(nc.sbuf_tensor(name, list(shape), dt))
        B = {}

        def nb(name):
            B[name] = Buf(name)
            return B[name]

        def act(out_, in_, func, R, W, **kw):
            P.op("scalar", lambda e: e.activation(out=out_, in_=in_, func=func, **kw), R, W)

        def ts(eng, out_, in0, s1, s2, op0, op1, R, W):
            if s2 is None:
                P.op(eng, lambda e: e.tensor_scalar(out=out_, in0=in0, scalar1=s1, scalar2=None, op0=op0), R, W)
            else:
                P.op(eng, lambda e: e.tensor_scalar(out=out_, in0=in0, scalar1=s1, scalar2=s2, op0=op0, op1=op1), R, W)

        def tt(eng, out_, in0, in1, op, R, W):
            P.op(eng, lambda e: e.tensor_tensor(out=out_, in0=in0, in1=in1, op=op), R, W)

        def stt(out_, in0, scalar, in1, op0, op1, R, W):
            P.op("vector", lambda e: e.scalar_tensor_tensor(out=out_, in0=in0, scalar=scalar, in1=in1, op0=op0, op1=op1), R, W)

        def cp(eng, out_, in_, R, W):
            P.op(eng, lambda e: e.tensor_copy(out=out_, in_=in_), R, W)

        def mset(eng, ap, val, W):
            P.op(eng, lambda e: e.memset(ap, val), (), W)

        def mm(out_, lhsT, rhs, start, stop, R, W, sig):
            P.op("tensor", lambda e: e.matmul(out_, lhsT, rhs, start=start, stop=stop), R, W, sig=sig)

        def tr(out_, in_, idn, R, W, sig):
            P.op("tensor", lambda e: e.transpose(out_, in_, idn), R, W, sig=sig)

        dma_rr = [0]

        def dma(out_, in_, R, W, sem, eng="sync"):
            P.op(eng, lambda e: e.dma_start(out=out_, in_=in_), R, W, dma_sem=sem)

        dumps = []

        def dump(name, ap_, shape, dt_, bufs):
            if not dbg:
                return
            t_ = nc.dram_tensor(name, list(shape), dt_, kind="ExternalOutput").ap()
            dma(t_, ap_, bufs, (), "dbg_" + name)
            dumps.append("dbg_" + name)

        IDENT = sb("IDENT", [128, 128], BF16); nb("IDENT")
        TRI = sb("TRI", [128, 3, 128]); nb("TRI")
        MSKL = sb("MSKL", [128, 512]); nb("MSKL")
        MSKT = sb("MSKT", [128, 512]); nb("MSKT")
        ID4 = sb("ID4", [128, 512]); nb("ID4")
        ONES32 = sb("ONES32", [128, 128]); nb("ONES32")
        NH = sb("NH", [128, NT]); nb("NH")
        dma(IDENT[:], ident_d, (), [B["IDENT"]], "c0")
        dma(TRI[:], tri_d, (), [B["TRI"]], "c1")
        dma(MSKL[:], mskL_d, (), [B["MSKL"]], "c2")
        dma(MSKT[:], mskT_d, (), [B["MSKT"]], "c3")
        dma(ID4[:], id4_d, (), [B["ID4"]], "c4")
        mset("gpsimd", ONES32[:], 1.0, [B["ONES32"]])
        mset("gpsimd", NH[:], -0.5, [B["NH"]])

        with ExitStack() as pst:
            def psb(name, shape, dt=F32):
                return pst.enter_context(nc.sbuf_tensor(name, list(shape), dt))
            STG = [psb(f"STG{i}", [128, 4096]) for i in range(2)]
            STB = [psb(f"STB{i}", [128, 4608], BF16) for i in range(2)]
            for i in range(2):
                nb(f"STG{i}"); nb(f"STB{i}")
            VB = psb("VB", [128, 3, 1024]); nb("VB")
            VB1 = psb("VB1", [128, 3, 1024]); nb("VB1")
            VT = psb("VT", [128, 8, NVEC]); nb("VT")
            VT1 = psb("VT1", [128, 8, 4]); nb("VT1")
            pk = [0]

            def plain(dst2d, src2d, n):
                for c0 in range(0, n, 4096):
                    c1 = min(n, c0 + 4096)
                    i = pk[0] % 2; pk[0] += 1
                    dma(STG[i][:, 0:c1 - c0], src2d[:, c0:c1], (), [B[f"STG{i}"]], f"pi{i}")
                    if i == 0:
                        cp("vector", STB[i][:, 0:c1 - c0], STG[i][:, 0:c1 - c0], [B[f"STG{i}"]], [B[f"STB{i}"]])
                    else:
                        act(STB[i][:, 0:c1 - c0], STG[i][:, 0:c1 - c0], AF.Copy, [B[f"STG{i}"]], [B[f"STB{i}"]])
                    dma(dst2d[:, c0:c1], STB[i][:, 0:c1 - c0], [B[f"STB{i}"]], (), f"po{i}", eng="gpsimd")

            for l in layers:
                W_, S_ = WN[l], SC[l]
                for j in range(3):
                    dma(VB[:, j, :], W_["vec"][V_MUR + j, :].partition_broadcast(128), (), [B["VB"]], "pv0")
                dma(VT[:], W_["vecT"], (), [B["VT"]], "pv1")
                ts("vector", VB1[:], VB[:], -1.0, 1.0, ALU.mult, ALU.add, [B["VB"]], [B["VB1"]])
                ts("vector", VT1[:], VT[:, :, V_MUW:V_MUW + 4], -1.0, 1.0, ALU.mult, ALU.add, [B["VT"]], [B["VT1"]])
                for j in range(12):
                    i = pk[0] % 2; pk[0] += 1
                    wi = j // 4
                    dma(STG[i][:, 0:2048].rearrange("p (k c) -> p k c", k=8), W_["tok"][:, :, j * 256:(j + 1) * 256],
                        (), [B[f"STG{i}"]], f"pi{i}")
                    for s in range(2):
                        vb = (VB1 if s == 0 else VB)
                        for kc in range(8):
                            tt("vector" if kc % 2 == 0 else "gpsimd",
                               STB[i][:, (kc * 2 + s) * 256:(kc * 2 + s + 1) * 256],
                               STG[i][:, kc * 256:(kc + 1) * 256], vb[:, wi, (j % 4) * 256:(j % 4 + 1) * 256], ALU.mult,
                               [B[f"STG{i}"], B["VB"], B["VB1"]], [B[f"STB{i}"]])
                    dma(S_["tok"][:, j, :, :], STB[i][:, 0:4096].rearrange("p (k c) -> p k c", k=16),
                        [B[f"STB{i}"]], (), f"po{i}", eng="gpsimd")
                i = pk[0] % 2; pk[0] += 1
                dma(STG[i][:, 0:2304].rearrange("p (k c) -> p k c", k=8), W_["l1"], (), [B[f"STG{i}"]], f"pi{i}")
                for (c0, c1, mi) in ((0, 64, 0), (64, 128, 1), (128, 256, 2), (256, 288, 3)):
                    for kc in range(8):
                        for s in range(2):
                            sc_ = (VT1[:, kc, mi:mi + 1] if s == 0 else VT[:, kc, V_MUW + mi:V_MUW + mi + 1])
                            ts("vector", STB[i][:, (kc * 2 + s) * 288 + c0:(kc * 2 + s) * 288 + c1],
                               STG[i][:, kc * 288 + c0:kc * 288 + c1], sc_, None, ALU.mult, None,
                               [B[f"STG{i}"], B["VT"], B["VT1"]], [B[f"STB{i}"]])
                dma(S_["l1"].rearrange("p k s c -> p (k s c)"), STB[i][:, 0:4608],
                    [B[f"STB{i}"]], (), f"po{i}", eng="gpsimd")
                plain(S_["l2"].rearrange("p a c -> p (a c)"), W_["l2"].rearrange("p a c -> p (a c)"), 4096)
                plain(S_["fm"].rearrange("p a k c -> p (a k c)"), W_["fm"].rearrange("p a k c -> p (a k c)"), 32768)
                plain(S_["ro"].rearrange("p a k c -> p (a k c)"), W_["ro"].rearrange("p a k c -> p (a k c)"), 8192)
                plain(S_["co"].rearrange("p a k c -> p (a k c)"), W_["co"].rearrange("p a k c -> p (a k c)"), 8192)
                plain(S_["wo"].rearrange("p k c -> p (k c)"), W_["wo"].rearrange("p k c -> p (k c)"), 8192)
                plain(S_["g"].rearrange("p a k c -> p (a k c)"), W_["g"].rearrange("p a k c -> p (a k c)"), NFF * 1024)
                plain(S_["u"].rearrange("p a k c -> p (a k c)"), W_["u"].rearrange("p a k c -> p (a k c)"), NFF * 1024)
                plain(S_["d"].rearrange("p k c -> p (k c)"), W_["d"].rearrange("p k c -> p (k c)"), NFF * 1024)
            P.barrier()
        RING = [sb(f"RING{i}", [128, 4608], BF16) for i in range(3)]
        for i in range(3):
            nb(f"RING{i}")
        rk = [0]

        def slab(src2d, n):
            i = rk[0] % 3; rk[0] += 1
            dma(RING[i][:, 0:n], src2d, (), [B[f"RING{i}"]], f"rg{i}")
            return RING[i], B[f"RING{i}"]

        PS = [st.enter_context(nc.psum_tensor(f"PSB{i}", [128, 512], F32)) for i in range(7)]
        PT = st.enter_context(nc.psum_tensor("PTB", [128, 1024], BF16)); nb("PT")
        for i in range(7):
            nb(f"PS{i}")
        pk2 = [0]

        def bank():
            i = pk2[0] % 7; pk2[0] += 1
            return PS[i], B[f"PS{i}"]

        def T_(name, shape, dt=F32):
            nb(name)
            return sb(name, shape, dt)
        X = T_("X", [128, NS, D])
        XNT = T_("XNT", [128, 8, NT + 1], BF16)
        CARRY = T_("CARRY", [128, 8, 1], BF16)
        L2BUF = T_("L2BUF", [128, 4096], BF16)
        CB = T_("CB", [128, 8, NT + 30])
        ACC = T_("ACC", [128, 8, NT])
        HT = T_("HT", [128, NFF, NT], BF16)
        CCT = T_("CCT", [128, 8, NT], BF16)
        YT = T_("YT", [128, 8, NT], BF16)
        MRG = T_("MRG", [128, 8, NT], BF16)
        XNB = T_("XNB", [128, D], BF16)
        SS = T_("SS", [128, 4]); MS = T_("MS", [128, 4]); RSTD = T_("RSTD", [128, 4])
        LW1 = T_("LW1", [65, NT], BF16); LA1 = T_("LA1", [65, NT], BF16); LV1 = T_("LV1", [33, NT], BF16)
        LG1 = T_("LG1", [128, NT], BF16)
        TMPF = [T_(f"TMPF{i}", [128, NT]) for i in range(2)]
        MEAN = T_("MEAN", [128, NT]); VAR = T_("VAR", [128, NT]); RS = T_("RS", [128, NT])
        PV = T_("PV", [128, 8, NVEC])
        HLN = T_("HLN", [128, 8, 2])
        DW = T_("DW", [128, 8, 31])
        GPOST = T_("GPOST", [128, D]); GFPOST = T_("GFPOST", [128, D])
        KKT = T_("KKT", [128, D]); KAT = T_("KAT", [128, D]); RKT = T_("RKT", [128, D])
        GNW = T_("GNW", [128, D]); GNB = T_("GNB", [128, D])
        A = [T_(f"A{i}", [128, D]) for i in range(7)]
        EB = [T_(f"EB{i}", [128, D], BF16) for i in range(4)]
        OB = [T_(f"OB{i}", [128, D], BF16) for i in range(2)]
        JUNK = OB[1]; B["JUNK"] = B["OB1"]
        KH = T_("KH", [128, D], BF16); BH = T_("BH", [128, D], BF16); VBF = T_("VBF", [128, D], BF16)
        TAR = T_("TAR", [128, 8, 2, 128], BF16)
        TBT = T_("TBT", [128, 8, 128], BF16); TKT = T_("TKT", [128, 8, 128], BF16)
        SM = T_("SM", [128, 16, 4])
        S32 = T_("S32", [128, 8, 64]); SBF = T_("SBF", [128, 8, 64], BF16); PC = T_("PC", [128, 8])
        XB = T_("XB", [128, 16, 64], BF16); UB = T_("UB", [128, 16, 64], BF16)
        R0 = [T_(f"R0_{g}", [128, 512], BF16) for g in range(1)]
        QRG = [[T_(f"QRG{g}_{i}", [128, 3, 512], BF16) for i in range(2)] for g in range(1)]
        MB = T_("MB", [128, 16, 2, 128], BF16)
        MK = T_("MK", [128, 16, 2, 128], BF16)
        G7 = T_("G7", [128, 16, 128], BF16)
        TARm = [T_(f"TARm{i}", [128, 8, 2, 128], BF16) for i in range(2)]
        TBTm = [T_(f"TBTm{i}", [128, 8, 128], BF16) for i in range(2)]
        SBFm = [T_(f"SBFm{i}", [128, 8, 64], BF16) for i in range(2)]
        for i_ in range(2):
            for (tn, tl) in (("TARm", TARm), ("TBTm", TBTm), ("SBFm", SBFm)):
                mset("vector", tl[i_][:], 0.0, [B[f"{tn}{i_}"]])

        mset("vector", LW1[64:65, :], 1.0, [B["LW1"]])
        mset("vector", LA1[64:65, :], 1.0, [B["LA1"]])
        mset("vector", LV1[32:33, :], 1.0, [B["LV1"]])

        def rmsnorm_to(dst, coff, gcol):
            for s_ in range(NS):
                act(JUNK[:], X[:, s_, :], AF.Square, [B["X"]], [B["JUNK"], B["SS"]], accum_out=SS[:, 0:1])
                ts("vector", MS[:, 0:1], SS[:, 0:1], 1.0 / D, 1e-6, ALU.mult, ALU.add, [B["SS"]], [B["MS"]])
                tt("gpsimd", RSTD[:, 0:1], MS[:, 0:1], NH[:, 0:1], ALU.pow, [B["MS"], B["NH"]], [B["RSTD"]])
                act(XNB[:], X[:, s_, :], AF.Copy, [B["X"], B["RSTD"]], [B["XNB"]], scale=RSTD[:, 0:1])
                for kc in range(8):
                    tr(PT[:, kc * 128:(kc + 1) * 128], XNB[:, kc * 128:(kc + 1) * 128], IDENT[:],
                       [B["XNB"], B["IDENT"]], [B["PT"]], sig=(kc == 7))
                for kc in range(8):
                    ts("vector" if kc % 2 else "gpsimd" if False else "vector",
                       dst[:, kc, coff + s_ * 128:coff + (s_ + 1) * 128], PT[:, kc * 128:(kc + 1) * 128],
                       PV[:, kc, gcol:gcol + 1], None, ALU.mult, None, [B["PT"], B["PV"]], [B[dst_name[id(dst)]]])

        dst_name = {id(XNT): "XNT"}

        def sigm_from_tanh(eng, ap, R, W):
            ts(eng, ap, ap, 0.5, 0.5, ALU.mult, ALU.add, R, W)

        OUTB = {}
        VFB = {}
        try:
            chk(1)
            for l in layers:
                W_, S_ = WN[l], SC[l]
                has_v = (l != 0)
                dma(PV[:], W_["vecT"], (), [B["PV"]], "lp0")
                dma(DW[:], W_["dw"], (), [B["DW"]], "lp1")
                for (tile_, bn, row) in ((GPOST, "GPOST", V_GPOST), (GFPOST, "GFPOST", V_GFPOST), (KKT, "KKT", V_KK),
                                         (KAT, "KAT", V_KA), (RKT, "RKT", V_RK), (GNW, "GNW", V_GNW), (GNB, "GNB", V_GNB)):
                    dma(tile_[:], W_["vec"][row, :].partition_broadcast(128), (), [B[bn]], "lp_" + bn)
                ts("vector", HLN[:], PV[:, :, V_LNW:V_LNW + 2], 0.5, None, ALU.mult, None, [B["PV"]], [B["HLN"]])
                dma(L2BUF[:], S_["l2"].rearrange("p a c -> p (a c)"), (), [B["L2BUF"]], "lp2")
                for seq in range(nseq):
                    mset("vector", S32[:], 0.0, [B["S32"]])
                    mset("vector", SBF[:], 0.0, [B["SBF"]])
                    mset("gpsimd", CB[:, :, 0:30], 0.0, [B["CB"]])
                    for ti in range(T // NT):
                        t0 = ti * NT
                        xsrc = (x_in if l == layers[0] else out)[seq, t0:t0 + NT, :].rearrange("(s p) d -> p s d", p=128)
                        ob_ = OUTB.setdefault((seq, ti), Buf())
                        vb_ = VFB.setdefault((seq, ti), Buf())
                        dma(X[:], xsrc, ([] if l == layers[0] else [ob_]), [B["X"]], "dx")
                        if ti == 0:
                            mset("vector", XNT[:, :, 0:1], 0.0, [B["XNT"]])
                        else:
                            cp("vector", XNT[:, :, 0:1], CARRY[:], [B["CARRY"]], [B["XNT"]])
                        rmsnorm_to(XNT, 1, V_GPRE)
                        cp("vector", CARRY[:], XNT[:, :, NT:NT + 1], [B["XNT"]], [B["CARRY"]])
                        chk(2)
                        L1, BL1 = slab(S_["l1"].rearrange("p k s c -> p (k s c)"), 4608)
                        L1v = L1[:, 0:4608].rearrange("p (k s c) -> p k s c", k=8, s=2)
                        for (c0, c1, dstt, dn, fn_) in ((0, 64, LW1, "LW1", AF.Tanh), (64, 128, LA1, "LA1", AF.Copy),
                                                       (128, 256, LG1, "LG1", AF.Tanh), (256, 288, LV1, "LV1", AF.Copy)):
                            if c0 == 256 and not has_v:
                                continue
                            M_ = c1 - c0
                            pb, bpb = bank()
                            for kc in range(8):
                                for s2 in range(2):
                                    mm(pb[0:M_, 0:NT], L1v[:, kc, s2, c0:c1], XNT[:, kc, 1 - s2:1 - s2 + NT],
                                       kc == 0 and s2 == 0, kc == 7 and s2 == 1, [BL1, B["XNT"]], [bpb], sig=(kc == 7 and s2 == 1))
                            if dn == "LG1":
                                act(LG1[:], pb[0:128, 0:NT], AF.Tanh, [bpb], [B["LG1"]], scale=0.5)
                                sigm_from_tanh("gpsimd", LG1[:], [B["LG1"]], [B["LG1"]])
                            else:
                                act(dstt[0:M_, :], pb[0:M_, 0:NT], fn_, [bpb], [B[dn]])
                        BL2 = B["L2BUF"]
                        L2v = L2BUF[:, 0:4096].rearrange("p (a c) -> p a c", a=4)
                        chk(3)
                        for q in range(2):
                            FU, BFU = slab(S_["fm"][:, q * 4:(q + 1) * 4].rearrange("p a k c -> p (a k c)"), 4096)
                            FG, BFG = slab(S_["fm"][:, 8 + q * 4:8 + (q + 1) * 4].rearrange("p a k c -> p (a k c)"), 4096)
                            FUv = FU[:, 0:4096].rearrange("p (a k c) -> p a k c", a=4, k=8)
                            FGv = FG[:, 0:4096].rearrange("p (a k c) -> p a k c", a=4, k=8)
                            for o4 in range(4):
                                oc = q * 4 + o4
                                bu, bbu = bank(); bg, bbg = bank()
                                for kc in range(8):
                                    mm(bu[:, 0:NT], FUv[:, o4, kc, :], XNT[:, kc, 1:1 + NT], kc == 0, kc == 7, [BFU, B["XNT"]], [bbu], sig=(kc == 7))
                                for kc in range(8):
                                    mm(bg[:, 0:NT], FGv[:, o4, kc, :], XNT[:, kc, 1:1 + NT], kc == 0, kc == 7, [BFG, B["XNT"]], [bbg], sig=(kc == 7))
                                tf = TMPF[oc % 2]; btf = B[f"TMPF{oc % 2}"]
                                act(tf[:], bg[:, 0:NT], AF.Tanh, [bbg], [btf], scale=0.5)
                                sigm_from_tanh("gpsimd", tf[:], [btf], [btf])
                                tt("vector", CB[:, oc, 30:30 + NT], bu[:, 0:NT], tf[:], ALU.mult, [bbu, btf], [B["CB"]])
                        for oc in range(8):
                            ts("vector", ACC[:, oc, :], CB[:, oc, 0:NT], DW[:, oc, 0:1], PV[:, oc, V_CB:V_CB + 1], ALU.mult, ALU.add,
                               [B["CB"], B["DW"], B["PV"]], [B["ACC"]])
                            for j in range(1, 31):
                                stt(ACC[:, oc, :], CB[:, oc, j:j + NT], DW[:, oc, j:j + 1], ACC[:, oc, :], ALU.mult, ALU.add,
                                    [B["CB"], B["DW"], B["ACC"]], [B["ACC"]])
                        cp("gpsimd", CB[:, :, 0:30], CB[:, :, NT:NT + 30], [B["CB"]], [B["CB"]])
                        s1, bs1 = bank(); s2b, bs2 = bank()
                        for oc in range(8):
                            mm(s1[:, 0:NT], ONES32[:], ACC[:, oc, :], oc == 0, oc == 7, [B["ONES32"], B["ACC"]], [bs1], sig=(oc == 7))
                        for oc in range(8):
                            tf = TMPF[oc % 2]; btf = B[f"TMPF{oc % 2}"]
                            act(tf[:], ACC[:, oc, :], AF.Square, [B["ACC"]], [btf])
                            mm(s2b[:, 0:NT], ONES32[:], tf[:], oc == 0, oc == 7, [B["ONES32"], btf], [bs2], sig=True)
                        act(MEAN[:], s1[:, 0:NT], AF.Copy, [bs1], [B["MEAN"]], scale=1.0 / D)
                        act(VAR[:], s2b[:, 0:NT], AF.Copy, [bs2], [B["VAR"]], scale=1.0 / D)
                        tt("gpsimd", RS[:], MEAN[:], MEAN[:], ALU.mult, [B["MEAN"]], [B["RS"]])
                        tt("gpsimd", VAR[:], VAR[:], RS[:], ALU.subtract, [B["VAR"], B["RS"]], [B["VAR"]])
                        ts("gpsimd", VAR[:], VAR[:], 1e-5, None, ALU.add, None, [B["VAR"]], [B["VAR"]])
                        tt("gpsimd", RS[:], VAR[:], NH[:], ALU.pow, [B["VAR"], B["NH"]], [B["RS"]])
                        for oc in range(8):
                            t1 = TMPF[0]; t2 = TMPF[1]
                            tt("vector", t1[:], ACC[:, oc, :], MEAN[:], ALU.subtract, [B["ACC"], B["MEAN"]], [B["TMPF0"]])
                            tt("gpsimd", t1[:], t1[:], RS[:], ALU.mult, [B["TMPF0"], B["RS"]], [B["TMPF0"]])
                            act(t2[:], t1[:], AF.Tanh, [B["TMPF0"], B["HLN"]], [B["TMPF1"]], scale=HLN[:, oc, 0:1], bias=HLN[:, oc, 1:2])
                            ts("vector", t1[:], t1[:], PV[:, oc, V_LNW:V_LNW + 1], PV[:, oc, V_LNB:V_LNB + 1], ALU.mult, ALU.add,
                               [B["TMPF0"], B["PV"]], [B["TMPF0"]])
                            sigm_from_tanh("gpsimd", t2[:], [B["TMPF1"]], [B["TMPF1"]])
                            tt("vector", CCT[:, oc, :], t1[:], t2[:], ALU.mult, [B["TMPF0"], B["TMPF1"]], [B["CCT"]])

                        chk(4)
                        r32, k32, v32, asg, lw, t1, t2 = A
                        bR, bK, bV, bAS, bLW, bT1, bT2 = [B[f"A{i}"] for i in range(7)]
                        for j in range(12):
                            TK, BTK = slab(S_["tok"][:, j].rearrange("p k c -> p (k c)"), 4096)
                            TKv = TK[:, 0:4096].rearrange("p (k c) -> p k c", k=16)
                            pb, bpb = bank()
                            for kc in range(8):
                                for s2 in range(2):
                                    mm(pb[:, 0:256], XNT[:, kc, 1 - s2:1 - s2 + NT], TKv[:, kc * 2 + s2, :],
                                       kc == 0 and s2 == 0, kc == 7 and s2 == 1, [BTK, B["XNT"]], [bpb], sig=(kc == 7 and s2 == 1))
                            dstA = A[j // 4]
                            act(dstA[:, (j % 4) * 256:(j % 4 + 1) * 256], pb[:, 0:256], AF.Copy, [bpb], [B[f"A{j // 4}"]])
                        def lora2(src, K_, a_idx):
                            res = []
                            for hf in range(2):
                                pb, bpb = bank()
                                mm(pb[:, 0:512], src[0:K_, :], L2v[0:K_, a_idx, hf * 512:(hf + 1) * 512], True, True,
                                   [B["LW1"], B["LA1"], B["LV1"], B["LG1"], BL2], [bpb], sig=True)
                                res.append((pb, bpb))
                            return res
                        if l == 0:
                            dma(vf_d[seq, t0:t0 + NT, :], v32[:], [bV], [vb_], "vfo")
                        else:
                            VF = t2
                            dma(VF[:], vf_d[seq, t0:t0 + NT, :], [vb_], [bT2], "vfi")
                            rr = lora2(LV1, 33, 2)
                            for hf, (pb, bpb) in enumerate(rr):
                                act(t1[:, hf * 512:(hf + 1) * 512], pb[:, 0:512], AF.Tanh, [bpb], [bT1], scale=0.5)
                            sigm_from_tanh("gpsimd", t1[:], [bT1], [bT1])
                            tt("gpsimd", VF[:], VF[:], v32[:], ALU.subtract, [bT2, bV], [bT2])
                            tt("gpsimd", VF[:], VF[:], t1[:], ALU.mult, [bT2, bT1], [bT2])
                            tt("gpsimd", v32[:], v32[:], VF[:], ALU.add, [bV, bT2], [bV])
                        rr = lora2(LA1, 65, 1)
                        for hf, (pb, bpb) in enumerate(rr):
                            act(asg[:, hf * 512:(hf + 1) * 512], pb[:, 0:512], AF.Tanh, [bpb], [bAS], scale=0.5)
                        sigm_from_tanh("gpsimd", asg[:], [bAS], [bAS])
                        rr = lora2(LW1, 65, 0)
                        for hf, (pb, bpb) in enumerate(rr):
                            act(lw[:, hf * 512:(hf + 1) * 512], pb[:, 0:512], AF.Tanh, [bpb], [bLW], scale=0.5)
                        ts("gpsimd", lw[:], lw[:], -0.30326533, -0.30326533, ALU.mult, ALU.add, [bLW], [bLW])
                        chk(5)
                        for (ti_, dsts) in ((0, ((EB[0], "EB0", 1.0), (EB[1], "EB1", -1.0))), (1, ((EB[2], "EB2", 1.0),)), (2, ((EB[3], "EB3", 1.0),))):
                            for hf in range(2):
                                pb, bpb = bank()
                                mm(pb[:, 0:512], TRI[:, ti_, :], lw[:, hf * 512:(hf + 1) * 512], True, True, [B["TRI"], bLW], [bpb], sig=True)
                                for (dd, dn, sc_) in dsts:
                                    act(dd[:, hf * 512:(hf + 1) * 512], pb[:, 0:512], AF.Exp, [bpb], [B[dn]], scale=sc_)
                        pb, bpb = bank()
                        for h in range(H):
                            po = (h % 2) * 64
                            mm(pb[po:po + 64, h // 2:h // 2 + 1], lw[:, h * 64:(h + 1) * 64], ONES32[:, 0:1], True, True,
                               [bLW, B["ONES32"]], [bpb], sig=(h == H - 1))
                        act(PC[:], pb[:, 0:8], AF.Exp, [bpb], [B["PC"]])
                        tt("gpsimd", t1[:], k32[:], KKT[:], ALU.mult, [bK, B["KKT"]], [bT1])
                        tt("gpsimd", t2[:], t1[:], t1[:], ALU.mult, [bT1], [bT2])
                        P.op("vector", lambda e: e.tensor_reduce(out=SM[:, :, 0], in_=t2[:].rearrange("p (h c) -> p h c", h=H), axis=AX.X, op=ALU.add), [bT2], [B["SM"]])
                        ts("vector", SM[:, :, 0], SM[:, :, 0], 1e-24, None, ALU.max, None, [B["SM"]], [B["SM"]])
                        tt("gpsimd", SM[:, :, 1], SM[:, :, 0], NH[:, 0:H], ALU.pow, [B["SM"], B["NH"]], [B["SM"]])
                        for h in range(H):
                            ts("vector", t1[:, h * 64:(h + 1) * 64], t1[:, h * 64:(h + 1) * 64], SM[:, h, 1:2], None, ALU.mult, None, [bT1, B["SM"]], [bT1])
                        kk = t1
                        stt(t2[:], asg[:], -1.0, KAT[:], ALU.add, ALU.mult, [bAS, B["KAT"]], [bT2])
                        stt(k32[:], t2[:], 1.0, k32[:], ALU.add, ALU.mult, [bT2, bK], [bK])
                        tt("gpsimd", t2[:], r32[:], k32[:], ALU.mult, [bR, bK], [bT2])
                        tt("gpsimd", t2[:], t2[:], RKT[:], ALU.mult, [bT2, B["RKT"]], [bT2])
                        P.op("vector", lambda e: e.tensor_reduce(out=SM[:, :, 2], in_=t2[:].rearrange("p (h c) -> p h c", h=H), axis=AX.X, op=ALU.add), [bT2], [B["SM"]])
                        tt("gpsimd", asg[:], asg[:], kk[:], ALU.mult, [bAS, bT1], [bAS])
                        bvec = asg
                        tt("vector", KH[:], k32[:], EB[3][:], ALU.mult, [bK, B["EB3"]], [B["KH"]])
                        tt("gpsimd", BH[:], bvec[:], EB[3][:], ALU.mult, [bAS, B["EB3"]], [B["BH"]])
                        cp("gpsimd", VBF[:], v32[:], [bV], [B["VBF"]])
                        def trans_to(srcf, bsrc, eb, ebn, neg, dst_fn, dname, oi):
                            ob = OB[oi]; bob = B[f"OB{oi}"]
                            if neg:
                                stt(ob[:], srcf[:], -1.0, eb[:], ALU.mult, ALU.mult, [bsrc, B[ebn]], [bob])
                            else:
                                tt("vector", ob[:], srcf[:], eb[:], ALU.mult, [bsrc, B[ebn]], [bob])
                            for kc in range(8):
                                tr(PT[:, kc * 128:(kc + 1) * 128], ob[:, kc * 128:(kc + 1) * 128], IDENT[:], [bob, B["IDENT"]], [B["PT"]], sig=(kc == 7))
                            cp("vector", dst_fn, PT[:].rearrange("p (k t) -> p k t", k=8), [B["PT"]], [B[dname]])
                        trans_to(r32, bR, EB[0], "EB0", False, TAR[:, :, 1, :], "TAR", 0)
                        trans_to(kk, bT1, EB[2], "EB2", True, TAR[:, :, 0, :], "TAR", 1)
                        trans_to(bvec, bAS, EB[1], "EB1", False, TBT[:], "TBT", 0)
                        trans_to(k32, bK, EB[1], "EB1", False, TKT[:], "TKT", 1)
                        for i_ in range(2):
                            ps_ = slice(i_ * 64, (i_ + 1) * 64)
                            cp("vector", TARm[i_][ps_], TAR[ps_], [B["TAR"]], [B[f"TARm{i_}"]])
                            cp("gpsimd", TBTm[i_][ps_], TBT[ps_], [B["TBT"]], [B[f"TBTm{i_}"]])
                            cp("gpsimd", SBFm[i_][ps_], SBF[ps_], [B["SBF"]], [B[f"SBFm{i_}"]])
                        chk(6)
                        for g4 in range(4):
                            gi = 0
                            pl, bpl = bank()
                            for hh in range(4):
                                h = g4 * 4 + hh; kc = h // 2; po = (h % 2) * 64
                                mm(pl[:, hh * 128:(hh + 1) * 128], TAR[:, kc, 0, :], TBTm[h % 2][:, kc, :], True, True,
                                   [B["TAR"], B[f"TBTm{h % 2}"]], [bpl], sig=(hh == 3))
                            tt("vector", R0[gi][:], pl[:, 0:512], MSKL[:], ALU.mult, [bpl, B["MSKL"]], [B[f"R0_{gi}"]])
                            for (lt, ltn, dstm, dmn) in ((TBT, "TBT", MB, "MB"), (TKT, "TKT", MK, "MK")):
                                for h2 in range(2):
                                    pb, bpb = bank()
                                    for hh in range(2):
                                        h = g4 * 4 + h2 * 2 + hh; kc = h // 2; po = (h % 2) * 64
                                        mm(pb[:, hh * 256:(hh + 1) * 256], lt[:, kc, :], TARm[h % 2][:, kc, :, :].rearrange("p a t -> p (a t)"),
                                           True, True, [B[ltn], B[f"TARm{h % 2}"]], [bpb], sig=(hh == 1))
                                    h0 = g4 * 4 + h2 * 2
                                    tt("vector", dstm[:, h0:h0 + 2, :, :].rearrange("p h a t -> p (h a t)"), pb[:, 0:512], MSKT[:], ALU.mult,
                                       [bpb, B["MSKT"]], [B[dmn]])
                            cur = QRG[gi][0]; nxt = QRG[gi][1]; bcur = B[f"QRG{gi}_0"]; bnxt = B[f"QRG{gi}_1"]
                            cp("vector", cur[:, 0, :].rearrange("p (h t) -> p h t", h=4), MB[:, g4 * 4:g4 * 4 + 4, 0, :], [B["MB"]], [bcur])
                            cp("gpsimd", cur[:, 1, :], R0[gi][:], [B[f"R0_{gi}"]], [bcur])
                            tt("gpsimd", cur[:, 2, :], cur[:, 0, :], ID4[:], ALU.add, [bcur, B["ID4"]], [bcur])
                            for jj in range(1, 7):
                                pq, bpq = bank(); pr, bpr = bank(); pg, bpg = bank()
                                for hh in range(4):
                                    cs_ = slice(hh * 128, (hh + 1) * 128)
                                    if jj < 6:
                                        mm(pq[:, cs_], cur[:, 1, cs_], cur[:, 0, cs_], True, True, [bcur], [bpq], sig=(hh == 3))
                                for hh in range(4):
                                    cs_ = slice(hh * 128, (hh + 1) * 128)
                                    mm(pr[:, cs_], cur[:, 0, cs_], cur[:, 1, cs_], True, True, [bcur], [bpr], sig=(hh == 3))
                                if jj < 6:
                                    act(nxt[:, 0, :], pq[:, 0:512], AF.Copy, [bpq], [bnxt])
                                cp("vector", nxt[:, 1, :], pr[:, 0:512], [bpr], [bnxt])
                                for hh in range(4):
                                    cs_ = slice(hh * 128, (hh + 1) * 128)
                                    mm(pg[:, cs_], nxt[:, 1, cs_], cur[:, 2, cs_], True, True, [bnxt, bcur], [bpg], sig=(hh == 3))
                                if jj < 6:
                                    tt("vector", nxt[:, 2, :], pg[:, 0:512], cur[:, 2, :], ALU.add, [bpg, bcur], [bnxt])
                                else:
                                    tt("vector", G7[:, g4 * 4:g4 * 4 + 4, :].rearrange("p h t -> p (h t)"), pg[:, 0:512], cur[:, 2, :], ALU.add,
                                       [bpg, bcur], [B["G7"]])
                                cur, nxt = nxt, cur; bcur, bnxt = bnxt, bcur
                        chk(7)
                        for h8 in range(2):
                            px, bpx = bank()
                            for hh in range(8):
                                h = h8 * 8 + hh; kc = h // 2; po = (h % 2) * 64
                                mm(px[:, hh * 64:(hh + 1) * 64], TAR[:, kc, 0, :], SBFm[h % 2][:, kc, :], True, False, [B["TAR"], B[f"SBFm{h % 2}"]], [bpx], sig=False)
                                mm(px[:, hh * 64:(hh + 1) * 64], MK[:, h, 0, :], VBF[:, h * 64:(h + 1) * 64], False, True, [B["MK"], B["VBF"]], [bpx], sig=(hh == 7))
                            cp("vector", XB[:, h8 * 8:(h8 + 1) * 8, :].rearrange("p h c -> p (h c)"), px[:, 0:512], [bpx], [B["XB"]])
                        for h8 in range(2):
                            pu, bpu = bank()
                            for hh in range(8):
                                h = h8 * 8 + hh
                                mm(pu[:, hh * 64:(hh + 1) * 64], G7[:, h, :], XB[:, h, :], True, True, [B["G7"], B["XB"]], [bpu], sig=(hh == 7))
                            cp("vector", UB[:, h8 * 8:(h8 + 1) * 8, :].rearrange("p h c -> p (h c)"), pu[:, 0:512], [bpu], [B["UB"]])
                        ybanks = []
                        for h8 in range(2):
                            py, bpy = bank()
                            for hh in range(8):
                                h = h8 * 8 + hh; kc = h // 2; po = (h % 2) * 64
                                o_ = py[:, hh * 64:(hh + 1) * 64]
                                mm(o_, TAR[:, kc, 1, :], SBFm[h % 2][:, kc, :], True, False, [B["TAR"], B[f"SBFm{h % 2}"]], [bpy], sig=False)
                                mm(o_, MB[:, h, 1, :], UB[:, h, :], False, False, [B["MB"], B["UB"]], [bpy], sig=False)
                                mm(o_, MK[:, h, 1, :], VBF[:, h * 64:(h + 1) * 64], False, True, [B["MK"], B["VBF"]], [bpy], sig=(hh == 7))
                            ybanks.append((py, bpy))
                        pss, bpss = bank()
                        for h in range(H):
                            kc = h // 2; po = (h % 2) * 64
                            o_ = pss[po:po + 64, kc * 64:(kc + 1) * 64]
                            mm(o_, BH[:, h * 64:(h + 1) * 64], UB[:, h, :], True, False, [B["BH"], B["UB"]], [bpss], sig=False)
                            mm(o_, KH[:, h * 64:(h + 1) * 64], VBF[:, h * 64:(h + 1) * 64], False, True, [B["KH"], B["VBF"]], [bpss], sig=(h == H - 1))
                        for kc in range(8):
                            stt(S32[:, kc, :], S32[:, kc, :], PC[:, kc:kc + 1], pss[:, kc * 64:(kc + 1) * 64], ALU.mult, ALU.add,
                                [B["S32"], B["PC"], bpss], [B["S32"]])
                        y32 = r32
                        for h8, (py, bpy) in enumerate(ybanks):
                            act(y32[:, h8 * 512:(h8 + 1) * 512], py[:, 0:512], AF.Copy, [bpy], [bR])
                        cp("gpsimd", SBF[:], S32[:], [B["S32"]], [B["SBF"]])
                        P.op("vector", lambda e: e.tensor_reduce(out=SM[:, :, 0], in_=y32[:].rearrange("p (h c) -> p h c", h=H), axis=AX.X, op=ALU.add), [bR], [B["SM"]])
                        tt("gpsimd", t2[:], y32[:], y32[:], ALU.mult, [bR], [bT2])
                        P.op("vector", lambda e: e.tensor_reduce(out=SM[:, :, 1], in_=t2[:].rearrange("p (h c) -> p h c", h=H), axis=AX.X, op=ALU.add), [bT2], [B["SM"]])
                        ts("vector", SM[:, :, 0], SM[:, :, 0], 1.0 / 64, None, ALU.mult, None, [B["SM"]], [B["SM"]])
                        tt("vector", SM[:, :, 3], SM[:, :, 0], SM[:, :, 0], ALU.mult, [B["SM"]], [B["SM"]])
                        stt(SM[:, :, 1], SM[:, :, 1], 1.0 / 64, SM[:, :, 3], ALU.mult, ALU.subtract, [B["SM"]], [B["SM"]])
                        ts("vector", SM[:, :, 1], SM[:, :, 1], 64e-5, None, ALU.add, None, [B["SM"]], [B["SM"]])
                        tt("gpsimd", SM[:, :, 3], SM[:, :, 1], NH[:, 0:H], ALU.pow, [B["SM"], B["NH"]], [B["SM"]])
                        for h in range(H):
                            hs = slice(h * 64, (h + 1) * 64)
                            ts("vector", y32[:, hs], y32[:, hs], SM[:, h, 0:1], SM[:, h, 3:4], ALU.subtract, ALU.mult, [bR, B["SM"]], [bR])
                        tt("gpsimd", y32[:], y32[:], GNW[:], ALU.mult, [bR, B["GNW"]], [bR])
                        tt("gpsimd", y32[:], y32[:], GNB[:], ALU.add, [bR, B["GNB"]], [bR])
                        for h in range(H):
                            hs = slice(h * 64, (h + 1) * 64)
                            stt(y32[:, hs], v32[:, hs], SM[:, h, 2:3], y32[:, hs], ALU.mult, ALU.add, [bV, B["SM"], bR], [bR])
                        rr = lora2(LG1, 128, 3)
                        for hf, (pb, bpb) in enumerate(rr):
                            tt("vector", OB[0][:, hf * 512:(hf + 1) * 512], pb[:, 0:512], y32[:, hf * 512:(hf + 1) * 512], ALU.mult, [bpb, bR], [B["OB0"]])
                        for kc in range(8):
                            tr(PT[:, kc * 128:(kc + 1) * 128], OB[0][:, kc * 128:(kc + 1) * 128], IDENT[:], [B["OB0"], B["IDENT"]], [B["PT"]], sig=(kc == 7))
                        cp("vector", YT[:], PT[:].rearrange("p (k t) -> p k t", k=8), [B["PT"]], [B["YT"]])

                        chk(8)
                        def fm_proj(slab_src_fn, rhs_t, brhs, oc):
                            pass
                        for q in range(2):
                            RO_, BRO = slab(S_["ro"][:, q * 4:(q + 1) * 4].rearrange("p a k c -> p (a k c)"), 4096)
                            ZR_, BZR = slab(S_["fm"][:, 16 + q * 4:16 + (q + 1) * 4].rearrange("p a k c -> p (a k c)"), 4096)
                            for o4 in range(4):
                                oc = q * 4 + o4
                                py_, bpy_ = bank(); pz_, bpz_ = bank()
                                ROv = RO_[:, 0:4096].rearrange("p (a k c) -> p a k c", a=4, k=8)
                                ZRv = ZR_[:, 0:4096].rearrange("p (a k c) -> p a k c", a=4, k=8)
                                for kc in range(8):
                                    mm(py_[:, 0:NT], ROv[:, o4, kc, :], YT[:, kc, :], kc == 0, kc == 7, [BRO, B["YT"]], [bpy_], sig=(kc == 7))
                                for kc in range(8):
                                    mm(pz_[:, 0:NT], ZRv[:, o4, kc, :], XNT[:, kc, 1:1 + NT], kc == 0, kc == 7, [BZR, B["XNT"]], [bpz_], sig=(kc == 7))
                                tf = TMPF[0]
                                act(tf[:], pz_[:, 0:NT], AF.Tanh, [bpz_], [B["TMPF0"]], scale=0.5)
                                sigm_from_tanh("gpsimd", tf[:], [B["TMPF0"]], [B["TMPF0"]])
                                tt("vector", ACC[:, oc, :], py_[:, 0:NT], tf[:], ALU.mult, [bpy_, B["TMPF0"]], [B["ACC"]])
                        for q in range(2):
                            CO_, BCO = slab(S_["co"][:, q * 4:(q + 1) * 4].rearrange("p a k c -> p (a k c)"), 4096)
                            ZC_, BZC = slab(S_["fm"][:, 24 + q * 4:24 + (q + 1) * 4].rearrange("p a k c -> p (a k c)"), 4096)
                            for o4 in range(4):
                                oc = q * 4 + o4
                                py_, bpy_ = bank(); pz_, bpz_ = bank()
                                COv = CO_[:, 0:4096].rearrange("p (a k c) -> p a k c", a=4, k=8)
                                ZCv = ZC_[:, 0:4096].rearrange("p (a k c) -> p a k c", a=4, k=8)
                                for kc in range(8):
                                    mm(py_[:, 0:NT], COv[:, o4, kc, :], CCT[:, kc, :], kc == 0, kc == 7, [BCO, B["CCT"]], [bpy_], sig=(kc == 7))
                                for kc in range(8):
                                    mm(pz_[:, 0:NT], ZCv[:, o4, kc, :], XNT[:, kc, 1:1 + NT], kc == 0, kc == 7, [BZC, B["XNT"]], [bpz_], sig=(kc == 7))
                                tf = TMPF[1]
                                act(tf[:], pz_[:, 0:NT], AF.Tanh, [bpz_], [B["TMPF1"]], scale=0.5)
                                sigm_from_tanh("gpsimd", tf[:], [B["TMPF1"]], [B["TMPF1"]])
                                tt("vector", tf[:], py_[:, 0:NT], tf[:], ALU.mult, [bpy_, B["TMPF1"]], [B["TMPF1"]])
                                tt("gpsimd", MRG[:, oc, :], tf[:], ACC[:, oc, :], ALU.add, [B["TMPF1"], B["ACC"]], [B["MRG"]])

                        def out_norm_resid(pbs, gtile, gname):
                            for hf, (pb, bpb) in enumerate(pbs):
                                act(JUNK[:, 0:512], pb[:, 0:512], AF.Square, [bpb], [B["JUNK"], B["SS"]], accum_out=SS[:, hf:hf + 1])
                            tt("vector", MS[:, 0:1], SS[:, 0:1], SS[:, 1:2], ALU.add, [B["SS"]], [B["MS"]])
                            ts("vector", MS[:, 0:1], MS[:, 0:1], 1.0 / D, 1e-6, ALU.mult, ALU.add, [B["MS"]], [B["MS"]])
                            tt("gpsimd", RSTD[:, 0:1], MS[:, 0:1], NH[:, 0:1], ALU.pow, [B["MS"], B["NH"]], [B["RSTD"]])
                            for hf, (pb, bpb) in enumerate(pbs):
                                hs = slice(hf * 512, (hf + 1) * 512)
                                stt(A[5][:, hs], pb[:, 0:512], RSTD[:, 0:1], gtile[:, hs], ALU.mult, ALU.mult, [bpb, B["RSTD"], B[gname]], [B["A5"]])
                            tt("gpsimd", X[:, 0, :], X[:, 0, :], A[5][:], ALU.add, [B["X"], B["A5"]], [B["X"]])

                        WO_, BWO = [], []
                        pbs = []
                        for hf in range(2):
                            w_, bw_ = slab(S_["wo"][:, :, hf * 512:(hf + 1) * 512], 4096)
                            wv = w_[:, 0:4096].rearrange("p (k c) -> p k c", k=8)
                            pb, bpb = bank()
                            for kc in range(8):
                                mm(pb[:, 0:512], MRG[:, kc, :], wv[:, kc, :], kc == 0, kc == 7, [B["MRG"], bw_], [bpb], sig=(kc == 7))
                            pbs.append((pb, bpb))
                        out_norm_resid(pbs, GPOST, "GPOST")
                        if ti == 0 and seq == 0 and l == layers[0]:
                            dump("d_x1", X[:, 0, :], [128, D], F32, [B["X"]])
                            dump("d_yt", YT[:], [128, 8, NT], BF16, [B["YT"]])
                            dump("d_cct", CCT[:], [128, 8, NT], BF16, [B["CCT"]])
                            dump("d_mrg", MRG[:], [128, 8, NT], BF16, [B["MRG"]])
                            dump("d_xnt", XNT[:], [128, 8, NT + 1], BF16, [B["XNT"]])
                            dump("d_v", A[2][:], [128, D], F32, [B["A2"]])
                            dump("d_k", A[1][:], [128, D], F32, [B["A1"]])
                            dump("d_y", A[0][:], [128, D], F32, [B["A0"]])
                            dump("d_b", A[3][:], [128, D], F32, [B["A3"]])
                            dump("d_lw", A[4][:], [128, D], F32, [B["A4"]])
                        rmsnorm_to(XNT, 1, V_GFPRE)
                        for q in range(6):
                            n4 = 4 if q < 5 else 2
                            G_, BG_ = slab(S_["g"][:, q * 4:q * 4 + n4].rearrange("p a k c -> p (a k c)"), n4 * 1024)
                            U_, BU_ = slab(S_["u"][:, q * 4:q * 4 + n4].rearrange("p a k c -> p (a k c)"), n4 * 1024)
                            Gv = G_[:, 0:n4 * 1024].rearrange("p (a k c) -> p a k c", a=n4, k=8)
                            Uv = U_[:, 0:n4 * 1024].rearrange("p (a k c) -> p a k c", a=n4, k=8)
                            for o4 in range(n4):
                                oc = q * 4 + o4
                                pg_, bpg_ = bank(); pu_, bpu_ = bank()
                                for kc in range(8):
                                    mm(pg_[:, 0:NT], Gv[:, o4, kc, :], XNT[:, kc, 1:1 + NT], kc == 0, kc == 7, [BG_, B["XNT"]], [bpg_], sig=(kc == 7))
                                for kc in range(8):
                                    mm(pu_[:, 0:NT], Uv[:, o4, kc, :], XNT[:, kc, 1:1 + NT], kc == 0, kc == 7, [BU_, B["XNT"]], [bpu_], sig=(kc == 7))
                                tf = TMPF[oc % 2]; btf = B[f"TMPF{oc % 2}"]
                                act(tf[:], pg_[:, 0:NT], AF.Tanh, [bpg_], [btf], scale=0.5)
                                sigm_from_tanh("gpsimd", tf[:], [btf], [btf])
                                tt("vector", tf[:], pg_[:, 0:NT], tf[:], ALU.mult, [bpg_, btf], [btf])
                                tt("vector", HT[:, oc, :], pu_[:, 0:NT], tf[:], ALU.mult, [bpu_, btf], [B["HT"]])
                        pbs = []
                        for hf in range(2):
                            pb, bpb = bank()
                            for q in range(3):
                                n8 = 8 if q < 2 else 6
                                w_, bw_ = slab(S_["d"][:, q * 8:q * 8 + n8, hf * 512:(hf + 1) * 512], n8 * 512)
                                wv = w_[:, 0:n8 * 512].rearrange("p (k c) -> p k c", k=n8)
                                for k8 in range(n8):
                                    kc = q * 8 + k8
                                    mm(pb[:, 0:512], HT[:, kc, :], wv[:, k8, :], kc == 0, kc == NFF - 1, [B["HT"], bw_], [bpb], sig=(k8 == n8 - 1))
                            pbs.append((pb, bpb))
                        out_norm_resid(pbs, GFPOST, "GFPOST")
                        dma(out[seq, t0:t0 + NT, :], X[:, 0, :], [B["X"]], [ob_], "dxo")
        except StopBuild:
            pass
        sems = {}
        for k in list(Plan.ENGS) + list(P.dma_cnt.keys()):
            sems[k] = st.enter_context(nc.semaphore("zq_" + k + "_sm"))
        P.emit(nc, sems, {"sync": ([("dxo", P.dma_cnt["dxo"])] if "dxo" in P.dma_cnt else []) + ([("vfo", P.dma_cnt["vfo"])] if "vfo" in P.dma_cnt else []) + [(d_, 16) for d_ in dumps]})
    return nc, P


def _fm_layout(W):
    n = W.shape[1] // 128
    return np.ascontiguousarray(W.reshape(8, 128, n, 128).transpose(1, 2, 0, 3))


def _tok_layout(W):
    K = W.shape[0] // 128
    return np.ascontiguousarray(W.reshape(K, 128, W.shape[1]).transpose(1, 0, 2))


def host_consts():
    s = np.arange(128)[:, None]
    t = np.arange(128)[None, :]
    c = {}
    c["ident"] = np.eye(128).astype(ml_dtypes.bfloat16)
    c["tri3"] = np.ascontiguousarray(np.stack([(s <= t), (s < t), (s > t)], axis=1).astype(np.float32))
    c["mskL"] = np.ascontiguousarray(np.tile((s > t).astype(np.float32), (1, 4)))
    mT = np.concatenate([(t > s), (t >= s)], axis=1).astype(np.float32)
    c["mskT"] = np.ascontiguousarray(np.tile(mT, (1, 2)))
    c["ident4"] = np.ascontiguousarray(np.tile(np.eye(128, dtype=np.float32), (1, 4)))
    return c


def host_layer(inp, l):
    f = lambda a: np.asarray(a, dtype=np.float32)
    w_in = f(inp["w_in"][l])
    d = {}
    d[f"wtok{l}"] = _tok_layout(w_in[:, 0:3072])
    d[f"wfm{l}"] = _fm_layout(w_in[:, 3072:7168])
    v1 = f(inp["vres_1"][l - 1]) if l > 0 else np.zeros((D, 32), np.float32)
    d[f"wl1{l}"] = _tok_layout(np.concatenate([f(inp["decay_w1"][l]), f(inp["a_1"][l]), f(inp["g_1"][l]), v1], axis=1))
    l2 = np.zeros((128, 4, D), np.float32)
    l2[0:64, 0] = f(inp["decay_w2"][l]); l2[64, 0] = f(inp["decay_w0"][l])
    l2[0:64, 1] = f(inp["a_2"][l]); l2[64, 1] = f(inp["a_0"][l])
    if l > 0:
        l2[0:32, 2] = f(inp["vres_2"][l - 1]); l2[32, 2] = f(inp["vres_0"][l - 1])
    l2[0:128, 3] = f(inp["g_2"][l])
    d[f"wl2{l}"] = l2
    d[f"wro{l}"] = _fm_layout(f(inp["w_rwkv_out"][l]))
    d[f"wco{l}"] = _fm_layout(f(inp["w_conv_out"][l]))
    d[f"wo{l}"] = _tok_layout(f(inp["w_out"][l]))
    d[f"wg{l}"] = _fm_layout(f(inp["ffn_w_gate"][l]))
    d[f"wu{l}"] = _fm_layout(f(inp["ffn_w_up"][l]))
    d[f"wd{l}"] = _tok_layout(f(inp["ffn_w_down"][l]))
    vres_mu = f(inp["vres_mu"][l - 1]) if l > 0 else np.zeros(D, np.float32)
    rows = [inp["pre_mix_norm"][l], inp["post_mix_norm"][l], inp["pre_ffn_norm"][l], inp["post_ffn_norm"][l],
            inp["mu_rkv"][l][0], inp["mu_rkv"][l][1], inp["mu_rkv"][l][2],
            inp["mu_wag"][l][0], inp["mu_wag"][l][1], inp["mu_wag"][l][2], vres_mu,
            inp["k_k"][l], inp["k_a"][l], np.asarray(inp["r_k"][l]).reshape(-1), inp["gn_w"][l], inp["gn_b"][l],
            inp["conv_b"][l], inp["conv_ln_w"][l], inp["conv_ln_b"][l]]
    d[f"vec{l}"] = np.ascontiguousarray(np.stack([f(r) for r in rows], axis=0))
    d[f"vecT{l}"] = np.ascontiguousarray(d[f"vec{l}"].reshape(NVEC, 8, 128).transpose(2, 1, 0))
    d[f"dwT{l}"] = np.ascontiguousarray(f(inp["conv_dw"][l]).reshape(31, 8, 128).transpose(2, 1, 0))
    return d


_PROG = {}


def kernel(**inputs):
    x = np.asarray(inputs["x"], dtype=np.float32)
    Bt, T, _ = x.shape
    nseq = Bt // 8
    key = (T, nseq)
    if key not in _PROG:
        _PROG[key] = build(T, nseq, [0, 1, 2, 3])[0]
    nc = _PROG[key]
    base = host_consts()
    for l in range(4):
        base.update(host_layer(inputs, l))
    in_maps = []
    for c in range(8):
        m = {"x": np.ascontiguousarray(x[c * nseq:(c + 1) * nseq])}
        m.update(base)
        in_maps.append(m)
    res = run_bass_kernel_spmd(nc, in_maps, core_ids=list(range(8)))
    return np.concatenate([res.results[c]["out"] for c in range(8)], axis=0).astype(np.float32)
```

```python
import numpy as np
import ml_dtypes
from contextlib import ExitStack
import concourse.bass as bass
import concourse.mybir as mybir
from concourse.bass_utils import run_bass_kernel_spmd

F32 = mybir.dt.float32
BF16 = mybir.dt.bfloat16
AF = mybir.ActivationFunctionType
ALU = mybir.AluOpType
AX = mybir.AxisListType

D = 1024
H = 16
NFF = 22
NT = 128
NS = NT // 128
NVEC = 19
(V_GPRE, V_GPOST, V_GFPRE, V_GFPOST, V_MUR, V_MUK, V_MUV, V_MUW, V_MUA, V_MUG, V_MUVR, V_KK, V_KA, V_RK,
 V_GNW, V_GNB, V_CB, V_LNW, V_LNB) = range(NVEC)


class Buf:
    __slots__ = ("name", "w", "r")

    def __init__(self, name=""):
        self.name = name
        self.w = None
        self.r = []


class Plan:
    ENGS = ("tensor", "vector", "scalar", "gpsimd", "sync")

    def __init__(self):
        self.streams = {e: [] for e in self.ENGS}
        self.cnt = {e: 0 for e in self.ENGS}
        self.waited = {e: {} for e in self.ENGS}
        self.dma_cnt = {}

    def _need(self, eng, ev, waits):
        if ev is None:
            return
        k, v = ev
        if k == eng and v > self.cnt[eng]:
            return
        if self.waited[eng].get(k, 0) >= v:
            return
        if waits.get(k, 0) < v:
            waits[k] = v

    def op(self, eng, fn, reads=(), writes=(), sig=True, dma_sem=None, noself=False):
        waits = {}
        for b in reads:
            self._need(eng, b.w, waits)
        for b in writes:
            self._need(eng, b.w, waits)
            for ev in b.r:
                self._need(eng, ev, waits)
        wl = []
        for k, v in waits.items():
            if noself and k == eng:
                continue
            self.waited[eng][k] = v
            wl.append((k, v))
        if dma_sem is not None:
            self.dma_cnt[dma_sem] = self.dma_cnt.get(dma_sem, 0) + 16
            ev = (dma_sem, self.dma_cnt[dma_sem])
            self.streams[eng].append((wl, fn, dma_sem, 16))
        elif sig:
            self.cnt[eng] += 1
            ev = (eng, self.cnt[eng])
            self.streams[eng].append((wl, fn, eng, 1))
        else:
            self.streams[eng].append((wl, fn, None, 0))
            ev = (eng, self.cnt[eng] + 1)
        for b in reads:
            if len(b.r) > 8:
                b.r = [e for e in b.r if not (e[0] == ev[0] and e[1] <= ev[1])]
            b.r.append(ev)
        for b in writes:
            b.w = ev
            b.r = []

    def barrier(self):
        evs = [(e, self.cnt[e]) for e in self.ENGS if self.cnt[e] > 0]
        evs += [(k, v) for k, v in self.dma_cnt.items()]
        for e in self.ENGS:
            wl = []
            for (k, v) in evs:
                if k == e:
                    continue
                if self.waited[e].get(k, 0) < v:
                    self.waited[e][k] = v
                    wl.append((k, v))
            if wl:
                self.streams[e].append((wl, None, None, 0))

    def emit(self, nc, sems, final_waits):
        with nc.Block() as block:
            def mk(engname):
                def body(e):
                    for (wl, fn, sk, inc) in self.streams[engname]:
                        for (k, v) in wl:
                            e.wait_ge(sems[k], v)
                        if fn is None:
                            continue
                        ins = fn(e)
                        if sk is not None:
                            ins.then_inc(sems[sk], inc)
                    for (k, v) in final_waits.get(engname, []):
                        e.wait_ge(sems[k], v)
                return body
            block.tensor(mk("tensor"))
            block.vector(mk("vector"))
            block.scalar(mk("scalar"))
            block.gpsimd(mk("gpsimd"))
            block.sync(mk("sync"))


class StopBuild(Exception):
    pass


def build(T, nseq, layers, dbg=False, kstop=0):
    def chk(n):
        if kstop == n:
            raise StopBuild()
    nc = bass.Bass("TRN2", target_bir_lowering=False)
    P = Plan()
    NL = len(layers)
    dr = {}

    def din(name, shape, dt=F32):
        dr[name] = nc.dram_tensor(name, list(shape), dt, kind="ExternalInput").ap()
        return dr[name]

    x_in = din("x", [nseq, T, D])
    ident_d = din("ident", [128, 128], BF16)
    tri_d = din("tri3", [128, 3, 128])
    mskL_d = din("mskL", [128, 512])
    mskT_d = din("mskT", [128, 512])
    id4_d = din("ident4", [128, 512])
    WN = {}
    for l in layers:
        WN[l] = dict(
            tok=din(f"wtok{l}", [128, 8, 3072]), fm=din(f"wfm{l}", [128, 32, 8, 128]),
            l1=din(f"wl1{l}", [128, 8, 288]), l2=din(f"wl2{l}", [128, 4, 1024]),
            ro=din(f"wro{l}", [128, 8, 8, 128]), co=din(f"wco{l}", [128, 8, 8, 128]),
            wo=din(f"wo{l}", [128, 8, 1024]), g=din(f"wg{l}", [128, NFF, 8, 128]),
            u=din(f"wu{l}", [128, NFF, 8, 128]), d=din(f"wd{l}", [128, NFF, 1024]),
            vec=din(f"vec{l}", [NVEC, D]), vecT=din(f"vecT{l}", [128, 8, NVEC]), dw=din(f"dwT{l}", [128, 8, 31]))
    out = nc.dram_tensor("out", [nseq, T, D], F32, kind="ExternalOutput").ap()
    if 0 in layers and NL == 1:
        vf_d = nc.dram_tensor("vf", [nseq, T, D], F32, kind="ExternalOutput").ap()
    elif 0 in layers:
        vf_d = nc.dram_tensor("vf", [nseq, T, D], F32).ap()
    else:
        vf_d = din("vf", [nseq, T, D])
    SC = {}
    for l in layers:
        SC[l] = dict(
            tok=nc.dram_tensor(f"s_tok{l}", [128, 12, 16, 256], BF16).ap(),
            fm=nc.dram_tensor(f"s_fm{l}", [128, 32, 8, 128], BF16).ap(),
            l1=nc.dram_tensor(f"s_l1{l}", [128, 8, 2, 288], BF16).ap(),
            l2=nc.dram_tensor(f"s_l2{l}", [128, 4, 1024], BF16).ap(),
            ro=nc.dram_tensor(f"s_ro{l}", [128, 8, 8, 128], BF16).ap(),
            co=nc.dram_tensor(f"s_co{l}", [128, 8, 8, 128], BF16).ap(),
            wo=nc.dram_tensor(f"s_wo{l}", [128, 8, 1024], BF16).ap(),
            g=nc.dram_tensor(f"s_g{l}", [128, NFF, 8, 128], BF16).ap(),
            u=nc.dram_tensor(f"s_u{l}", [128, NFF, 8, 128], BF16).ap(),
            d=nc.dram_tensor(f"s_d{l}", [128, NFF, 1024], BF16).ap())

    with ExitStack() as st:
        def sb(name, shape, dt=F32):
            return st.enter_context(nc.sbuf_tensor(name, list(shape), dt))
        B = {}

        def nb(name):
            B[name] = Buf(name)
            return B[name]

        def act(out_, in_, func, R, W, **kw):
            P.op("scalar", lambda e: e.activation(out=out_, in_=in_, func=func, **kw), R, W)

        def ts(eng, out_, in0, s1, s2, op0, op1, R, W):
            if s2 is None:
                P.op(eng, lambda e: e.tensor_scalar(out=out_, in0=in0, scalar1=s1, scalar2=None, op0=op0), R, W)
            else:
                P.op(eng, lambda e: e.tensor_scalar(out=out_, in0=in0, scalar1=s1, scalar2=s2, op0=op0, op1=op1), R, W)

        def tt(eng, out_, in0, in1, op, R, W):
            P.op(eng, lambda e: e.tensor_tensor(out=out_, in0=in0, in1=in1, op=op), R, W)

        def stt(out_, in0, scalar, in1, op0, op1, R, W, noself=False):
            P.op("vector", lambda e: e.scalar_tensor_tensor(out=out_, in0=in0, scalar=scalar, in1=in1, op0=op0, op1=op1), R, W, noself=noself)

        def cp(eng, out_, in_, R, W):
            P.op(eng, lambda e: e.tensor_copy(out=out_, in_=in_), R, W)

        def mset(eng, ap, val, W):
            P.op(eng, lambda e: e.memset(ap, val), (), W)

        def mm(out_, lhsT, rhs, start, stop, R, W, sig):
            P.op("tensor", lambda e: e.matmul(out_, lhsT, rhs, start=start, stop=stop), R, W, sig=sig)

        def tr(out_, in_, idn, R, W, sig):
            P.op("tensor", lambda e: e.transpose(out_, in_, idn), R, W, sig=sig)

        dma_rr = [0]

        def dma(out_, in_, R, W, sem, eng="sync"):
            P.op(eng, lambda e: e.dma_start(out=out_, in_=in_), R, W, dma_sem=sem)

        dumps = []

        def dump(name, ap_, shape, dt_, bufs):
            if not dbg:
                return
            t_ = nc.dram_tensor(name, list(shape), dt_, kind="ExternalOutput").ap()
            dma(t_, ap_, bufs, (), "dbg_" + name)
            dumps.append("dbg_" + name)

        IDENT = sb("IDENT", [128, 128], BF16); nb("IDENT")
        TRI = sb("TRI", [128, 3, 128]); nb("TRI")
        MSKL = sb("MSKL", [128, 512]); nb("MSKL")
        MSKT = sb("MSKT", [128, 512]); nb("MSKT")
        ID4 = sb("ID4", [128, 512]); nb("ID4")
        ONES32 = sb("ONES32", [128, 128]); nb("ONES32")
        NH = sb("NH", [128, NT]); nb("NH")
        dma(IDENT[:], ident_d, (), [B["IDENT"]], "c0")
        dma(TRI[:], tri_d, (), [B["TRI"]], "c1")
        dma(MSKL[:], mskL_d, (), [B["MSKL"]], "c2")
        dma(MSKT[:], mskT_d, (), [B["MSKT"]], "c3")
        dma(ID4[:], id4_d, (), [B["ID4"]], "c4")
        mset("gpsimd", ONES32[:], 1.0, [B["ONES32"]])
        mset("gpsimd", NH[:], -0.5, [B["NH"]])

        with ExitStack() as pst:
            def psb(name, shape, dt=F32):
                return pst.enter_context(nc.sbuf_tensor(name, list(shape), dt))
            STG = [psb(f"STG{i}", [128, 4096]) for i in range(2)]
            STB = [psb(f"STB{i}", [128, 4608], BF16) for i in range(2)]
            for i in range(2):
                nb(f"STG{i}"); nb(f"STB{i}")
            VB = psb("VB", [128, 3, 1024]); nb("VB")
            VB1 = psb("VB1", [128, 3, 1024]); nb("VB1")
            VT = psb("VT", [128, 8, NVEC]); nb("VT")
            VT1 = psb("VT1", [128, 8, 4]); nb("VT1")
            pk = [0]

            def plain(dst2d, src2d, n):
                for c0 in range(0, n, 4096):
                    c1 = min(n, c0 + 4096)
                    i = pk[0] % 2; pk[0] += 1
                    dma(STG[i][:, 0:c1 - c0], src2d[:, c0:c1], (), [B[f"STG{i}"]], f"pi{i}")
                    if i == 0:
                        cp("vector", STB[i][:, 0:c1 - c0], STG[i][:, 0:c1 - c0], [B[f"STG{i}"]], [B[f"STB{i}"]])
                    else:
                        act(STB[i][:, 0:c1 - c0], STG[i][:, 0:c1 - c0], AF.Copy, [B[f"STG{i}"]], [B[f"STB{i}"]])
                    dma(dst2d[:, c0:c1], STB[i][:, 0:c1 - c0], [B[f"STB{i}"]], (), f"po{i}", eng="gpsimd")

            for l in layers:
                W_, S_ = WN[l], SC[l]
                for j in range(3):
                    dma(VB[:, j, :], W_["vec"][V_MUR + j, :].partition_broadcast(128), (), [B["VB"]], "pv0")
                dma(VT[:], W_["vecT"], (), [B["VT"]], "pv1")
                ts("vector", VB1[:], VB[:], -1.0, 1.0, ALU.mult, ALU.add, [B["VB"]], [B["VB1"]])
                ts("vector", VT1[:], VT[:, :, V_MUW:V_MUW + 4], -1.0, 1.0, ALU.mult, ALU.add, [B["VT"]], [B["VT1"]])
                for j in range(12):
                    i = pk[0] % 2; pk[0] += 1
                    wi = j // 4
                    dma(STG[i][:, 0:2048].rearrange("p (k c) -> p k c", k=8), W_["tok"][:, :, j * 256:(j + 1) * 256],
                        (), [B[f"STG{i}"]], f"pi{i}")
                    for s in range(2):
                        vb = (VB1 if s == 0 else VB)
                        for kc in range(8):
                            tt("vector" if kc % 2 == 0 else "gpsimd",
                               STB[i][:, (kc * 2 + s) * 256:(kc * 2 + s + 1) * 256],
                               STG[i][:, kc * 256:(kc + 1) * 256], vb[:, wi, (j % 4) * 256:(j % 4 + 1) * 256], ALU.mult,
                               [B[f"STG{i}"], B["VB"], B["VB1"]], [B[f"STB{i}"]])
                    dma(S_["tok"][:, j, :, :], STB[i][:, 0:4096].rearrange("p (k c) -> p k c", k=16),
                        [B[f"STB{i}"]], (), f"po{i}", eng="gpsimd")
                i = pk[0] % 2; pk[0] += 1
                dma(STG[i][:, 0:2304].rearrange("p (k c) -> p k c", k=8), W_["l1"], (), [B[f"STG{i}"]], f"pi{i}")
                for (c0, c1, mi) in ((0, 64, 0), (64, 128, 1), (128, 256, 2), (256, 288, 3)):
                    for kc in range(8):
                        for s in range(2):
                            sc_ = (VT1[:, kc, mi:mi + 1] if s == 0 else VT[:, kc, V_MUW + mi:V_MUW + mi + 1])
                            ts("vector", STB[i][:, (kc * 2 + s) * 288 + c0:(kc * 2 + s) * 288 + c1],
                               STG[i][:, kc * 288 + c0:kc * 288 + c1], sc_, None, ALU.mult, None,
                               [B[f"STG{i}"], B["VT"], B["VT1"]], [B[f"STB{i}"]])
                dma(S_["l1"].rearrange("p k s c -> p (k s c)"), STB[i][:, 0:4608],
                    [B[f"STB{i}"]], (), f"po{i}", eng="gpsimd")
                plain(S_["l2"].rearrange("p a c -> p (a c)"), W_["l2"].rearrange("p a c -> p (a c)"), 4096)
                plain(S_["fm"].rearrange("p a k c -> p (a k c)"), W_["fm"].rearrange("p a k c -> p (a k c)"), 32768)
                plain(S_["ro"].rearrange("p a k c -> p (a k c)"), W_["ro"].rearrange("p a k c -> p (a k c)"), 8192)
                plain(S_["co"].rearrange("p a k c -> p (a k c)"), W_["co"].rearrange("p a k c -> p (a k c)"), 8192)
                plain(S_["wo"].rearrange("p k c -> p (k c)"), W_["wo"].rearrange("p k c -> p (k c)"), 8192)
                plain(S_["g"].rearrange("p a k c -> p (a k c)"), W_["g"].rearrange("p a k c -> p (a k c)"), NFF * 1024)
                plain(S_["u"].rearrange("p a k c -> p (a k c)"), W_["u"].rearrange("p a k c -> p (a k c)"), NFF * 1024)
                plain(S_["d"].rearrange("p k c -> p (k c)"), W_["d"].rearrange("p k c -> p (k c)"), NFF * 1024)
            P.barrier()
        RING = [sb(f"RING{i}", [128, 4608], BF16) for i in range(3)]
        for i in range(3):
            nb(f"RING{i}")
        rk = [0]

        def slab(src2d, n):
            i = rk[0] % 3; rk[0] += 1
            dma(RING[i][:, 0:n], src2d, (), [B[f"RING{i}"]], f"rg{i}")
            return RING[i], B[f"RING{i}"]

        PS = [st.enter_context(nc.psum_tensor(f"PSB{i}", [128, 512], F32)) for i in range(7)]
        PT = st.enter_context(nc.psum_tensor("PTB", [128, 1024], BF16)); nb("PT")
        for i in range(7):
            nb(f"PS{i}")
        pk2 = [0]

        def bank():
            i = pk2[0] % 7; pk2[0] += 1
            return PS[i], B[f"PS{i}"]

        def T_(name, shape, dt=F32):
            nb(name)
            return sb(name, shape, dt)
        X = T_("X", [128, NS, D])
        XNT = T_("XNT", [128, 8, NT + 1], BF16)
        CARRY = T_("CARRY", [128, 8, 1], BF16)
        L2BUF = T_("L2BUF", [128, 4096], BF16)
        CB = T_("CB", [128, 8, NT + 30])
        ACC = T_("ACC", [128, 8, NT])
        CBo = [Buf() for _ in range(8)]
        CTb = None
        ACCo = [Buf() for _ in range(8)]
        CCT = T_("CCT", [128, 8, NT], BF16)
        YT = T_("YT", [128, 8, NT], BF16)
        MRG = T_("MRG", [128, 8, NT], BF16)
        SS = T_("SS", [128, 4]); MS = T_("MS", [128, 4]); RSTD = T_("RSTD", [128, 4])
        LW1 = T_("LW1", [65, NT], BF16); LA1 = T_("LA1", [65, NT], BF16); LV1 = T_("LV1", [33, NT], BF16)
        LG1 = T_("LG1", [128, NT], BF16)
        TMPF = [T_(f"TMPF{i}", [128, NT]) for i in range(2)]
        MEAN = T_("MEAN", [128, NT]); VAR = T_("VAR", [128, NT]); RS = T_("RS", [128, NT])
        PV = T_("PV", [128, 8, NVEC])
        HLN = T_("HLN", [128, 8, 2])
        DW = T_("DW", [128, 8, 31])
        GPOST = T_("GPOST", [128, D]); GFPOST = T_("GFPOST", [128, D])
        KKT = T_("KKT", [128, D]); KAT = T_("KAT", [128, D]); RKT = T_("RKT", [128, D])
        GNW = T_("GNW", [128, D]); GNB = T_("GNB", [128, D])
        A = [T_(f"A{i}", [128, D]) for i in range(7)]
        EB = [T_(f"EB{i}", [128, D], BF16) for i in range(4)]
        OB = [T_(f"OB{i}", [128, D], BF16) for i in range(2)]
        JUNK = OB[1]; B["JUNK"] = B["OB1"]
        XNB = OB[0]; B["XNB"] = B["OB0"]
        KH = T_("KH", [128, D], BF16); BH = T_("BH", [128, D], BF16); VBF = T_("VBF", [128, D], BF16)
        TAR = T_("TAR", [128, 8, 2, 128], BF16)
        TBT = T_("TBT", [128, 8, 128], BF16); TKT = T_("TKT", [128, 8, 128], BF16)
        SM = T_("SM", [128, 16, 4])
        S32 = T_("S32", [128, 8, 64]); SBF = T_("SBF", [128, 8, 64], BF16); PC = T_("PC", [128, 8])
        XB = T_("XB", [128, 16, 64], BF16); UB = T_("UB", [128, 16, 64], BF16)
        R0 = [T_(f"R0_{g}", [128, 512], BF16) for g in range(2)]
        QRG = [[T_(f"QRG{g}_{i}", [128, 3, 512], BF16) for i in range(2)] for g in range(2)]
        MB = T_("MB", [128, 16, 2, 128], BF16)
        MK = T_("MK", [128, 16, 2, 128], BF16)
        G7 = T_("G7", [128, 16, 128], BF16)
        HT = MB[:].rearrange("p h a t -> p (h a t)")[:, 0:NFF * NT].rearrange("p (k t) -> p k t", k=NFF)
        B["HT"] = B["MB"]
        TARm = [T_(f"TARm{i}", [128, 8, 2, 128], BF16) for i in range(2)]
        TBTm = [T_(f"TBTm{i}", [128, 8, 128], BF16) for i in range(2)]
        SBFm = [T_(f"SBFm{i}", [128, 8, 64], BF16) for i in range(2)]
        for i_ in range(2):
            for (tn, tl) in (("TARm", TARm), ("TBTm", TBTm), ("SBFm", SBFm)):
                mset("vector", tl[i_][:], 0.0, [B[f"{tn}{i_}"]])

        mset("vector", LW1[64:65, :], 1.0, [B["LW1"]])
        mset("vector", LA1[64:65, :], 1.0, [B["LA1"]])
        mset("vector", LV1[32:33, :], 1.0, [B["LV1"]])

        def rmsnorm_to(dst, coff, gcol):
            for s_ in range(NS):
                act(JUNK[:], X[:, s_, :], AF.Square, [B["X"]], [B["JUNK"], B["SS"]], accum_out=SS[:, 0:1])
                ts("vector", MS[:, 0:1], SS[:, 0:1], 1.0 / D, 1e-6, ALU.mult, ALU.add, [B["SS"]], [B["MS"]])
                tt("gpsimd", RSTD[:, 0:1], MS[:, 0:1], NH[:, 0:1], ALU.pow, [B["MS"], B["NH"]], [B["RSTD"]])
                act(XNB[:], X[:, s_, :], AF.Copy, [B["X"], B["RSTD"]], [B["XNB"]], scale=RSTD[:, 0:1])
                for kc in range(8):
                    tr(PT[:, kc * 128:(kc + 1) * 128], XNB[:, kc * 128:(kc + 1) * 128], IDENT[:],
                       [B["XNB"], B["IDENT"]], [B["PT"]], sig=(kc == 7))
                for kc in range(8):
                    ts("vector" if kc % 2 else "gpsimd" if False else "vector",
                       dst[:, kc, coff + s_ * 128:coff + (s_ + 1) * 128], PT[:, kc * 128:(kc + 1) * 128],
                       PV[:, kc, gcol:gcol + 1], None, ALU.mult, None, [B["PT"], B["PV"]], [B[dst_name[id(dst)]]])

        dst_name = {id(XNT): "XNT"}

        def sigm_from_tanh(eng, ap, R, W):
            act(ap, ap, AF.Identity, R, W, scale=0.5, bias=0.5)

        OUTB = {}
        VFB = {}
        try:
            chk(1)
            for l in layers:
                W_, S_ = WN[l], SC[l]
                has_v = (l != 0)
                dma(PV[:], W_["vecT"], (), [B["PV"]], "lp0")
                dma(DW[:], W_["dw"], (), [B["DW"]], "lp1")
                for (tile_, bn, row) in ((GPOST, "GPOST", V_GPOST), (GFPOST, "GFPOST", V_GFPOST), (KKT, "KKT", V_KK),
                                         (KAT, "KAT", V_KA), (RKT, "RKT", V_RK), (GNW, "GNW", V_GNW), (GNB, "GNB", V_GNB)):
                    dma(tile_[:], W_["vec"][row, :].partition_broadcast(128), (), [B[bn]], "lp_" + bn)
                ts("vector", HLN[:], PV[:, :, V_LNW:V_LNW + 2], 0.5, None, ALU.mult, None, [B["PV"]], [B["HLN"]])
                dma(L2BUF[:], S_["l2"].rearrange("p a c -> p (a c)"), (), [B["L2BUF"]], "lp2")
                for seq in range(nseq):
                    mset("vector", S32[:], 0.0, [B["S32"]])
                    mset("vector", SBF[:], 0.0, [B["SBF"]])
                    mset("gpsimd", CB[:, :, 0:30], 0.0, CBo)
                    for ti in range(T // NT):
                        t0 = ti * NT
                        xsrc = (x_in if l == layers[0] else out)[seq, t0:t0 + NT, :].rearrange("(s p) d -> p s d", p=128)
                        ob_ = OUTB.setdefault((seq, ti), Buf())
                        vb_ = VFB.setdefault((seq, ti), Buf())
                        dma(X[:], xsrc, ([] if l == layers[0] else [ob_]), [B["X"]], "dx")
                        if ti == 0:
                            mset("vector", XNT[:, :, 0:1], 0.0, [B["XNT"]])
                        else:
                            cp("vector", XNT[:, :, 0:1], CARRY[:], [B["CARRY"]], [B["XNT"]])
                        rmsnorm_to(XNT, 1, V_GPRE)
                        cp("vector", CARRY[:], XNT[:, :, NT:NT + 1], [B["XNT"]], [B["CARRY"]])
                        chk(2)
                        L1, BL1 = slab(S_["l1"].rearrange("p k s c -> p (k s c)"), 4608)
                        L1v = L1[:, 0:4608].rearrange("p (k s c) -> p k s c", k=8, s=2)
                        for (c0, c1, dstt, dn, fn_) in ((0, 64, LW1, "LW1", AF.Tanh), (64, 128, LA1, "LA1", AF.Copy),
                                                       (128, 256, LG1, "LG1", AF.Tanh), (256, 288, LV1, "LV1", AF.Copy)):
                            if c0 == 256 and not has_v:
                                continue
                            M_ = c1 - c0
                            pb, bpb = bank()
                            for kc in range(8):
                                for s2 in range(2):
                                    mm(pb[0:M_, 0:NT], L1v[:, kc, s2, c0:c1], XNT[:, kc, 1 - s2:1 - s2 + NT],
                                       kc == 0 and s2 == 0, kc == 7 and s2 == 1, [BL1, B["XNT"]], [bpb], sig=(kc == 7 and s2 == 1))
                            if dn == "LG1":
                                act(LG1[:], pb[0:128, 0:NT], AF.Tanh, [bpb], [B["LG1"]], scale=0.5)
                                sigm_from_tanh("gpsimd", LG1[:], [B["LG1"]], [B["LG1"]])
                            else:
                                act(dstt[0:M_, :], pb[0:M_, 0:NT], fn_, [bpb], [B[dn]])
                        BL2 = B["L2BUF"]
                        L2v = L2BUF[:, 0:4096].rearrange("p (a c) -> p a c", a=4)
                        chk(3)
                        for q in range(2):
                            FU, BFU = slab(S_["fm"][:, q * 4:(q + 1) * 4].rearrange("p a k c -> p (a k c)"), 4096)
                            FG, BFG = slab(S_["fm"][:, 8 + q * 4:8 + (q + 1) * 4].rearrange("p a k c -> p (a k c)"), 4096)
                            FUv = FU[:, 0:4096].rearrange("p (a k c) -> p a k c", a=4, k=8)
                            FGv = FG[:, 0:4096].rearrange("p (a k c) -> p a k c", a=4, k=8)
                            for o4 in range(4):
                                oc = q * 4 + o4
                                bu, bbu = bank(); bg, bbg = bank()
                                for kc in range(8):
                                    mm(bu[:, 0:NT], FUv[:, o4, kc, :], XNT[:, kc, 1:1 + NT], kc == 0, kc == 7, [BFU, B["XNT"]], [bbu], sig=(kc == 7))
                                for kc in range(8):
                                    mm(bg[:, 0:NT], FGv[:, o4, kc, :], XNT[:, kc, 1:1 + NT], kc == 0, kc == 7, [BFG, B["XNT"]], [bbg], sig=(kc == 7))
                                tf = TMPF[oc % 2]; btf = B[f"TMPF{oc % 2}"]
                                act(tf[:], bg[:, 0:NT], AF.Tanh, [bbg], [btf], scale=0.5)
                                sigm_from_tanh("gpsimd", tf[:], [btf], [btf])
                                tt("vector", CB[:, oc, 30:30 + NT], bu[:, 0:NT], tf[:], ALU.mult, [bbu, btf], [CBo[oc]])
                        for oc in range(8):
                            ts("vector", ACC[:, oc, :], CB[:, oc, 0:NT], DW[:, oc, 0:1], PV[:, oc, V_CB:V_CB + 1], ALU.mult, ALU.add,
                               [CBo[oc], B["DW"], B["PV"]], [ACCo[oc]])
                        for j in range(1, 31):
                            for oc in range(8):
                                stt(ACC[:, oc, :], CB[:, oc, j:j + NT], DW[:, oc, j:j + 1], ACC[:, oc, :], ALU.mult, ALU.add,
                                    [CBo[oc], B["DW"], ACCo[oc]], [ACCo[oc]])
                        cp("gpsimd", CB[:, :, 0:30], CB[:, :, NT:NT + 30], CBo, CBo)
                        s1, bs1 = bank(); s2b, bs2 = bank()
                        for oc in range(8):
                            mm(s1[:, 0:NT], ONES32[:], ACC[:, oc, :], oc == 0, oc == 7, [B["ONES32"], ACCo[oc]], [bs1], sig=(oc == 7))
                        for oc in range(8):
                            tf = TMPF[oc % 2]; btf = B[f"TMPF{oc % 2}"]
                            act(tf[:], ACC[:, oc, :], AF.Square, [ACCo[oc]], [btf])
                            mm(s2b[:, 0:NT], ONES32[:], tf[:], oc == 0, oc == 7, [B["ONES32"], btf], [bs2], sig=True)
                        act(MEAN[:], s1[:, 0:NT], AF.Copy, [bs1], [B["MEAN"]], scale=1.0 / D)
                        act(VAR[:], s2b[:, 0:NT], AF.Copy, [bs2], [B["VAR"]], scale=1.0 / D)
                        tt("gpsimd", RS[:], MEAN[:], MEAN[:], ALU.mult, [B["MEAN"]], [B["RS"]])
                        tt("gpsimd", VAR[:], VAR[:], RS[:], ALU.subtract, [B["VAR"], B["RS"]], [B["VAR"]])
                        ts("gpsimd", VAR[:], VAR[:], 1e-5, None, ALU.add, None, [B["VAR"]], [B["VAR"]])
                        tt("gpsimd", RS[:], VAR[:], NH[:], ALU.pow, [B["VAR"], B["NH"]], [B["RS"]])
                        for oc in range(8):
                            t1 = TMPF[0]; t2 = TMPF[1]
                            tt("vector", t1[:], ACC[:, oc, :], MEAN[:], ALU.subtract, [ACCo[oc], B["MEAN"]], [B["TMPF0"]])
                            tt("gpsimd", t1[:], t1[:], RS[:], ALU.mult, [B["TMPF0"], B["RS"]], [B["TMPF0"]])
                            act(t2[:], t1[:], AF.Tanh, [B["TMPF0"], B["HLN"]], [B["TMPF1"]], scale=HLN[:, oc, 0:1], bias=HLN[:, oc, 1:2])
                            ts("vector", t1[:], t1[:], PV[:, oc, V_LNW:V_LNW + 1], PV[:, oc, V_LNB:V_LNB + 1], ALU.mult, ALU.add,
                               [B["TMPF0"], B["PV"]], [B["TMPF0"]])
                            sigm_from_tanh("gpsimd", t2[:], [B["TMPF1"]], [B["TMPF1"]])
                            tt("vector", CCT[:, oc, :], t1[:], t2[:], ALU.mult, [B["TMPF0"], B["TMPF1"]], [B["CCT"]])

                        chk(4)
                        r32, k32, v32, asg, lw, t1, t2 = A
                        bR, bK, bV, bAS, bLW, bT1, bT2 = [B[f"A{i}"] for i in range(7)]
                        for j in range(12):
                            TK, BTK = slab(S_["tok"][:, j].rearrange("p k c -> p (k c)"), 4096)
                            TKv = TK[:, 0:4096].rearrange("p (k c) -> p k c", k=16)
                            pb, bpb = bank()
                            for kc in range(8):
                                for s2 in range(2):
                                    mm(pb[:, 0:256], XNT[:, kc, 1 - s2:1 - s2 + NT], TKv[:, kc * 2 + s2, :],
                                       kc == 0 and s2 == 0, kc == 7 and s2 == 1, [BTK, B["XNT"]], [bpb], sig=(kc == 7 and s2 == 1))
                            dstA = A[j // 4]
                            act(dstA[:, (j % 4) * 256:(j % 4 + 1) * 256], pb[:, 0:256], AF.Copy, [bpb], [B[f"A{j // 4}"]])
                        def lora2(src, K_, a_idx):
                            res = []
                            for hf in range(2):
                                pb, bpb = bank()
                                mm(pb[:, 0:512], src[0:K_, :], L2v[0:K_, a_idx, hf * 512:(hf + 1) * 512], True, True,
                                   [B["LW1"], B["LA1"], B["LV1"], B["LG1"], BL2], [bpb], sig=True)
                                res.append((pb, bpb))
                            return res
                        if l == 0:
                            dma(vf_d[seq, t0:t0 + NT, :], v32[:], [bV], [vb_], "vfo")
                        else:
                            VF = t2
                            dma(VF[:], vf_d[seq, t0:t0 + NT, :], [vb_], [bT2], "vfi")
                            rr = lora2(LV1, 33, 2)
                            for hf, (pb, bpb) in enumerate(rr):
                                act(t1[:, hf * 512:(hf + 1) * 512], pb[:, 0:512], AF.Tanh, [bpb], [bT1], scale=0.5)
                            sigm_from_tanh("gpsimd", t1[:], [bT1], [bT1])
                            tt("gpsimd", VF[:], VF[:], v32[:], ALU.subtract, [bT2, bV], [bT2])
                            tt("gpsimd", VF[:], VF[:], t1[:], ALU.mult, [bT2, bT1], [bT2])
                            tt("gpsimd", v32[:], v32[:], VF[:], ALU.add, [bV, bT2], [bV])
                        rr = lora2(LA1, 65, 1)
                        for hf, (pb, bpb) in enumerate(rr):
                            act(asg[:, hf * 512:(hf + 1) * 512], pb[:, 0:512], AF.Tanh, [bpb], [bAS], scale=0.5)
                        sigm_from_tanh("gpsimd", asg[:], [bAS], [bAS])
                        rr = lora2(LW1, 65, 0)
                        for hf, (pb, bpb) in enumerate(rr):
                            act(lw[:, hf * 512:(hf + 1) * 512], pb[:, 0:512], AF.Tanh, [bpb], [bLW], scale=0.5)
                        ts("gpsimd", lw[:], lw[:], -0.30326533, -0.30326533, ALU.mult, ALU.add, [bLW], [bLW])
                        chk(5)
                        for (ti_, dsts) in ((0, ((EB[0], "EB0", 1.0), (EB[1], "EB1", -1.0))), (1, ((EB[2], "EB2", 1.0),)), (2, ((EB[3], "EB3", 1.0),))):
                            for hf in range(2):
                                pb, bpb = bank()
                                mm(pb[:, 0:512], TRI[:, ti_, :], lw[:, hf * 512:(hf + 1) * 512], True, True, [B["TRI"], bLW], [bpb], sig=True)
                                for (dd, dn, sc_) in dsts:
                                    act(dd[:, hf * 512:(hf + 1) * 512], pb[:, 0:512], AF.Exp, [bpb], [B[dn]], scale=sc_)
                        pb, bpb = bank()
                        for h in range(H):
                            po = (h % 2) * 64
                            mm(pb[po:po + 64, h // 2:h // 2 + 1], lw[:, h * 64:(h + 1) * 64], ONES32[:, 0:1], True, True,
                               [bLW, B["ONES32"]], [bpb], sig=(h == H - 1))
                        act(PC[:], pb[:, 0:8], AF.Exp, [bpb], [B["PC"]])
                        tt("gpsimd", t1[:], k32[:], KKT[:], ALU.mult, [bK, B["KKT"]], [bT1])
                        tt("gpsimd", t2[:], t1[:], t1[:], ALU.mult, [bT1], [bT2])
                        P.op("vector", lambda e: e.tensor_reduce(out=SM[:, :, 0], in_=t2[:].rearrange("p (h c) -> p h c", h=H), axis=AX.X, op=ALU.add), [bT2], [B["SM"]])
                        ts("vector", SM[:, :, 0], SM[:, :, 0], 1e-24, None, ALU.max, None, [B["SM"]], [B["SM"]])
                        tt("gpsimd", SM[:, :, 1], SM[:, :, 0], NH[:, 0:H], ALU.pow, [B["SM"], B["NH"]], [B["SM"]])
                        for h in range(H):
                            ts("vector", t1[:, h * 64:(h + 1) * 64], t1[:, h * 64:(h + 1) * 64], SM[:, h, 1:2], None, ALU.mult, None, [bT1, B["SM"]], [bT1])
                        kk = t1
                        stt(t2[:], asg[:], -1.0, KAT[:], ALU.add, ALU.mult, [bAS, B["KAT"]], [bT2])
                        stt(k32[:], t2[:], 1.0, k32[:], ALU.add, ALU.mult, [bT2, bK], [bK])
                        tt("gpsimd", t2[:], r32[:], k32[:], ALU.mult, [bR, bK], [bT2])
                        tt("gpsimd", t2[:], t2[:], RKT[:], ALU.mult, [bT2, B["RKT"]], [bT2])
                        P.op("vector", lambda e: e.tensor_reduce(out=SM[:, :, 2], in_=t2[:].rearrange("p (h c) -> p h c", h=H), axis=AX.X, op=ALU.add), [bT2], [B["SM"]])
                        tt("gpsimd", asg[:], asg[:], kk[:], ALU.mult, [bAS, bT1], [bAS])
                        bvec = asg
                        tt("vector", KH[:], k32[:], EB[3][:], ALU.mult, [bK, B["EB3"]], [B["KH"]])
                        tt("gpsimd", BH[:], bvec[:], EB[3][:], ALU.mult, [bAS, B["EB3"]], [B["BH"]])
                        cp("gpsimd", VBF[:], v32[:], [bV], [B["VBF"]])
                        def trans_to(srcf, bsrc, eb, ebn, neg, dst_fn, dname, oi):
                            ob = OB[oi]; bob = B[f"OB{oi}"]
                            if neg:
                                stt(ob[:], srcf[:], -1.0, eb[:], ALU.mult, ALU.mult, [bsrc, B[ebn]], [bob])
                            else:
                                tt("vector", ob[:], srcf[:], eb[:], ALU.mult, [bsrc, B[ebn]], [bob])
                            for kc in range(8):
                                tr(PT[:, kc * 128:(kc + 1) * 128], ob[:, kc * 128:(kc + 1) * 128], IDENT[:], [bob, B["IDENT"]], [B["PT"]], sig=(kc == 7))
                            cp("vector", dst_fn, PT[:].rearrange("p (k t) -> p k t", k=8), [B["PT"]], [B[dname]])
                        trans_to(r32, bR, EB[0], "EB0", False, TAR[:, :, 1, :], "TAR", 0)
                        trans_to(kk, bT1, EB[2], "EB2", True, TAR[:, :, 0, :], "TAR", 1)
                        trans_to(bvec, bAS, EB[1], "EB1", False, TBT[:], "TBT", 0)
                        trans_to(k32, bK, EB[1], "EB1", False, TKT[:], "TKT", 1)
                        for i_ in range(2):
                            ps_ = slice(i_ * 64, (i_ + 1) * 64)
                            cp("vector", TARm[i_][ps_], TAR[ps_], [B["TAR"]], [B[f"TARm{i_}"]])
                            cp("gpsimd", TBTm[i_][ps_], TBT[ps_], [B["TBT"]], [B[f"TBTm{i_}"]])
                            cp("gpsimd", SBFm[i_][ps_], SBF[ps_], [B["SBF"]], [B[f"SBFm{i_}"]])
                        chk(6)
                        for pr_ in range(2):
                            st_ = {}
                            for g4 in (2 * pr_, 2 * pr_ + 1):
                                gi = g4 % 2
                                pl, bpl = bank()
                                for hh in range(4):
                                    h = g4 * 4 + hh; kc = h // 2
                                    mm(pl[:, hh * 128:(hh + 1) * 128], TAR[:, kc, 0, :], TBTm[h % 2][:, kc, :], True, True,
                                       [B["TAR"], B[f"TBTm{h % 2}"]], [bpl], sig=(hh == 3))
                                tt("vector", R0[gi][:], pl[:, 0:512], MSKL[:], ALU.mult, [bpl, B["MSKL"]], [B[f"R0_{gi}"]])
                                for (lt, ltn, dstm, dmn) in ((TBT, "TBT", MB, "MB"), (TKT, "TKT", MK, "MK")):
                                    for h2 in range(2):
                                        pb, bpb = bank()
                                        for hh in range(2):
                                            h = g4 * 4 + h2 * 2 + hh; kc = h // 2
                                            mm(pb[:, hh * 256:(hh + 1) * 256], lt[:, kc, :], TARm[h % 2][:, kc, :, :].rearrange("p a t -> p (a t)"),
                                               True, True, [B[ltn], B[f"TARm{h % 2}"]], [bpb], sig=(hh == 1))
                                        h0 = g4 * 4 + h2 * 2
                                        tt("vector", dstm[:, h0:h0 + 2, :, :].rearrange("p h a t -> p (h a t)"), pb[:, 0:512], MSKT[:], ALU.mult,
                                           [bpb, B["MSKT"]], [B[dmn]])
                                cur = QRG[gi][0]; nxt = QRG[gi][1]; bcur = B[f"QRG{gi}_0"]; bnxt = B[f"QRG{gi}_1"]
                                cp("vector", cur[:, 0, :].rearrange("p (h t) -> p h t", h=4), MB[:, g4 * 4:g4 * 4 + 4, 0, :], [B["MB"]], [bcur])
                                cp("gpsimd", cur[:, 1, :], R0[gi][:], [B[f"R0_{gi}"]], [bcur])
                                tt("gpsimd", cur[:, 2, :], cur[:, 0, :], ID4[:], ALU.add, [bcur, B["ID4"]], [bcur])
                                st_[g4] = [cur, nxt, bcur, bnxt]
                            for jj in range(1, 7):
                                pbk = {}
                                for g4 in (2 * pr_, 2 * pr_ + 1):
                                    cur, nxt, bcur, bnxt = st_[g4]
                                    pq, bpq = bank() if jj < 6 else (None, None)
                                    pr, bpr = bank()
                                    for hh in range(4):
                                        cs_ = slice(hh * 128, (hh + 1) * 128)
                                        if jj < 6:
                                            mm(pq[:, cs_], cur[:, 1, cs_], cur[:, 0, cs_], True, True, [bcur], [bpq], sig=(hh == 3))
                                    for hh in range(4):
                                        cs_ = slice(hh * 128, (hh + 1) * 128)
                                        mm(pr[:, cs_], cur[:, 0, cs_], cur[:, 1, cs_], True, True, [bcur], [bpr], sig=(hh == 3))
                                    pbk[g4] = (pq, bpq, pr, bpr)
                                for g4 in (2 * pr_, 2 * pr_ + 1):
                                    cur, nxt, bcur, bnxt = st_[g4]
                                    pq, bpq, pr, bpr = pbk[g4]
                                    if jj < 6:
                                        act(nxt[:, 0, :], pq[:, 0:512], AF.Copy, [bpq], [bnxt])
                                    cp("vector", nxt[:, 1, :], pr[:, 0:512], [bpr], [bnxt])
                                pgk = {}
                                for g4 in (2 * pr_, 2 * pr_ + 1):
                                    cur, nxt, bcur, bnxt = st_[g4]
                                    pg, bpg = bank()
                                    for hh in range(4):
                                        cs_ = slice(hh * 128, (hh + 1) * 128)
                                        mm(pg[:, cs_], nxt[:, 1, cs_], cur[:, 2, cs_], True, True, [bnxt, bcur], [bpg], sig=(hh == 3))
                                    pgk[g4] = (pg, bpg)
                                for g4 in (2 * pr_, 2 * pr_ + 1):
                                    cur, nxt, bcur, bnxt = st_[g4]
                                    pg, bpg = pgk[g4]
                                    if jj < 6:
                                        tt("vector", nxt[:, 2, :], pg[:, 0:512], cur[:, 2, :], ALU.add, [bpg, bcur], [bnxt])
                                    else:
                                        tt("vector", G7[:, g4 * 4:g4 * 4 + 4, :].rearrange("p h t -> p (h t)"), pg[:, 0:512], cur[:, 2, :], ALU.add,
                                           [bpg, bcur], [B["G7"]])
                                    st_[g4] = [nxt, cur, bnxt, bcur]
                        chk(7)
                        for h8 in range(2):
                            px, bpx = bank()
                            for hh in range(8):
                                h = h8 * 8 + hh; kc = h // 2; po = (h % 2) * 64
                                mm(px[:, hh * 64:(hh + 1) * 64], TAR[:, kc, 0, :], SBFm[h % 2][:, kc, :], True, False, [B["TAR"], B[f"SBFm{h % 2}"]], [bpx], sig=False)
                                mm(px[:, hh * 64:(hh + 1) * 64], MK[:, h, 0, :], VBF[:, h * 64:(h + 1) * 64], False, True, [B["MK"], B["VBF"]], [bpx], sig=(hh == 7))
                            cp("vector", XB[:, h8 * 8:(h8 + 1) * 8, :].rearrange("p h c -> p (h c)"), px[:, 0:512], [bpx], [B["XB"]])
                        for h8 in range(2):
                            pu, bpu = bank()
                            for hh in range(8):
                                h = h8 * 8 + hh
                                mm(pu[:, hh * 64:(hh + 1) * 64], G7[:, h, :], XB[:, h, :], True, True, [B["G7"], B["XB"]], [bpu], sig=(hh == 7))
                            cp("vector", UB[:, h8 * 8:(h8 + 1) * 8, :].rearrange("p h c -> p (h c)"), pu[:, 0:512], [bpu], [B["UB"]])
                        ybanks = []
                        for h8 in range(2):
                            py, bpy = bank()
                            for hh in range(8):
                                h = h8 * 8 + hh; kc = h // 2; po = (h % 2) * 64
                                o_ = py[:, hh * 64:(hh + 1) * 64]
                                mm(o_, TAR[:, kc, 1, :], SBFm[h % 2][:, kc, :], True, False, [B["TAR"], B[f"SBFm{h % 2}"]], [bpy], sig=False)
                                mm(o_, MB[:, h, 1, :], UB[:, h, :], False, False, [B["MB"], B["UB"]], [bpy], sig=False)
                                mm(o_, MK[:, h, 1, :], VBF[:, h * 64:(h + 1) * 64], False, True, [B["MK"], B["VBF"]], [bpy], sig=(hh == 7))
                            ybanks.append((py, bpy))
                        pss, bpss = bank()
                        for h in range(H):
                            kc = h // 2; po = (h % 2) * 64
                            o_ = pss[po:po + 64, kc * 64:(kc + 1) * 64]
                            mm(o_, BH[:, h * 64:(h + 1) * 64], UB[:, h, :], True, False, [B["BH"], B["UB"]], [bpss], sig=False)
                            mm(o_, KH[:, h * 64:(h + 1) * 64], VBF[:, h * 64:(h + 1) * 64], False, True, [B["KH"], B["VBF"]], [bpss], sig=(h == H - 1))
                        for kc in range(8):
                            stt(S32[:, kc, :], S32[:, kc, :], PC[:, kc:kc + 1], pss[:, kc * 64:(kc + 1) * 64], ALU.mult, ALU.add,
                                [B["S32"], B["PC"], bpss], [B["S32"]])
                        y32 = r32
                        for h8, (py, bpy) in enumerate(ybanks):
                            act(y32[:, h8 * 512:(h8 + 1) * 512], py[:, 0:512], AF.Copy, [bpy], [bR])
                        cp("gpsimd", SBF[:], S32[:], [B["S32"]], [B["SBF"]])
                        P.op("vector", lambda e: e.tensor_reduce(out=SM[:, :, 0], in_=y32[:].rearrange("p (h c) -> p h c", h=H), axis=AX.X, op=ALU.add), [bR], [B["SM"]])
                        tt("gpsimd", t2[:], y32[:], y32[:], ALU.mult, [bR], [bT2])
                        P.op("vector", lambda e: e.tensor_reduce(out=SM[:, :, 1], in_=t2[:].rearrange("p (h c) -> p h c", h=H), axis=AX.X, op=ALU.add), [bT2], [B["SM"]])
                        ts("vector", SM[:, :, 0], SM[:, :, 0], 1.0 / 64, None, ALU.mult, None, [B["SM"]], [B["SM"]])
                        tt("vector", SM[:, :, 3], SM[:, :, 0], SM[:, :, 0], ALU.mult, [B["SM"]], [B["SM"]])
                        stt(SM[:, :, 1], SM[:, :, 1], 1.0 / 64, SM[:, :, 3], ALU.mult, ALU.subtract, [B["SM"]], [B["SM"]])
                        ts("vector", SM[:, :, 1], SM[:, :, 1], 64e-5, None, ALU.add, None, [B["SM"]], [B["SM"]])
                        tt("gpsimd", SM[:, :, 3], SM[:, :, 1], NH[:, 0:H], ALU.pow, [B["SM"], B["NH"]], [B["SM"]])
                        for h in range(H):
                            hs = slice(h * 64, (h + 1) * 64)
                            ts("vector", y32[:, hs], y32[:, hs], SM[:, h, 0:1], SM[:, h, 3:4], ALU.subtract, ALU.mult, [bR, B["SM"]], [bR])
                        tt("gpsimd", y32[:], y32[:], GNW[:], ALU.mult, [bR, B["GNW"]], [bR])
                        tt("gpsimd", y32[:], y32[:], GNB[:], ALU.add, [bR, B["GNB"]], [bR])
                        for h in range(H):
                            hs = slice(h * 64, (h + 1) * 64)
                            stt(y32[:, hs], v32[:, hs], SM[:, h, 2:3], y32[:, hs], ALU.mult, ALU.add, [bV, B["SM"], bR], [bR])
                        rr = lora2(LG1, 128, 3)
                        for hf, (pb, bpb) in enumerate(rr):
                            tt("vector", OB[0][:, hf * 512:(hf + 1) * 512], pb[:, 0:512], y32[:, hf * 512:(hf + 1) * 512], ALU.mult, [bpb, bR], [B["OB0"]])
                        for kc in range(8):
                            tr(PT[:, kc * 128:(kc + 1) * 128], OB[0][:, kc * 128:(kc + 1) * 128], IDENT[:], [B["OB0"], B["IDENT"]], [B["PT"]], sig=(kc == 7))
                        cp("vector", YT[:], PT[:].rearrange("p (k t) -> p k t", k=8), [B["PT"]], [B["YT"]])

                        chk(8)
                        def fm_proj(slab_src_fn, rhs_t, brhs, oc):
                            pass
                        for q in range(2):
                            RO_, BRO = slab(S_["ro"][:, q * 4:(q + 1) * 4].rearrange("p a k c -> p (a k c)"), 4096)
                            ZR_, BZR = slab(S_["fm"][:, 16 + q * 4:16 + (q + 1) * 4].rearrange("p a k c -> p (a k c)"), 4096)
                            for o4 in range(4):
                                oc = q * 4 + o4
                                py_, bpy_ = bank(); pz_, bpz_ = bank()
                                ROv = RO_[:, 0:4096].rearrange("p (a k c) -> p a k c", a=4, k=8)
                                ZRv = ZR_[:, 0:4096].rearrange("p (a k c) -> p a k c", a=4, k=8)
                                for kc in range(8):
                                    mm(py_[:, 0:NT], ROv[:, o4, kc, :], YT[:, kc, :], kc == 0, kc == 7, [BRO, B["YT"]], [bpy_], sig=(kc == 7))
                                for kc in range(8):
                                    mm(pz_[:, 0:NT], ZRv[:, o4, kc, :], XNT[:, kc, 1:1 + NT], kc == 0, kc == 7, [BZR, B["XNT"]], [bpz_], sig=(kc == 7))
                                tf = TMPF[0]
                                act(tf[:], pz_[:, 0:NT], AF.Tanh, [bpz_], [B["TMPF0"]], scale=0.5)
                                sigm_from_tanh("gpsimd", tf[:], [B["TMPF0"]], [B["TMPF0"]])
                                tt("vector", ACC[:, oc, :], py_[:, 0:NT], tf[:], ALU.mult, [bpy_, B["TMPF0"]], [ACCo[oc]])
                        for q in range(2):
                            CO_, BCO = slab(S_["co"][:, q * 4:(q + 1) * 4].rearrange("p a k c -> p (a k c)"), 4096)
                            ZC_, BZC = slab(S_["fm"][:, 24 + q * 4:24 + (q + 1) * 4].rearrange("p a k c -> p (a k c)"), 4096)
                            for o4 in range(4):
                                oc = q * 4 + o4
                                py_, bpy_ = bank(); pz_, bpz_ = bank()
                                COv = CO_[:, 0:4096].rearrange("p (a k c) -> p a k c", a=4, k=8)
                                ZCv = ZC_[:, 0:4096].rearrange("p (a k c) -> p a k c", a=4, k=8)
                                for kc in range(8):
                                    mm(py_[:, 0:NT], COv[:, o4, kc, :], CCT[:, kc, :], kc == 0, kc == 7, [BCO, B["CCT"]], [bpy_], sig=(kc == 7))
                                for kc in range(8):
                                    mm(pz_[:, 0:NT], ZCv[:, o4, kc, :], XNT[:, kc, 1:1 + NT], kc == 0, kc == 7, [BZC, B["XNT"]], [bpz_], sig=(kc == 7))
                                tf = TMPF[1]
                                act(tf[:], pz_[:, 0:NT], AF.Tanh, [bpz_], [B["TMPF1"]], scale=0.5)
                                sigm_from_tanh("gpsimd", tf[:], [B["TMPF1"]], [B["TMPF1"]])
                                tt("vector", tf[:], py_[:, 0:NT], tf[:], ALU.mult, [bpy_, B["TMPF1"]], [B["TMPF1"]])
                                tt("gpsimd", MRG[:, oc, :], tf[:], ACC[:, oc, :], ALU.add, [B["TMPF1"], ACCo[oc]], [B["MRG"]])

                        def out_norm_resid(pbs, gtile, gname):
                            for hf, (pb, bpb) in enumerate(pbs):
                                act(JUNK[:, 0:512], pb[:, 0:512], AF.Square, [bpb], [B["JUNK"], B["SS"]], accum_out=SS[:, hf:hf + 1])
                            tt("vector", MS[:, 0:1], SS[:, 0:1], SS[:, 1:2], ALU.add, [B["SS"]], [B["MS"]])
                            ts("vector", MS[:, 0:1], MS[:, 0:1], 1.0 / D, 1e-6, ALU.mult, ALU.add, [B["MS"]], [B["MS"]])
                            tt("gpsimd", RSTD[:, 0:1], MS[:, 0:1], NH[:, 0:1], ALU.pow, [B["MS"], B["NH"]], [B["RSTD"]])
                            for hf, (pb, bpb) in enumerate(pbs):
                                hs = slice(hf * 512, (hf + 1) * 512)
                                stt(A[5][:, hs], pb[:, 0:512], RSTD[:, 0:1], gtile[:, hs], ALU.mult, ALU.mult, [bpb, B["RSTD"], B[gname]], [B["A5"]])
                            tt("gpsimd", X[:, 0, :], X[:, 0, :], A[5][:], ALU.add, [B["X"], B["A5"]], [B["X"]])

                        WO_, BWO = [], []
                        pbs = []
                        for hf in range(2):
                            w_, bw_ = slab(S_["wo"][:, :, hf * 512:(hf + 1) * 512], 4096)
                            wv = w_[:, 0:4096].rearrange("p (k c) -> p k c", k=8)
                            pb, bpb = bank()
                            for kc in range(8):
                                mm(pb[:, 0:512], MRG[:, kc, :], wv[:, kc, :], kc == 0, kc == 7, [B["MRG"], bw_], [bpb], sig=(kc == 7))
                            pbs.append((pb, bpb))
                        out_norm_resid(pbs, GPOST, "GPOST")
                        if ti == 0 and seq == 0 and l == layers[0]:
                            dump("d_x1", X[:, 0, :], [128, D], F32, [B["X"]])
                            dump("d_yt", YT[:], [128, 8, NT], BF16, [B["YT"]])
                            dump("d_cct", CCT[:], [128, 8, NT], BF16, [B["CCT"]])
                            dump("d_mrg", MRG[:], [128, 8, NT], BF16, [B["MRG"]])
                            dump("d_xnt", XNT[:], [128, 8, NT + 1], BF16, [B["XNT"]])
                            dump("d_v", A[2][:], [128, D], F32, [B["A2"]])
                            dump("d_k", A[1][:], [128, D], F32, [B["A1"]])
                            dump("d_y", A[0][:], [128, D], F32, [B["A0"]])
                            dump("d_b", A[3][:], [128, D], F32, [B["A3"]])
                            dump("d_lw", A[4][:], [128, D], F32, [B["A4"]])
                        rmsnorm_to(XNT, 1, V_GFPRE)
                        for q in range(6):
                            n4 = 4 if q < 5 else 2
                            G_, BG_ = slab(S_["g"][:, q * 4:q * 4 + n4].rearrange("p a k c -> p (a k c)"), n4 * 1024)
                            U_, BU_ = slab(S_["u"][:, q * 4:q * 4 + n4].rearrange("p a k c -> p (a k c)"), n4 * 1024)
                            Gv = G_[:, 0:n4 * 1024].rearrange("p (a k c) -> p a k c", a=n4, k=8)
                            Uv = U_[:, 0:n4 * 1024].rearrange("p (a k c) -> p a k c", a=n4, k=8)
                            for o4 in range(n4):
                                oc = q * 4 + o4
                                pg_, bpg_ = bank(); pu_, bpu_ = bank()
                                for kc in range(8):
                                    mm(pg_[:, 0:NT], Gv[:, o4, kc, :], XNT[:, kc, 1:1 + NT], kc == 0, kc == 7, [BG_, B["XNT"]], [bpg_], sig=(kc == 7))
                                for kc in range(8):
                                    mm(pu_[:, 0:NT], Uv[:, o4, kc, :], XNT[:, kc, 1:1 + NT], kc == 0, kc == 7, [BU_, B["XNT"]], [bpu_], sig=(kc == 7))
                                tf = TMPF[oc % 2]; btf = B[f"TMPF{oc % 2}"]
                                act(tf[:], pg_[:, 0:NT], AF.Tanh, [bpg_], [btf], scale=0.5)
                                sigm_from_tanh("gpsimd", tf[:], [btf], [btf])
                                tt("vector", tf[:], pg_[:, 0:NT], tf[:], ALU.mult, [bpg_, btf], [btf])
                                tt("vector", HT[:, oc, :], pu_[:, 0:NT], tf[:], ALU.mult, [bpu_, btf], [B["HT"]])
                        pbs = []
                        for hf in range(2):
                            pb, bpb = bank()
                            for q in range(3):
                                n8 = 8 if q < 2 else 6
                                w_, bw_ = slab(S_["d"][:, q * 8:q * 8 + n8, hf * 512:(hf + 1) * 512], n8 * 512)
                                wv = w_[:, 0:n8 * 512].rearrange("p (k c) -> p k c", k=n8)
                                for k8 in range(n8):
                                    kc = q * 8 + k8
                                    mm(pb[:, 0:512], HT[:, kc, :], wv[:, k8, :], kc == 0, kc == NFF - 1, [B["HT"], bw_], [bpb], sig=(k8 == n8 - 1))
                            pbs.append((pb, bpb))
                        out_norm_resid(pbs, GFPOST, "GFPOST")
                        dma(out[seq, t0:t0 + NT, :], X[:, 0, :], [B["X"]], [ob_], "dxo")
        except StopBuild:
            pass
        sems = {}
        for k in list(Plan.ENGS) + list(P.dma_cnt.keys()):
            sems[k] = st.enter_context(nc.semaphore("zq_" + k + "_sm"))
        P.emit(nc, sems, {"sync": ([("dxo", P.dma_cnt["dxo"])] if "dxo" in P.dma_cnt else []) + ([("vfo", P.dma_cnt["vfo"])] if "vfo" in P.dma_cnt else []) + [(d_, 16) for d_ in dumps]})
    return nc, P


def _fm_layout(W):
    n = W.shape[1] // 128
    return np.ascontiguousarray(W.reshape(8, 128, n, 128).transpose(1, 2, 0, 3))


def _tok_layout(W):
    K = W.shape[0] // 128
    return np.ascontiguousarray(W.reshape(K, 128, W.shape[1]).transpose(1, 0, 2))


def host_consts():
    s = np.arange(128)[:, None]
    t = np.arange(128)[None, :]
    c = {}
    c["ident"] = np.eye(128).astype(ml_dtypes.bfloat16)
    c["tri3"] = np.ascontiguousarray(np.stack([(s <= t), (s < t), (s > t)], axis=1).astype(np.float32))
    c["mskL"] = np.ascontiguousarray(np.tile((s > t).astype(np.float32), (1, 4)))
    mT = np.concatenate([(t > s), (t >= s)], axis=1).astype(np.float32)
    c["mskT"] = np.ascontiguousarray(np.tile(mT, (1, 2)))
    c["ident4"] = np.ascontiguousarray(np.tile(np.eye(128, dtype=np.float32), (1, 4)))
    return c


def host_layer(inp, l):
    f = lambda a: np.asarray(a, dtype=np.float32)
    w_in = f(inp["w_in"][l])
    d = {}
    d[f"wtok{l}"] = _tok_layout(w_in[:, 0:3072])
    d[f"wfm{l}"] = _fm_layout(w_in[:, 3072:7168])
    v1 = f(inp["vres_1"][l - 1]) if l > 0 else np.zeros((D, 32), np.float32)
    d[f"wl1{l}"] = _tok_layout(np.concatenate([f(inp["decay_w1"][l]), f(inp["a_1"][l]), f(inp["g_1"][l]), v1], axis=1))
    l2 = np.zeros((128, 4, D), np.float32)
    l2[0:64, 0] = f(inp["decay_w2"][l]); l2[64, 0] = f(inp["decay_w0"][l])
    l2[0:64, 1] = f(inp["a_2"][l]); l2[64, 1] = f(inp["a_0"][l])
    if l > 0:
        l2[0:32, 2] = f(inp["vres_2"][l - 1]); l2[32, 2] = f(inp["vres_0"][l - 1])
    l2[0:128, 3] = f(inp["g_2"][l])
    d[f"wl2{l}"] = l2
    d[f"wro{l}"] = _fm_layout(f(inp["w_rwkv_out"][l]))
    d[f"wco{l}"] = _fm_layout(f(inp["w_conv_out"][l]))
    d[f"wo{l}"] = _tok_layout(f(inp["w_out"][l]))
    d[f"wg{l}"] = _fm_layout(f(inp["ffn_w_gate"][l]))
    d[f"wu{l}"] = _fm_layout(f(inp["ffn_w_up"][l]))
    d[f"wd{l}"] = _tok_layout(f(inp["ffn_w_down"][l]))
    vres_mu = f(inp["vres_mu"][l - 1]) if l > 0 else np.zeros(D, np.float32)
    rows = [inp["pre_mix_norm"][l], inp["post_mix_norm"][l], inp["pre_ffn_norm"][l], inp["post_ffn_norm"][l],
            inp["mu_rkv"][l][0], inp["mu_rkv"][l][1], inp["mu_rkv"][l][2],
            inp["mu_wag"][l][0], inp["mu_wag"][l][1], inp["mu_wag"][l][2], vres_mu,
            inp["k_k"][l], inp["k_a"][l], np.asarray(inp["r_k"][l]).reshape(-1), inp["gn_w"][l], inp["gn_b"][l],
            inp["conv_b"][l], inp["conv_ln_w"][l], inp["conv_ln_b"][l]]
    d[f"vec{l}"] = np.ascontiguousarray(np.stack([f(r) for r in rows], axis=0))
    d[f"vecT{l}"] = np.ascontiguousarray(d[f"vec{l}"].reshape(NVEC, 8, 128).transpose(2, 1, 0))
    d[f"dwT{l}"] = np.ascontiguousarray(f(inp["conv_dw"][l]).reshape(31, 8, 128).transpose(2, 1, 0))
    return d


_PROG = {}


def kernel(**inputs):
    x = np.asarray(inputs["x"], dtype=np.float32)
    Bt, T, _ = x.shape
    nseq = Bt // 8
    key = (T, nseq)
    if key not in _PROG:
        _PROG[key] = build(T, nseq, [0, 1, 2, 3])[0]
    nc = _PROG[key]
    base = host_consts()
    for l in range(4):
        base.update(host_layer(inputs, l))
    in_maps = []
    for c in range(8):
        m = {"x": np.ascontiguousarray(x[c * nseq:(c + 1) * nseq])}
        m.update(base)
        in_maps.append(m)
    res = run_bass_kernel_spmd(nc, in_maps, core_ids=list(range(8)))
    return np.concatenate([res.results[c]["out"] for c in range(8)], axis=0).astype(np.float32)
```

```python
import numpy as np
import ml_dtypes
from contextlib import ExitStack
import concourse.bass as bass
import concourse.mybir as mybir
from concourse.bass_utils import run_bass_kernel_spmd

F32 = mybir.dt.float32
BF16 = mybir.dt.bfloat16
AF = mybir.ActivationFunctionType
ALU = mybir.AluOpType
AX = mybir.AxisListType

D = 1024
H = 16
NFF = 22
NT = 128
NS = NT // 128
NVEC = 19
(V_GPRE, V_GPOST, V_GFPRE, V_GFPOST, V_MUR, V_MUK, V_MUV, V_MUW, V_MUA, V_MUG, V_MUVR, V_KK, V_KA, V_RK,
 V_GNW, V_GNB, V_CB, V_LNW, V_LNB) = range(NVEC)


class Buf:
    __slots__ = ("name", "w", "r")

    def __init__(self, name=""):
        self.name = name
        self.w = None
        self.r = []


class Plan:
    ENGS = ("tensor", "vector", "scalar", "gpsimd", "sync")

    def __init__(self):
        self.streams = {e: [] for e in self.ENGS}
        self.cnt = {e: 0 for e in self.ENGS}
        self.waited = {e: {} for e in self.ENGS}
        self.dma_cnt = {}
        self.hook = None
        self.in_hook = False
        self.hook_n = 0

    def _need(self, eng, ev, waits):
        if ev is None:
            return
        k, v = ev
        if k == eng and v > self.cnt[eng]:
            return
        if self.waited[eng].get(k, 0) >= v:
            return
        if waits.get(k, 0) < v:
            waits[k] = v

    def op(self, eng, fn, reads=(), writes=(), sig=True, dma_sem=None, noself=False):
        waits = {}
        for b in reads:
            self._need(eng, b.w, waits)
        for b in writes:
            self._need(eng, b.w, waits)
            for ev in b.r:
                self._need(eng, ev, waits)
        wl = []
        for k, v in waits.items():
            if noself and k == eng:
                continue
            self.waited[eng][k] = v
            wl.append((k, v))
        if dma_sem is not None:
            self.dma_cnt[dma_sem] = self.dma_cnt.get(dma_sem, 0) + 16
            ev = (dma_sem, self.dma_cnt[dma_sem])
            self.streams[eng].append((wl, fn, dma_sem, 16))
        elif sig:
            self.cnt[eng] += 1
            ev = (eng, self.cnt[eng])
            self.streams[eng].append((wl, fn, eng, 1))
        else:
            self.streams[eng].append((wl, fn, None, 0))
            ev = (eng, self.cnt[eng] + 1)
        for b in reads:
            if len(b.r) > 8:
                b.r = [e for e in b.r if not (e[0] == ev[0] and e[1] <= ev[1])]
            b.r.append(ev)
        for b in writes:
            b.w = ev
            b.r = []
        if self.hook is not None and not self.in_hook:
            self.hook_n += 1
            if self.hook_n % 3 == 0:
                self.in_hook = True
                try:
                    next(self.hook)
                except StopIteration:
                    self.hook = None
                self.in_hook = False

    def barrier(self):
        evs = [(e, self.cnt[e]) for e in self.ENGS if self.cnt[e] > 0]
        evs += [(k, v) for k, v in self.dma_cnt.items()]
        for e in self.ENGS:
            wl = []
            for (k, v) in evs:
                if k == e:
                    continue
                if self.waited[e].get(k, 0) < v:
                    self.waited[e][k] = v
                    wl.append((k, v))
            if wl:
                self.streams[e].append((wl, None, None, 0))

    def emit(self, nc, sems, final_waits):
        with nc.Block() as block:
            def mk(engname):
                def body(e):
                    for (wl, fn, sk, inc) in self.streams[engname]:
                        for (k, v) in wl:
                            e.wait_ge(sems[k], v)
                        if fn is None:
                            continue
                        ins = fn(e)
                        if sk is not None:
                            ins.then_inc(sems[sk], inc)
                    for (k, v) in final_waits.get(engname, []):
                        e.wait_ge(sems[k], v)
                return body
            block.tensor(mk("tensor"))
            block.vector(mk("vector"))
            block.scalar(mk("scalar"))
            block.gpsimd(mk("gpsimd"))
            block.sync(mk("sync"))


class StopBuild(Exception):
    pass


def build(T, nseq, layers, dbg=False, kstop=0):
    def chk(n):
        if kstop == n:
            raise StopBuild()
    nc = bass.Bass("TRN2", target_bir_lowering=False)
    P = Plan()
    NL = len(layers)
    dr = {}

    def din(name, shape, dt=F32):
        dr[name] = nc.dram_tensor(name, list(shape), dt, kind="ExternalInput").ap()
        return dr[name]

    x_in = din("x", [nseq, T, D])
    ident_d = din("ident", [128, 128], BF16)
    tri_d = din("tri3", [128, 3, 128])
    mskL_d = din("mskL", [128, 512])
    mskT_d = din("mskT", [128, 512])
    id4_d = din("ident4", [128, 512])
    WN = {}
    for l in layers:
        WN[l] = dict(
            tok=din(f"wtok{l}", [128, 8, 3072]), fm=din(f"wfm{l}", [128, 32, 8, 128]),
            l1=din(f"wl1{l}", [128, 8, 288]), l2=din(f"wl2{l}", [128, 4, 1024]),
            ro=din(f"wro{l}", [128, 8, 8, 128]), co=din(f"wco{l}", [128, 8, 8, 128]),
            wo=din(f"wo{l}", [128, 8, 1024]), g=din(f"wg{l}", [128, NFF, 8, 128]),
            u=din(f"wu{l}", [128, NFF, 8, 128]), d=din(f"wd{l}", [128, NFF, 1024]),
            vec=din(f"vec{l}", [NVEC, D]), vecT=din(f"vecT{l}", [128, 8, NVEC]), dw=din(f"dwT{l}", [128, 8, 31]))
    out = nc.dram_tensor("out", [nseq, T, D], F32, kind="ExternalOutput").ap()
    if 0 in layers and NL == 1:
        vf_d = nc.dram_tensor("vf", [nseq, T, D], F32, kind="ExternalOutput").ap()
    elif 0 in layers:
        vf_d = nc.dram_tensor("vf", [nseq, T, D], F32).ap()
    else:
        vf_d = din("vf", [nseq, T, D])
    SC = {}
    for l in layers:
        SC[l] = dict(
            tok=nc.dram_tensor(f"s_tok{l}", [128, 12, 16, 256], BF16).ap(),
            fm=nc.dram_tensor(f"s_fm{l}", [128, 32, 8, 128], BF16).ap(),
            l1=nc.dram_tensor(f"s_l1{l}", [128, 8, 2, 288], BF16).ap(),
            l2=nc.dram_tensor(f"s_l2{l}", [128, 4, 1024], BF16).ap(),
            ro=nc.dram_tensor(f"s_ro{l}", [128, 8, 8, 128], BF16).ap(),
            co=nc.dram_tensor(f"s_co{l}", [128, 8, 8, 128], BF16).ap(),
            wo=nc.dram_tensor(f"s_wo{l}", [128, 8, 1024], BF16).ap(),
            g=nc.dram_tensor(f"s_g{l}", [128, NFF, 8, 128], BF16).ap(),
            u=nc.dram_tensor(f"s_u{l}", [128, NFF, 8, 128], BF16).ap(),
            d=nc.dram_tensor(f"s_d{l}", [128, NFF, 1024], BF16).ap())

    with ExitStack() as st:
        def sb(name, shape, dt=F32):
            return st.enter_context(nc.sbuf_tensor(name, list(shape), dt))
        B = {}

        def nb(name):
            B[name] = Buf(name)
            return B[name]

        def act(out_, in_, func, R, W, **kw):
            P.op("scalar", lambda e: e.activation(out=out_, in_=in_, func=func, **kw), R, W)

        def ts(eng, out_, in0, s1, s2, op0, op1, R, W):
            if s2 is None:
                P.op(eng, lambda e: e.tensor_scalar(out=out_, in0=in0, scalar1=s1, scalar2=None, op0=op0), R, W)
            else:
                P.op(eng, lambda e: e.tensor_scalar(out=out_, in0=in0, scalar1=s1, scalar2=s2, op0=op0, op1=op1), R, W)

        def tt(eng, out_, in0, in1, op, R, W):
            P.op(eng, lambda e: e.tensor_tensor(out=out_, in0=in0, in1=in1, op=op), R, W)

        def stt(out_, in0, scalar, in1, op0, op1, R, W, noself=False):
            P.op("vector", lambda e: e.scalar_tensor_tensor(out=out_, in0=in0, scalar=scalar, in1=in1, op0=op0, op1=op1), R, W, noself=noself)

        def cp(eng, out_, in_, R, W):
            P.op(eng, lambda e: e.tensor_copy(out=out_, in_=in_), R, W)

        def mset(eng, ap, val, W):
            P.op(eng, lambda e: e.memset(ap, val), (), W)

        def mm(out_, lhsT, rhs, start, stop, R, W, sig):
            P.op("tensor", lambda e: e.matmul(out_, lhsT, rhs, start=start, stop=stop), R, W, sig=sig)

        def tr(out_, in_, idn, R, W, sig):
            P.op("tensor", lambda e: e.transpose(out_, in_, idn), R, W, sig=sig)

        dma_rr = [0]

        def dma(out_, in_, R, W, sem, eng="sync"):
            P.op(eng, lambda e: e.dma_start(out=out_, in_=in_), R, W, dma_sem=sem)

        dumps = []

        def dump(name, ap_, shape, dt_, bufs):
            if not dbg:
                return
            t_ = nc.dram_tensor(name, list(shape), dt_, kind="ExternalOutput").ap()
            dma(t_, ap_, bufs, (), "dbg_" + name)
            dumps.append("dbg_" + name)

        IDENT = sb("IDENT", [128, 128], BF16); nb("IDENT")
        TRI = sb("TRI", [128, 3, 128]); nb("TRI")
        MSKL = sb("MSKL", [128, 512]); nb("MSKL")
        MSKT = sb("MSKT", [128, 512]); nb("MSKT")
        ID4 = sb("ID4", [128, 512]); nb("ID4")
        ONES32 = sb("ONES32", [128, 128]); nb("ONES32")
        NH = sb("NH", [128, NT]); nb("NH")
        dma(IDENT[:], ident_d, (), [B["IDENT"]], "c0")
        dma(TRI[:], tri_d, (), [B["TRI"]], "c1")
        dma(MSKL[:], mskL_d, (), [B["MSKL"]], "c2")
        dma(MSKT[:], mskT_d, (), [B["MSKT"]], "c3")
        dma(ID4[:], id4_d, (), [B["ID4"]], "c4")
        mset("gpsimd", ONES32[:], 1.0, [B["ONES32"]])
        mset("gpsimd", NH[:], -0.5, [B["NH"]])

        with ExitStack() as pst:
            def psb(name, shape, dt=F32):
                return pst.enter_context(nc.sbuf_tensor(name, list(shape), dt))
            STG = [psb(f"STG{i}", [128, 4096]) for i in range(2)]
            STB = [psb(f"STB{i}", [128, 4608], BF16) for i in range(2)]
            for i in range(2):
                nb(f"STG{i}"); nb(f"STB{i}")
            VB = psb("VB", [128, 3, 1024]); nb("VB")
            VB1 = psb("VB1", [128, 3, 1024]); nb("VB1")
            VT = psb("VT", [128, 8, NVEC]); nb("VT")
            VT1 = psb("VT1", [128, 8, 4]); nb("VT1")
            pk = [0]

            def plain(dst2d, src2d, n):
                for c0 in range(0, n, 4096):
                    c1 = min(n, c0 + 4096)
                    i = pk[0] % 2; pk[0] += 1
                    dma(STG[i][:, 0:c1 - c0], src2d[:, c0:c1], (), [B[f"STG{i}"]], f"pi{i}")
                    if i == 0:
                        cp("vector", STB[i][:, 0:c1 - c0], STG[i][:, 0:c1 - c0], [B[f"STG{i}"]], [B[f"STB{i}"]])
                    else:
                        act(STB[i][:, 0:c1 - c0], STG[i][:, 0:c1 - c0], AF.Copy, [B[f"STG{i}"]], [B[f"STB{i}"]])
                    dma(dst2d[:, c0:c1], STB[i][:, 0:c1 - c0], [B[f"STB{i}"]], (), f"po{i}", eng="gpsimd")

            for l in layers:
                W_, S_ = WN[l], SC[l]
                for j in range(3):
                    dma(VB[:, j, :], W_["vec"][V_MUR + j, :].partition_broadcast(128), (), [B["VB"]], "pv0")
                dma(VT[:], W_["vecT"], (), [B["VT"]], "pv1")
                ts("vector", VB1[:], VB[:], -1.0, 1.0, ALU.mult, ALU.add, [B["VB"]], [B["VB1"]])
                ts("vector", VT1[:], VT[:, :, V_MUW:V_MUW + 4], -1.0, 1.0, ALU.mult, ALU.add, [B["VT"]], [B["VT1"]])
                for j in range(12):
                    i = pk[0] % 2; pk[0] += 1
                    wi = j // 4
                    dma(STG[i][:, 0:2048].rearrange("p (k c) -> p k c", k=8), W_["tok"][:, :, j * 256:(j + 1) * 256],
                        (), [B[f"STG{i}"]], f"pi{i}")
                    for s in range(2):
                        vb = (VB1 if s == 0 else VB)
                        for kc in range(8):
                            tt("vector" if kc % 2 == 0 else "gpsimd",
                               STB[i][:, (kc * 2 + s) * 256:(kc * 2 + s + 1) * 256],
                               STG[i][:, kc * 256:(kc + 1) * 256], vb[:, wi, (j % 4) * 256:(j % 4 + 1) * 256], ALU.mult,
                               [B[f"STG{i}"], B["VB"], B["VB1"]], [B[f"STB{i}"]])
                    dma(S_["tok"][:, j, :, :], STB[i][:, 0:4096].rearrange("p (k c) -> p k c", k=16),
                        [B[f"STB{i}"]], (), f"po{i}", eng="gpsimd")
                i = pk[0] % 2; pk[0] += 1
                dma(STG[i][:, 0:2304].rearrange("p (k c) -> p k c", k=8), W_["l1"], (), [B[f"STG{i}"]], f"pi{i}")
                for (c0, c1, mi) in ((0, 64, 0), (64, 128, 1), (128, 256, 2), (256, 288, 3)):
                    for kc in range(8):
                        for s in range(2):
                            sc_ = (VT1[:, kc, mi:mi + 1] if s == 0 else VT[:, kc, V_MUW + mi:V_MUW + mi + 1])
                            ts("vector", STB[i][:, (kc * 2 + s) * 288 + c0:(kc * 2 + s) * 288 + c1],
                               STG[i][:, kc * 288 + c0:kc * 288 + c1], sc_, None, ALU.mult, None,
                               [B[f"STG{i}"], B["VT"], B["VT1"]], [B[f"STB{i}"]])
                dma(S_["l1"].rearrange("p k s c -> p (k s c)"), STB[i][:, 0:4608],
                    [B[f"STB{i}"]], (), f"po{i}", eng="gpsimd")
                plain(S_["l2"].rearrange("p a c -> p (a c)"), W_["l2"].rearrange("p a c -> p (a c)"), 4096)
                plain(S_["fm"].rearrange("p a k c -> p (a k c)"), W_["fm"].rearrange("p a k c -> p (a k c)"), 32768)
                plain(S_["ro"].rearrange("p a k c -> p (a k c)"), W_["ro"].rearrange("p a k c -> p (a k c)"), 8192)
                plain(S_["co"].rearrange("p a k c -> p (a k c)"), W_["co"].rearrange("p a k c -> p (a k c)"), 8192)
                plain(S_["wo"].rearrange("p k c -> p (k c)"), W_["wo"].rearrange("p k c -> p (k c)"), 8192)
                plain(S_["g"].rearrange("p a k c -> p (a k c)"), W_["g"].rearrange("p a k c -> p (a k c)"), NFF * 1024)
                plain(S_["u"].rearrange("p a k c -> p (a k c)"), W_["u"].rearrange("p a k c -> p (a k c)"), NFF * 1024)
                plain(S_["d"].rearrange("p k c -> p (k c)"), W_["d"].rearrange("p k c -> p (k c)"), NFF * 1024)
            P.barrier()
        RING = [sb(f"RING{i}", [128, 4608], BF16) for i in range(3)]
        for i in range(3):
            nb(f"RING{i}")
        rk = [0]

        def slab(src2d, n):
            i = rk[0] % 3; rk[0] += 1
            dma(RING[i][:, 0:n], src2d, (), [B[f"RING{i}"]], f"rg{i}")
            return RING[i], B[f"RING{i}"]

        PS = [st.enter_context(nc.psum_tensor(f"PSB{i}", [128, 512], F32)) for i in range(7)]
        PT = st.enter_context(nc.psum_tensor("PTB", [128, 1024], BF16)); nb("PT")
        for i in range(7):
            nb(f"PS{i}")
        pk2 = [0]

        def bank():
            i = pk2[0] % 5; pk2[0] += 1
            return PS[i], B[f"PS{i}"]

        def T_(name, shape, dt=F32):
            nb(name)
            return sb(name, shape, dt)
        X = T_("X", [128, NS, D])
        XNT = T_("XNT", [128, 8, NT + 1], BF16)
        CARRY = T_("CARRY", [128, 8, 1], BF16)
        L2BUF = T_("L2BUF", [128, 4096], BF16)
        CB = T_("CB", [128, 8, NT + 30])
        ACC = T_("ACC", [128, 8, NT])
        CBo = [Buf() for _ in range(8)]
        CTb = None
        ACCo = [Buf() for _ in range(8)]
        CCT = T_("CCT", [128, 8, NT], BF16)
        YT = T_("YT", [128, 8, NT], BF16)
        MRG = T_("MRG", [128, 8, NT], BF16)
        SS = T_("SS", [128, 4]); MS = T_("MS", [128, 4]); RSTD = T_("RSTD", [128, 4])
        LW1 = T_("LW1", [65, NT], BF16); LA1 = T_("LA1", [65, NT], BF16); LV1 = T_("LV1", [33, NT], BF16)
        LG1 = T_("LG1", [128, NT], BF16)
        TMPF = [T_(f"TMPF{i}", [128, NT]) for i in range(2)]
        MEAN = T_("MEAN", [128, NT]); VAR = T_("VAR", [128, NT]); RS = T_("RS", [128, NT])
        PV = T_("PV", [128, 8, NVEC])
        HLN = T_("HLN", [128, 8, 2])
        DW = T_("DW", [128, 8, 31])
        GPOST = T_("GPOST", [128, D]); GFPOST = T_("GFPOST", [128, D])
        KKT = T_("KKT", [128, D]); KAT = T_("KAT", [128, D]); RKT = T_("RKT", [128, D])
        GNW = T_("GNW", [128, D]); GNB = T_("GNB", [128, D])
        A = [T_(f"A{i}", [128, D]) for i in range(7)]
        EB = [T_(f"EB{i}", [128, D], BF16) for i in range(4)]
        OB = [T_(f"OB{i}", [128, D], BF16) for i in range(2)]
        JUNK = OB[1]; B["JUNK"] = B["OB1"]
        XNB = OB[0]; B["XNB"] = B["OB0"]
        KH = T_("KH", [128, D], BF16); BH = T_("BH", [128, D], BF16); VBF = T_("VBF", [128, D], BF16)
        TAR = T_("TAR", [128, 8, 2, 128], BF16)
        TBT = T_("TBT", [128, 8, 128], BF16); TKT = T_("TKT", [128, 8, 128], BF16)
        SM = T_("SM", [128, 16, 4])
        S32 = T_("S32", [128, 8, 64]); SBF = T_("SBF", [128, 8, 64], BF16); PC = T_("PC", [128, 8])
        XB = T_("XB", [128, 16, 64], BF16); UB = T_("UB", [128, 16, 64], BF16)
        R0 = [T_(f"R0_{g}", [128, 512], BF16) for g in range(2)]
        QRG = [[T_(f"QRG{g}_{i}", [128, 3, 512], BF16) for i in range(2)] for g in range(2)]
        MB = T_("MB", [128, 16, 2, 128], BF16)
        MK = T_("MK", [128, 16, 2, 128], BF16)
        G7 = T_("G7", [128, 16, 128], BF16)
        HT = MB[:].rearrange("p h a t -> p (h a t)")[:, 0:NFF * NT].rearrange("p (k t) -> p k t", k=NFF)
        B["HT"] = B["MB"]
        TARm = [T_(f"TARm{i}", [128, 8, 2, 128], BF16) for i in range(2)]
        TBTm = [T_(f"TBTm{i}", [128, 8, 128], BF16) for i in range(2)]
        SBFm = [T_(f"SBFm{i}", [128, 8, 64], BF16) for i in range(2)]
        for i_ in range(2):
            for (tn, tl) in (("TARm", TARm), ("TBTm", TBTm), ("SBFm", SBFm)):
                mset("vector", tl[i_][:], 0.0, [B[f"{tn}{i_}"]])

        mset("vector", LW1[64:65, :], 1.0, [B["LW1"]])
        mset("vector", LA1[64:65, :], 1.0, [B["LA1"]])
        mset("vector", LV1[32:33, :], 1.0, [B["LV1"]])

        def rmsnorm_to(dst, coff, gcol):
            for s_ in range(NS):
                act(JUNK[:], X[:, s_, :], AF.Square, [B["X"]], [B["JUNK"], B["SS"]], accum_out=SS[:, 0:1])
                ts("vector", MS[:, 0:1], SS[:, 0:1], 1.0 / D, 1e-6, ALU.mult, ALU.add, [B["SS"]], [B["MS"]])
                tt("gpsimd", RSTD[:, 0:1], MS[:, 0:1], NH[:, 0:1], ALU.pow, [B["MS"], B["NH"]], [B["RSTD"]])
                act(XNB[:], X[:, s_, :], AF.Copy, [B["X"], B["RSTD"]], [B["XNB"]], scale=RSTD[:, 0:1])
                for kc in range(8):
                    tr(PT[:, kc * 128:(kc + 1) * 128], XNB[:, kc * 128:(kc + 1) * 128], IDENT[:],
                       [B["XNB"], B["IDENT"]], [B["PT"]], sig=(kc == 7))
                for kc in range(8):
                    ts("vector" if kc % 2 else "gpsimd" if False else "vector",
                       dst[:, kc, coff + s_ * 128:coff + (s_ + 1) * 128], PT[:, kc * 128:(kc + 1) * 128],
                       PV[:, kc, gcol:gcol + 1], None, ALU.mult, None, [B["PT"], B["PV"]], [B[dst_name[id(dst)]]])

        dst_name = {id(XNT): "XNT"}

        def sigm_from_tanh(eng, ap, R, W):
            act(ap, ap, AF.Identity, R, W, scale=0.5, bias=0.5)

        OUTB = {}
        VFB = {}
        try:
            chk(1)
            for l in layers:
                W_, S_ = WN[l], SC[l]
                has_v = (l != 0)
                dma(PV[:], W_["vecT"], (), [B["PV"]], "lp0")
                dma(DW[:], W_["dw"], (), [B["DW"]], "lp1")
                for (tile_, bn, row) in ((GPOST, "GPOST", V_GPOST), (GFPOST, "GFPOST", V_GFPOST), (KKT, "KKT", V_KK),
                                         (KAT, "KAT", V_KA), (RKT, "RKT", V_RK), (GNW, "GNW", V_GNW), (GNB, "GNB", V_GNB)):
                    dma(tile_[:], W_["vec"][row, :].partition_broadcast(128), (), [B[bn]], "lp_" + bn)
                ts("vector", HLN[:], PV[:, :, V_LNW:V_LNW + 2], 0.5, None, ALU.mult, None, [B["PV"]], [B["HLN"]])
                dma(L2BUF[:], S_["l2"].rearrange("p a c -> p (a c)"), (), [B["L2BUF"]], "lp2")
                for seq in range(nseq):
                    mset("vector", S32[:], 0.0, [B["S32"]])
                    mset("vector", SBF[:], 0.0, [B["SBF"]])
                    mset("gpsimd", CB[:, :, 0:30], 0.0, CBo)
                    for ti in range(T // NT):
                        t0 = ti * NT
                        xsrc = (x_in if l == layers[0] else out)[seq, t0:t0 + NT, :].rearrange("(s p) d -> p s d", p=128)
                        ob_ = OUTB.setdefault((seq, ti), Buf())
                        vb_ = VFB.setdefault((seq, ti), Buf())
                        dma(X[:], xsrc, ([] if l == layers[0] else [ob_]), [B["X"]], "dx")
                        if ti == 0:
                            mset("vector", XNT[:, :, 0:1], 0.0, [B["XNT"]])
                        else:
                            cp("vector", XNT[:, :, 0:1], CARRY[:], [B["CARRY"]], [B["XNT"]])
                        rmsnorm_to(XNT, 1, V_GPRE)
                        cp("vector", CARRY[:], XNT[:, :, NT:NT + 1], [B["XNT"]], [B["CARRY"]])
                        chk(2)
                        L1, BL1 = slab(S_["l1"].rearrange("p k s c -> p (k s c)"), 4608)
                        L1v = L1[:, 0:4608].rearrange("p (k s c) -> p k s c", k=8, s=2)
                        for (c0, c1, dstt, dn, fn_) in ((0, 64, LW1, "LW1", AF.Tanh), (64, 128, LA1, "LA1", AF.Copy),
                                                       (128, 256, LG1, "LG1", AF.Tanh), (256, 288, LV1, "LV1", AF.Copy)):
                            if c0 == 256 and not has_v:
                                continue
                            M_ = c1 - c0
                            pb, bpb = bank()
                            for kc in range(8):
                                for s2 in range(2):
                                    mm(pb[0:M_, 0:NT], L1v[:, kc, s2, c0:c1], XNT[:, kc, 1 - s2:1 - s2 + NT],
                                       kc == 0 and s2 == 0, kc == 7 and s2 == 1, [BL1, B["XNT"]], [bpb], sig=(kc == 7 and s2 == 1))
                            if dn == "LG1":
                                act(LG1[:], pb[0:128, 0:NT], AF.Tanh, [bpb], [B["LG1"]], scale=0.5)
                                sigm_from_tanh("gpsimd", LG1[:], [B["LG1"]], [B["LG1"]])
                            else:
                                act(dstt[0:M_, :], pb[0:M_, 0:NT], fn_, [bpb], [B[dn]])
                        BL2 = B["L2BUF"]
                        L2v = L2BUF[:, 0:4096].rearrange("p (a c) -> p a c", a=4)
                        chk(3)
                        for q in range(2):
                            FU, BFU = slab(S_["fm"][:, q * 4:(q + 1) * 4].rearrange("p a k c -> p (a k c)"), 4096)
                            FG, BFG = slab(S_["fm"][:, 8 + q * 4:8 + (q + 1) * 4].rearrange("p a k c -> p (a k c)"), 4096)
                            FUv = FU[:, 0:4096].rearrange("p (a k c) -> p a k c", a=4, k=8)
                            FGv = FG[:, 0:4096].rearrange("p (a k c) -> p a k c", a=4, k=8)
                            for o4 in range(4):
                                oc = q * 4 + o4
                                bu, bbu = bank(); bg, bbg = bank()
                                for kc in range(8):
                                    mm(bu[:, 0:NT], FUv[:, o4, kc, :], XNT[:, kc, 1:1 + NT], kc == 0, kc == 7, [BFU, B["XNT"]], [bbu], sig=(kc == 7))
                                for kc in range(8):
                                    mm(bg[:, 0:NT], FGv[:, o4, kc, :], XNT[:, kc, 1:1 + NT], kc == 0, kc == 7, [BFG, B["XNT"]], [bbg], sig=(kc == 7))
                                tf = TMPF[oc % 2]; btf = B[f"TMPF{oc % 2}"]
                                act(tf[:], bg[:, 0:NT], AF.Tanh, [bbg], [btf], scale=0.5)
                                sigm_from_tanh("gpsimd", tf[:], [btf], [btf])
                                tt("vector", CB[:, oc, 30:30 + NT], bu[:, 0:NT], tf[:], ALU.mult, [bbu, btf], [CBo[oc]])
                        def conv_tail():
                            for oc in range(8):
                                ts("vector", ACC[:, oc, :], CB[:, oc, 0:NT], DW[:, oc, 0:1], PV[:, oc, V_CB:V_CB + 1], ALU.mult, ALU.add,
                                   [CBo[oc], B["DW"], B["PV"]], [ACCo[oc]])
                                yield
                            for j in range(1, 31):
                                for oc in range(8):
                                    stt(ACC[:, oc, :], CB[:, oc, j:j + NT], DW[:, oc, j:j + 1], ACC[:, oc, :], ALU.mult, ALU.add,
                                        [CBo[oc], B["DW"], ACCo[oc]], [ACCo[oc]])
                                    yield
                            cp("gpsimd", CB[:, :, 0:30], CB[:, :, NT:NT + 30], CBo, CBo)
                            yield
                            s1, bs1 = PS[5], B["PS5"]; s2b, bs2 = PS[6], B["PS6"]
                            for oc in range(8):
                                mm(s1[:, 0:NT], ONES32[:], ACC[:, oc, :], oc == 0, oc == 7, [B["ONES32"], ACCo[oc]], [bs1], sig=(oc == 7))
                                yield
                            for oc in range(8):
                                tf = TMPF[oc % 2]; btf = B[f"TMPF{oc % 2}"]
                                act(tf[:], ACC[:, oc, :], AF.Square, [ACCo[oc]], [btf])
                                yield
                                mm(s2b[:, 0:NT], ONES32[:], tf[:], oc == 0, oc == 7, [B["ONES32"], btf], [bs2], sig=True)
                                yield
                            act(MEAN[:], s1[:, 0:NT], AF.Copy, [bs1], [B["MEAN"]], scale=1.0 / D)
                            yield
                            act(VAR[:], s2b[:, 0:NT], AF.Copy, [bs2], [B["VAR"]], scale=1.0 / D)
                            yield
                            tt("gpsimd", RS[:], MEAN[:], MEAN[:], ALU.mult, [B["MEAN"]], [B["RS"]])
                            yield
                            tt("gpsimd", VAR[:], VAR[:], RS[:], ALU.subtract, [B["VAR"], B["RS"]], [B["VAR"]])
                            yield
                            ts("gpsimd", VAR[:], VAR[:], 1e-5, None, ALU.add, None, [B["VAR"]], [B["VAR"]])
                            yield
                            tt("gpsimd", RS[:], VAR[:], NH[:], ALU.pow, [B["VAR"], B["NH"]], [B["RS"]])
                            yield
                            for oc in range(8):
                                t1 = TMPF[0]; t2 = TMPF[1]
                                tt("vector", t1[:], ACC[:, oc, :], MEAN[:], ALU.subtract, [ACCo[oc], B["MEAN"]], [B["TMPF0"]])
                                yield
                                tt("gpsimd", t1[:], t1[:], RS[:], ALU.mult, [B["TMPF0"], B["RS"]], [B["TMPF0"]])
                                yield
                                act(t2[:], t1[:], AF.Tanh, [B["TMPF0"], B["HLN"]], [B["TMPF1"]], scale=HLN[:, oc, 0:1], bias=HLN[:, oc, 1:2])
                                yield
                                ts("vector", t1[:], t1[:], PV[:, oc, V_LNW:V_LNW + 1], PV[:, oc, V_LNB:V_LNB + 1], ALU.mult, ALU.add,
                                   [B["TMPF0"], B["PV"]], [B["TMPF0"]])
                                yield
                                sigm_from_tanh("gpsimd", t2[:], [B["TMPF1"]], [B["TMPF1"]])
                                yield
                                tt("vector", CCT[:, oc, :], t1[:], t2[:], ALU.mult, [B["TMPF0"], B["TMPF1"]], [B["CCT"]])
                                yield


                        P.hook = conv_tail(); P.hook_n = 0
                        chk(4)
                        r32, k32, v32, asg, lw, t1, t2 = A
                        bR, bK, bV, bAS, bLW, bT1, bT2 = [B[f"A{i}"] for i in range(7)]
                        for j in range(12):
                            TK, BTK = slab(S_["tok"][:, j].rearrange("p k c -> p (k c)"), 4096)
                            TKv = TK[:, 0:4096].rearrange("p (k c) -> p k c", k=16)
                            pb, bpb = bank()
                            for kc in range(8):
                                for s2 in range(2):
                                    mm(pb[:, 0:256], XNT[:, kc, 1 - s2:1 - s2 + NT], TKv[:, kc * 2 + s2, :],
                                       kc == 0 and s2 == 0, kc == 7 and s2 == 1, [BTK, B["XNT"]], [bpb], sig=(kc == 7 and s2 == 1))
                            dstA = A[j // 4]
                            act(dstA[:, (j % 4) * 256:(j % 4 + 1) * 256], pb[:, 0:256], AF.Copy, [bpb], [B[f"A{j // 4}"]])
                        def lora2(src, K_, a_idx):
                            res = []
                            for hf in range(2):
                                pb, bpb = bank()
                                mm(pb[:, 0:512], src[0:K_, :], L2v[0:K_, a_idx, hf * 512:(hf + 1) * 512], True, True,
                                   [B["LW1"], B["LA1"], B["LV1"], B["LG1"], BL2], [bpb], sig=True)
                                res.append((pb, bpb))
                            return res
                        if l == 0:
                            dma(vf_d[seq, t0:t0 + NT, :], v32[:], [bV], [vb_], "vfo")
                        else:
                            VF = t2
                            dma(VF[:], vf_d[seq, t0:t0 + NT, :], [vb_], [bT2], "vfi")
                            rr = lora2(LV1, 33, 2)
                            for hf, (pb, bpb) in enumerate(rr):
                                act(t1[:, hf * 512:(hf + 1) * 512], pb[:, 0:512], AF.Tanh, [bpb], [bT1], scale=0.5)
                            sigm_from_tanh("gpsimd", t1[:], [bT1], [bT1])
                            tt("gpsimd", VF[:], VF[:], v32[:], ALU.subtract, [bT2, bV], [bT2])
                            tt("gpsimd", VF[:], VF[:], t1[:], ALU.mult, [bT2, bT1], [bT2])
                            tt("gpsimd", v32[:], v32[:], VF[:], ALU.add, [bV, bT2], [bV])
                        rr = lora2(LA1, 65, 1)
                        for hf, (pb, bpb) in enumerate(rr):
                            act(asg[:, hf * 512:(hf + 1) * 512], pb[:, 0:512], AF.Tanh, [bpb], [bAS], scale=0.5)
                        sigm_from_tanh("gpsimd", asg[:], [bAS], [bAS])
                        rr = lora2(LW1, 65, 0)
                        for hf, (pb, bpb) in enumerate(rr):
                            act(lw[:, hf * 512:(hf + 1) * 512], pb[:, 0:512], AF.Tanh, [bpb], [bLW], scale=0.5)
                        ts("gpsimd", lw[:], lw[:], -0.30326533, -0.30326533, ALU.mult, ALU.add, [bLW], [bLW])
                        chk(5)
                        for (ti_, dsts) in ((0, ((EB[0], "EB0", 1.0), (EB[1], "EB1", -1.0))), (1, ((EB[2], "EB2", 1.0),)), (2, ((EB[3], "EB3", 1.0),))):
                            for hf in range(2):
                                pb, bpb = bank()
                                mm(pb[:, 0:512], TRI[:, ti_, :], lw[:, hf * 512:(hf + 1) * 512], True, True, [B["TRI"], bLW], [bpb], sig=True)
                                for (dd, dn, sc_) in dsts:
                                    act(dd[:, hf * 512:(hf + 1) * 512], pb[:, 0:512], AF.Exp, [bpb], [B[dn]], scale=sc_)
                        pb, bpb = bank()
                        for h in range(H):
                            po = (h % 2) * 64
                            mm(pb[po:po + 64, h // 2:h // 2 + 1], lw[:, h * 64:(h + 1) * 64], ONES32[:, 0:1], True, True,
                               [bLW, B["ONES32"]], [bpb], sig=(h == H - 1))
                        act(PC[:], pb[:, 0:8], AF.Exp, [bpb], [B["PC"]])
                        tt("gpsimd", t1[:], k32[:], KKT[:], ALU.mult, [bK, B["KKT"]], [bT1])
                        tt("gpsimd", t2[:], t1[:], t1[:], ALU.mult, [bT1], [bT2])
                        P.op("vector", lambda e: e.tensor_reduce(out=SM[:, :, 0], in_=t2[:].rearrange("p (h c) -> p h c", h=H), axis=AX.X, op=ALU.add), [bT2], [B["SM"]])
                        ts("vector", SM[:, :, 0], SM[:, :, 0], 1e-24, None, ALU.max, None, [B["SM"]], [B["SM"]])
                        tt("gpsimd", SM[:, :, 1], SM[:, :, 0], NH[:, 0:H], ALU.pow, [B["SM"], B["NH"]], [B["SM"]])
                        for h in range(H):
                            ts("vector", t1[:, h * 64:(h + 1) * 64], t1[:, h * 64:(h + 1) * 64], SM[:, h, 1:2], None, ALU.mult, None, [bT1, B["SM"]], [bT1])
                        kk = t1
                        stt(t2[:], asg[:], -1.0, KAT[:], ALU.add, ALU.mult, [bAS, B["KAT"]], [bT2])
                        stt(k32[:], t2[:], 1.0, k32[:], ALU.add, ALU.mult, [bT2, bK], [bK])
                        tt("gpsimd", t2[:], r32[:], k32[:], ALU.mult, [bR, bK], [bT2])
                        tt("gpsimd", t2[:], t2[:], RKT[:], ALU.mult, [bT2, B["RKT"]], [bT2])
                        P.op("vector", lambda e: e.tensor_reduce(out=SM[:, :, 2], in_=t2[:].rearrange("p (h c) -> p h c", h=H), axis=AX.X, op=ALU.add), [bT2], [B["SM"]])
                        tt("gpsimd", asg[:], asg[:], kk[:], ALU.mult, [bAS, bT1], [bAS])
                        bvec = asg
                        tt("vector", KH[:], k32[:], EB[3][:], ALU.mult, [bK, B["EB3"]], [B["KH"]])
                        tt("gpsimd", BH[:], bvec[:], EB[3][:], ALU.mult, [bAS, B["EB3"]], [B["BH"]])
                        cp("gpsimd", VBF[:], v32[:], [bV], [B["VBF"]])
                        def trans_to(srcf, bsrc, eb, ebn, neg, dst_fn, dname, oi):
                            ob = OB[oi]; bob = B[f"OB{oi}"]
                            if neg:
                                stt(ob[:], srcf[:], -1.0, eb[:], ALU.mult, ALU.mult, [bsrc, B[ebn]], [bob])
                            else:
                                tt("vector", ob[:], srcf[:], eb[:], ALU.mult, [bsrc, B[ebn]], [bob])
                            for kc in range(8):
                                tr(PT[:, kc * 128:(kc + 1) * 128], ob[:, kc * 128:(kc + 1) * 128], IDENT[:], [bob, B["IDENT"]], [B["PT"]], sig=(kc == 7))
                            cp("vector", dst_fn, PT[:].rearrange("p (k t) -> p k t", k=8), [B["PT"]], [B[dname]])
                        trans_to(r32, bR, EB[0], "EB0", False, TAR[:, :, 1, :], "TAR", 0)
                        trans_to(kk, bT1, EB[2], "EB2", True, TAR[:, :, 0, :], "TAR", 1)
                        trans_to(bvec, bAS, EB[1], "EB1", False, TBT[:], "TBT", 0)
                        trans_to(k32, bK, EB[1], "EB1", False, TKT[:], "TKT", 1)
                        for i_ in range(2):
                            ps_ = slice(i_ * 64, (i_ + 1) * 64)
                            cp("vector", TARm[i_][ps_], TAR[ps_], [B["TAR"]], [B[f"TARm{i_}"]])
                            cp("gpsimd", TBTm[i_][ps_], TBT[ps_], [B["TBT"]], [B[f"TBTm{i_}"]])
                            cp("gpsimd", SBFm[i_][ps_], SBF[ps_], [B["SBF"]], [B[f"SBFm{i_}"]])
                        chk(6)
                        for pr_ in range(2):
                            st_ = {}
                            for g4 in (2 * pr_, 2 * pr_ + 1):
                                gi = g4 % 2
                                pl, bpl = bank()
                                for hh in range(4):
                                    h = g4 * 4 + hh; kc = h // 2
                                    mm(pl[:, hh * 128:(hh + 1) * 128], TAR[:, kc, 0, :], TBTm[h % 2][:, kc, :], True, True,
                                       [B["TAR"], B[f"TBTm{h % 2}"]], [bpl], sig=(hh == 3))
                                tt("vector", R0[gi][:], pl[:, 0:512], MSKL[:], ALU.mult, [bpl, B["MSKL"]], [B[f"R0_{gi}"]])
                                for (lt, ltn, dstm, dmn) in ((TBT, "TBT", MB, "MB"), (TKT, "TKT", MK, "MK")):
                                    for h2 in range(2):
                                        pb, bpb = bank()
                                        for hh in range(2):
                                            h = g4 * 4 + h2 * 2 + hh; kc = h // 2
                                            mm(pb[:, hh * 256:(hh + 1) * 256], lt[:, kc, :], TARm[h % 2][:, kc, :, :].rearrange("p a t -> p (a t)"),
                                               True, True, [B[ltn], B[f"TARm{h % 2}"]], [bpb], sig=(hh == 1))
                                        h0 = g4 * 4 + h2 * 2
                                        tt("vector", dstm[:, h0:h0 + 2, :, :].rearrange("p h a t -> p (h a t)"), pb[:, 0:512], MSKT[:], ALU.mult,
                                           [bpb, B["MSKT"]], [B[dmn]])
                                cur = QRG[gi][0]; nxt = QRG[gi][1]; bcur = B[f"QRG{gi}_0"]; bnxt = B[f"QRG{gi}_1"]
                                cp("vector", cur[:, 0, :].rearrange("p (h t) -> p h t", h=4), MB[:, g4 * 4:g4 * 4 + 4, 0, :], [B["MB"]], [bcur])
                                cp("gpsimd", cur[:, 1, :], R0[gi][:], [B[f"R0_{gi}"]], [bcur])
                                tt("gpsimd", cur[:, 2, :], cur[:, 0, :], ID4[:], ALU.add, [bcur, B["ID4"]], [bcur])
                                st_[g4] = [cur, nxt, bcur, bnxt]
                            for jj in range(1, 7):
                                pbk = {}
                                for g4 in (2 * pr_, 2 * pr_ + 1):
                                    cur, nxt, bcur, bnxt = st_[g4]
                                    pq, bpq = bank() if jj < 6 else (None, None)
                                    pr, bpr = bank()
                                    for hh in range(4):
                                        cs_ = slice(hh * 128, (hh + 1) * 128)
                                        if jj < 6:
                                            mm(pq[:, cs_], cur[:, 1, cs_], cur[:, 0, cs_], True, True, [bcur], [bpq], sig=(hh == 3))
                                    for hh in range(4):
                                        cs_ = slice(hh * 128, (hh + 1) * 128)
                                        mm(pr[:, cs_], cur[:, 0, cs_], cur[:, 1, cs_], True, True, [bcur], [bpr], sig=(hh == 3))
                                    pbk[g4] = (pq, bpq, pr, bpr)
                                for g4 in (2 * pr_, 2 * pr_ + 1):
                                    cur, nxt, bcur, bnxt = st_[g4]
                                    pq, bpq, pr, bpr = pbk[g4]
                                    if jj < 6:
                                        act(nxt[:, 0, :], pq[:, 0:512], AF.Copy, [bpq], [bnxt])
                                    cp("vector", nxt[:, 1, :], pr[:, 0:512], [bpr], [bnxt])
                                pgk = {}
                                for g4 in (2 * pr_, 2 * pr_ + 1):
                                    cur, nxt, bcur, bnxt = st_[g4]
                                    pg, bpg = bank()
                                    for hh in range(4):
                                        cs_ = slice(hh * 128, (hh + 1) * 128)
                                        mm(pg[:, cs_], nxt[:, 1, cs_], cur[:, 2, cs_], True, True, [bnxt, bcur], [bpg], sig=(hh == 3))
                                    pgk[g4] = (pg, bpg)
                                for g4 in (2 * pr_, 2 * pr_ + 1):
                                    cur, nxt, bcur, bnxt = st_[g4]
                                    pg, bpg = pgk[g4]
                                    if jj < 6:
                                        tt("vector", nxt[:, 2, :], pg[:, 0:512], cur[:, 2, :], ALU.add, [bpg, bcur], [bnxt])
                                    else:
                                        tt("vector", G7[:, g4 * 4:g4 * 4 + 4, :].rearrange("p h t -> p (h t)"), pg[:, 0:512], cur[:, 2, :], ALU.add,
                                           [bpg, bcur], [B["G7"]])
                                    st_[g4] = [nxt, cur, bnxt, bcur]
                        chk(7)
                        for h8 in range(2):
                            px, bpx = bank()
                            for hh in range(8):
                                h = h8 * 8 + hh; kc = h // 2; po = (h % 2) * 64
                                mm(px[:, hh * 64:(hh + 1) * 64], TAR[:, kc, 0, :], SBFm[h % 2][:, kc, :], True, False, [B["TAR"], B[f"SBFm{h % 2}"]], [bpx], sig=False)
                                mm(px[:, hh * 64:(hh + 1) * 64], MK[:, h, 0, :], VBF[:, h * 64:(h + 1) * 64], False, True, [B["MK"], B["VBF"]], [bpx], sig=(hh == 7))
                            cp("vector", XB[:, h8 * 8:(h8 + 1) * 8, :].rearrange("p h c -> p (h c)"), px[:, 0:512], [bpx], [B["XB"]])
                        for h8 in range(2):
                            pu, bpu = bank()
                            for hh in range(8):
                                h = h8 * 8 + hh
                                mm(pu[:, hh * 64:(hh + 1) * 64], G7[:, h, :], XB[:, h, :], True, True, [B["G7"], B["XB"]], [bpu], sig=(hh == 7))
                            cp("vector", UB[:, h8 * 8:(h8 + 1) * 8, :].rearrange("p h c -> p (h c)"), pu[:, 0:512], [bpu], [B["UB"]])
                        ybanks = []
                        for h8 in range(2):
                            py, bpy = bank()
                            for hh in range(8):
                                h = h8 * 8 + hh; kc = h // 2; po = (h % 2) * 64
                                o_ = py[:, hh * 64:(hh + 1) * 64]
                                mm(o_, TAR[:, kc, 1, :], SBFm[h % 2][:, kc, :], True, False, [B["TAR"], B[f"SBFm{h % 2}"]], [bpy], sig=False)
                                mm(o_, MB[:, h, 1, :], UB[:, h, :], False, False, [B["MB"], B["UB"]], [bpy], sig=False)
                                mm(o_, MK[:, h, 1, :], VBF[:, h * 64:(h + 1) * 64], False, True, [B["MK"], B["VBF"]], [bpy], sig=(hh == 7))
                            ybanks.append((py, bpy))
                        pss, bpss = bank()
                        for h in range(H):
                            kc = h // 2; po = (h % 2) * 64
                            o_ = pss[po:po + 64, kc * 64:(kc + 1) * 64]
                            mm(o_, BH[:, h * 64:(h + 1) * 64], UB[:, h, :], True, False, [B["BH"], B["UB"]], [bpss], sig=False)
                            mm(o_, KH[:, h * 64:(h + 1) * 64], VBF[:, h * 64:(h + 1) * 64], False, True, [B["KH"], B["VBF"]], [bpss], sig=(h == H - 1))
                        for kc in range(8):
                            stt(S32[:, kc, :], S32[:, kc, :], PC[:, kc:kc + 1], pss[:, kc * 64:(kc + 1) * 64], ALU.mult, ALU.add,
                                [B["S32"], B["PC"], bpss], [B["S32"]])
                        y32 = r32
                        for h8, (py, bpy) in enumerate(ybanks):
                            act(y32[:, h8 * 512:(h8 + 1) * 512], py[:, 0:512], AF.Copy, [bpy], [bR])
                        cp("gpsimd", SBF[:], S32[:], [B["S32"]], [B["SBF"]])
                        P.op("vector", lambda e: e.tensor_reduce(out=SM[:, :, 0], in_=y32[:].rearrange("p (h c) -> p h c", h=H), axis=AX.X, op=ALU.add), [bR], [B["SM"]])
                        tt("gpsimd", t2[:], y32[:], y32[:], ALU.mult, [bR], [bT2])
                        P.op("vector", lambda e: e.tensor_reduce(out=SM[:, :, 1], in_=t2[:].rearrange("p (h c) -> p h c", h=H), axis=AX.X, op=ALU.add), [bT2], [B["SM"]])
                        ts("vector", SM[:, :, 0], SM[:, :, 0], 1.0 / 64, None, ALU.mult, None, [B["SM"]], [B["SM"]])
                        tt("vector", SM[:, :, 3], SM[:, :, 0], SM[:, :, 0], ALU.mult, [B["SM"]], [B["SM"]])
                        stt(SM[:, :, 1], SM[:, :, 1], 1.0 / 64, SM[:, :, 3], ALU.mult, ALU.subtract, [B["SM"]], [B["SM"]])
                        ts("vector", SM[:, :, 1], SM[:, :, 1], 64e-5, None, ALU.add, None, [B["SM"]], [B["SM"]])
                        tt("gpsimd", SM[:, :, 3], SM[:, :, 1], NH[:, 0:H], ALU.pow, [B["SM"], B["NH"]], [B["SM"]])
                        for h in range(H):
                            hs = slice(h * 64, (h + 1) * 64)
                            ts("vector", y32[:, hs], y32[:, hs], SM[:, h, 0:1], SM[:, h, 3:4], ALU.subtract, ALU.mult, [bR, B["SM"]], [bR])
                        tt("gpsimd", y32[:], y32[:], GNW[:], ALU.mult, [bR, B["GNW"]], [bR])
                        tt("gpsimd", y32[:], y32[:], GNB[:], ALU.add, [bR, B["GNB"]], [bR])
                        for h in range(H):
                            hs = slice(h * 64, (h + 1) * 64)
                            stt(y32[:, hs], v32[:, hs], SM[:, h, 2:3], y32[:, hs], ALU.mult, ALU.add, [bV, B["SM"], bR], [bR])
                        rr = lora2(LG1, 128, 3)
                        for hf, (pb, bpb) in enumerate(rr):
                            tt("vector", OB[0][:, hf * 512:(hf + 1) * 512], pb[:, 0:512], y32[:, hf * 512:(hf + 1) * 512], ALU.mult, [bpb, bR], [B["OB0"]])
                        for kc in range(8):
                            tr(PT[:, kc * 128:(kc + 1) * 128], OB[0][:, kc * 128:(kc + 1) * 128], IDENT[:], [B["OB0"], B["IDENT"]], [B["PT"]], sig=(kc == 7))
                        cp("vector", YT[:], PT[:].rearrange("p (k t) -> p k t", k=8), [B["PT"]], [B["YT"]])

                        if P.hook is not None:
                            P.in_hook = True
                            for _ in P.hook:
                                pass
                            P.in_hook = False
                            P.hook = None
                        chk(8)
                        def fm_proj(slab_src_fn, rhs_t, brhs, oc):
                            pass
                        for q in range(2):
                            RO_, BRO = slab(S_["ro"][:, q * 4:(q + 1) * 4].rearrange("p a k c -> p (a k c)"), 4096)
                            ZR_, BZR = slab(S_["fm"][:, 16 + q * 4:16 + (q + 1) * 4].rearrange("p a k c -> p (a k c)"), 4096)
                            for o4 in range(4):
                                oc = q * 4 + o4
                                py_, bpy_ = bank(); pz_, bpz_ = bank()
                                ROv = RO_[:, 0:4096].rearrange("p (a k c) -> p a k c", a=4, k=8)
                                ZRv = ZR_[:, 0:4096].rearrange("p (a k c) -> p a k c", a=4, k=8)
                                for kc in range(8):
                                    mm(py_[:, 0:NT], ROv[:, o4, kc, :], YT[:, kc, :], kc == 0, kc == 7, [BRO, B["YT"]], [bpy_], sig=(kc == 7))
                                for kc in range(8):
                                    mm(pz_[:, 0:NT], ZRv[:, o4, kc, :], XNT[:, kc, 1:1 + NT], kc == 0, kc == 7, [BZR, B["XNT"]], [bpz_], sig=(kc == 7))
                                tf = TMPF[0]
                                act(tf[:], pz_[:, 0:NT], AF.Tanh, [bpz_], [B["TMPF0"]], scale=0.5)
                                sigm_from_tanh("gpsimd", tf[:], [B["TMPF0"]], [B["TMPF0"]])
                                tt("vector", ACC[:, oc, :], py_[:, 0:NT], tf[:], ALU.mult, [bpy_, B["TMPF0"]], [ACCo[oc]])
                        for q in range(2):
                            CO_, BCO = slab(S_["co"][:, q * 4:(q + 1) * 4].rearrange("p a k c -> p (a k c)"), 4096)
                            ZC_, BZC = slab(S_["fm"][:, 24 + q * 4:24 + (q + 1) * 4].rearrange("p a k c -> p (a k c)"), 4096)
                            for o4 in range(4):
                                oc = q * 4 + o4
                                py_, bpy_ = bank(); pz_, bpz_ = bank()
                                COv = CO_[:, 0:4096].rearrange("p (a k c) -> p a k c", a=4, k=8)
                                ZCv = ZC_[:, 0:4096].rearrange("p (a k c) -> p a k c", a=4, k=8)
                                for kc in range(8):
                                    mm(py_[:, 0:NT], COv[:, o4, kc, :], CCT[:, kc, :], kc == 0, kc == 7, [BCO, B["CCT"]], [bpy_], sig=(kc == 7))
                                for kc in range(8):
                                    mm(pz_[:, 0:NT], ZCv[:, o4, kc, :], XNT[:, kc, 1:1 + NT], kc == 0, kc == 7, [BZC, B["XNT"]], [bpz_], sig=(kc == 7))
                                tf = TMPF[1]
                                act(tf[:], pz_[:, 0:NT], AF.Tanh, [bpz_], [B["TMPF1"]], scale=0.5)
                                sigm_from_tanh("gpsimd", tf[:], [B["TMPF1"]], [B["TMPF1"]])
                                tt("vector", tf[:], py_[:, 0:NT], tf[:], ALU.mult, [bpy_, B["TMPF1"]], [B["TMPF1"]])
                                tt("gpsimd", MRG[:, oc, :], tf[:], ACC[:, oc, :], ALU.add, [B["TMPF1"], ACCo[oc]], [B["MRG"]])

                        def out_norm_resid(pbs, gtile, gname):
                            for hf, (pb, bpb) in enumerate(pbs):
                                act(JUNK[:, 0:512], pb[:, 0:512], AF.Square, [bpb], [B["JUNK"], B["SS"]], accum_out=SS[:, hf:hf + 1])
                            tt("vector", MS[:, 0:1], SS[:, 0:1], SS[:, 1:2], ALU.add, [B["SS"]], [B["MS"]])
                            ts("vector", MS[:, 0:1], MS[:, 0:1], 1.0 / D, 1e-6, ALU.mult, ALU.add, [B["MS"]], [B["MS"]])
                            tt("gpsimd", RSTD[:, 0:1], MS[:, 0:1], NH[:, 0:1], ALU.pow, [B["MS"], B["NH"]], [B["RSTD"]])
                            for hf, (pb, bpb) in enumerate(pbs):
                                hs = slice(hf * 512, (hf + 1) * 512)
                                stt(A[5][:, hs], pb[:, 0:512], RSTD[:, 0:1], gtile[:, hs], ALU.mult, ALU.mult, [bpb, B["RSTD"], B[gname]], [B["A5"]])
                            tt("gpsimd", X[:, 0, :], X[:, 0, :], A[5][:], ALU.add, [B["X"], B["A5"]], [B["X"]])

                        WO_, BWO = [], []
                        pbs = []
                        for hf in range(2):
                            w_, bw_ = slab(S_["wo"][:, :, hf * 512:(hf + 1) * 512], 4096)
                            wv = w_[:, 0:4096].rearrange("p (k c) -> p k c", k=8)
                            pb, bpb = bank()
                            for kc in range(8):
                                mm(pb[:, 0:512], MRG[:, kc, :], wv[:, kc, :], kc == 0, kc == 7, [B["MRG"], bw_], [bpb], sig=(kc == 7))
                            pbs.append((pb, bpb))
                        out_norm_resid(pbs, GPOST, "GPOST")
                        if ti == 0 and seq == 0 and l == layers[0]:
                            dump("d_x1", X[:, 0, :], [128, D], F32, [B["X"]])
                            dump("d_yt", YT[:], [128, 8, NT], BF16, [B["YT"]])
                            dump("d_cct", CCT[:], [128, 8, NT], BF16, [B["CCT"]])
                            dump("d_mrg", MRG[:], [128, 8, NT], BF16, [B["MRG"]])
                            dump("d_xnt", XNT[:], [128, 8, NT + 1], BF16, [B["XNT"]])
                            dump("d_v", A[2][:], [128, D], F32, [B["A2"]])
                            dump("d_k", A[1][:], [128, D], F32, [B["A1"]])
                            dump("d_y", A[0][:], [128, D], F32, [B["A0"]])
                            dump("d_b", A[3][:], [128, D], F32, [B["A3"]])
                            dump("d_lw", A[4][:], [128, D], F32, [B["A4"]])
                        rmsnorm_to(XNT, 1, V_GFPRE)
                        for q in range(6):
                            n4 = 4 if q < 5 else 2
                            G_, BG_ = slab(S_["g"][:, q * 4:q * 4 + n4].rearrange("p a k c -> p (a k c)"), n4 * 1024)
                            U_, BU_ = slab(S_["u"][:, q * 4:q * 4 + n4].rearrange("p a k c -> p (a k c)"), n4 * 1024)
                            Gv = G_[:, 0:n4 * 1024].rearrange("p (a k c) -> p a k c", a=n4, k=8)
                            Uv = U_[:, 0:n4 * 1024].rearrange("p (a k c) -> p a k c", a=n4, k=8)
                            for o4 in range(n4):
                                oc = q * 4 + o4
                                pg_, bpg_ = bank(); pu_, bpu_ = bank()
                                for kc in range(8):
                                    mm(pg_[:, 0:NT], Gv[:, o4, kc, :], XNT[:, kc, 1:1 + NT], kc == 0, kc == 7, [BG_, B["XNT"]], [bpg_], sig=(kc == 7))
                                for kc in range(8):
                                    mm(pu_[:, 0:NT], Uv[:, o4, kc, :], XNT[:, kc, 1:1 + NT], kc == 0, kc == 7, [BU_, B["XNT"]], [bpu_], sig=(kc == 7))
                                tf = TMPF[oc % 2]; btf = B[f"TMPF{oc % 2}"]
                                act(tf[:], pg_[:, 0:NT], AF.Tanh, [bpg_], [btf], scale=0.5)
                                sigm_from_tanh("gpsimd", tf[:], [btf], [btf])
                                tt("vector", tf[:], pg_[:, 0:NT], tf[:], ALU.mult, [bpg_, btf], [btf])
                                tt("vector", HT[:, oc, :], pu_[:, 0:NT], tf[:], ALU.mult, [bpu_, btf], [B["HT"]])
                        pbs = []
                        for hf in range(2):
                            pb, bpb = bank()
                            for q in range(3):
                                n8 = 8 if q < 2 else 6
                                w_, bw_ = slab(S_["d"][:, q * 8:q * 8 + n8, hf * 512:(hf + 1) * 512], n8 * 512)
                                wv = w_[:, 0:n8 * 512].rearrange("p (k c) -> p k c", k=n8)
                                for k8 in range(n8):
                                    kc = q * 8 + k8
                                    mm(pb[:, 0:512], HT[:, kc, :], wv[:, k8, :], kc == 0, kc == NFF - 1, [B["HT"], bw_], [bpb], sig=(k8 == n8 - 1))
                            pbs.append((pb, bpb))
                        out_norm_resid(pbs, GFPOST, "GFPOST")
                        dma(out[seq, t0:t0 + NT, :], X[:, 0, :], [B["X"]], [ob_], "dxo")
        except StopBuild:
            pass
        sems = {}
        for k in list(Plan.ENGS) + list(P.dma_cnt.keys()):
            sems[k] = st.enter_context(nc.semaphore("zq_" + k + "_sm"))
        P.emit(nc, sems, {"sync": ([("dxo", P.dma_cnt["dxo"])] if "dxo" in P.dma_cnt else []) + ([("vfo", P.dma_cnt["vfo"])] if "vfo" in P.dma_cnt else []) + [(d_, 16) for d_ in dumps]})
    return nc, P


def _fm_layout(W):
    n = W.shape[1] // 128
    return np.ascontiguousarray(W.reshape(8, 128, n, 128).transpose(1, 2, 0, 3))


def _tok_layout(W):
    K = W.shape[0] // 128
    return np.ascontiguousarray(W.reshape(K, 128, W.shape[1]).transpose(1, 0, 2))


def host_consts():
    s = np.arange(128)[:, None]
    t = np.arange(128)[None, :]
    c = {}
    c["ident"] = np.eye(128).astype(ml_dtypes.bfloat16)
    c["tri3"] = np.ascontiguousarray(np.stack([(s <= t), (s < t), (s > t)], axis=1).astype(np.float32))
    c["mskL"] = np.ascontiguousarray(np.tile((s > t).astype(np.float32), (1, 4)))
    mT = np.concatenate([(t > s), (t >= s)], axis=1).astype(np.float32)
    c["mskT"] = np.ascontiguousarray(np.tile(mT, (1, 2)))
    c["ident4"] = np.ascontiguousarray(np.tile(np.eye(128, dtype=np.float32), (1, 4)))
    return c


def host_layer(inp, l):
    f = lambda a: np.asarray(a, dtype=np.float32)
    w_in = f(inp["w_in"][l])
    d = {}
    d[f"wtok{l}"] = _tok_layout(w_in[:, 0:3072])
    d[f"wfm{l}"] = _fm_layout(w_in[:, 3072:7168])
    v1 = f(inp["vres_1"][l - 1]) if l > 0 else np.zeros((D, 32), np.float32)
    d[f"wl1{l}"] = _tok_layout(np.concatenate([f(inp["decay_w1"][l]), f(inp["a_1"][l]), f(inp["g_1"][l]), v1], axis=1))
    l2 = np.zeros((128, 4, D), np.float32)
    l2[0:64, 0] = f(inp["decay_w2"][l]); l2[64, 0] = f(inp["decay_w0"][l])
    l2[0:64, 1] = f(inp["a_2"][l]); l2[64, 1] = f(inp["a_0"][l])
    if l > 0:
        l2[0:32, 2] = f(inp["vres_2"][l - 1]); l2[32, 2] = f(inp["vres_0"][l - 1])
    l2[0:128, 3] = f(inp["g_2"][l])
    d[f"wl2{l}"] = l2
    d[f"wro{l}"] = _fm_layout(f(inp["w_rwkv_out"][l]))
    d[f"wco{l}"] = _fm_layout(f(inp["w_conv_out"][l]))
    d[f"wo{l}"] = _tok_layout(f(inp["w_out"][l]))
    d[f"wg{l}"] = _fm_layout(f(inp["ffn_w_gate"][l]))
    d[f"wu{l}"] = _fm_layout(f(inp["ffn_w_up"][l]))
    d[f"wd{l}"] = _tok_layout(f(inp["ffn_w_down"][l]))
    vres_mu = f(inp["vres_mu"][l - 1]) if l > 0 else np.zeros(D, np.float32)
    rows = [inp["pre_mix_norm"][l], inp["post_mix_norm"][l], inp["pre_ffn_norm"][l], inp["post_ffn_norm"][l],
            inp["mu_rkv"][l][0], inp["mu_rkv"][l][1], inp["mu_rkv"][l][2],
            inp["mu_wag"][l][0], inp["mu_wag"][l][1], inp["mu_wag"][l][2], vres_mu,
            inp["k_k"][l], inp["k_a"][l], np.asarray(inp["r_k"][l]).reshape(-1), inp["gn_w"][l], inp["gn_b"][l],
            inp["conv_b"][l], inp["conv_ln_w"][l], inp["conv_ln_b"][l]]
    d[f"vec{l}"] = np.ascontiguousarray(np.stack([f(r) for r in rows], axis=0))
    d[f"vecT{l}"] = np.ascontiguousarray(d[f"vec{l}"].reshape(NVEC, 8, 128).transpose(2, 1, 0))
    d[f"dwT{l}"] = np.ascontiguousarray(f(inp["conv_dw"][l]).reshape(31, 8, 128).transpose(2, 1, 0))
    return d


_PROG = {}


def kernel(**inputs):
    x = np.asarray(inputs["x"], dtype=np.float32)
    Bt, T, _ = x.shape
    nseq = Bt // 8
    key = (T, nseq)
    if key not in _PROG:
        _PROG[key] = build(T, nseq, [0, 1, 2, 3])[0]
    nc = _PROG[key]
    base = host_consts()
    for l in range(4):
        base.update(host_layer(inputs, l))
    in_maps = []
    for c in range(8):
        m = {"x": np.ascontiguousarray(x[c * nseq:(c + 1) * nseq])}
        m.update(base)
        in_maps.append(m)
    res = run_bass_kernel_spmd(nc, in_maps, core_ids=list(range(8)))
    return np.concatenate([res.results[c]["out"] for c in range(8)], axis=0).astype(np.float32)
```

```python
import numpy as np
import ml_dtypes
from contextlib import ExitStack
import concourse.bass as bass
import concourse.mybir as mybir
from concourse.bass_utils import run_bass_kernel_spmd

F32 = mybir.dt.float32
BF16 = mybir.dt.bfloat16
AF = mybir.ActivationFunctionType
ALU = mybir.AluOpType
AX = mybir.AxisListType

D = 1024
H = 16
NFF = 22
NT = 128
NS = NT // 128
NVEC = 19
(V_GPRE, V_GPOST, V_GFPRE, V_GFPOST, V_MUR, V_MUK, V_MUV, V_MUW, V_MUA, V_MUG, V_MUVR, V_KK, V_KA, V_RK,
 V_GNW, V_GNB, V_CB, V_LNW, V_LNB) = range(NVEC)


class Buf:
    __slots__ = ("name", "w", "r")

    def __init__(self, name=""):
        self.name = name
        self.w = None
        self.r = []


class Plan:
    ENGS = ("tensor", "vector", "scalar", "gpsimd", "sync")

    def __init__(self):
        self.streams = {e: [] for e in self.ENGS}
        self.cnt = {e: 0 for e in self.ENGS}
        self.waited = {e: {} for e in self.ENGS}
        self.dma_cnt = {}
        self.hook = None
        self.in_hook = False
        self.hook_n = 0

    def _need(self, eng, ev, waits):
        if ev is None:
            return
        k, v = ev
        if k == eng and v > self.cnt[eng]:
            return
        if self.waited[eng].get(k, 0) >= v:
            return
        if waits.get(k, 0) < v:
            waits[k] = v

    def op(self, eng, fn, reads=(), writes=(), sig=True, dma_sem=None, noself=False):
        waits = {}
        for b in reads:
            self._need(eng, b.w, waits)
        for b in writes:
            self._need(eng, b.w, waits)
            for ev in b.r:
                self._need(eng, ev, waits)
        wl = []
        for k, v in waits.items():
            if noself and k == eng:
                continue
            self.waited[eng][k] = v
            wl.append((k, v))
        if dma_sem is not None:
            self.dma_cnt[dma_sem] = self.dma_cnt.get(dma_sem, 0) + 16
            ev = (dma_sem, self.dma_cnt[dma_sem])
            self.streams[eng].append((wl, fn, dma_sem, 16))
        elif sig:
            self.cnt[eng] += 1
            ev = (eng, self.cnt[eng])
            self.streams[eng].append((wl, fn, eng, 1))
        else:
            self.streams[eng].append((wl, fn, None, 0))
            ev = (eng, self.cnt[eng] + 1)
        for b in reads:
            if len(b.r) > 8:
                b.r = [e for e in b.r if not (e[0] == ev[0] and e[1] <= ev[1])]
            b.r.append(ev)
        for b in writes:
            b.w = ev
            b.r = []
        if self.hook is not None and not self.in_hook:
            self.hook_n += 1
            if self.hook_n % 3 == 0:
                self.in_hook = True
                try:
                    next(self.hook)
                except StopIteration:
                    self.hook = None
                self.in_hook = False

    def barrier(self):
        evs = [(e, self.cnt[e]) for e in self.ENGS if self.cnt[e] > 0]
        evs += [(k, v) for k, v in self.dma_cnt.items()]
        for e in self.ENGS:
            wl = []
            for (k, v) in evs:
                if k == e:
                    continue
                if self.waited[e].get(k, 0) < v:
                    self.waited[e][k] = v
                    wl.append((k, v))
            if wl:
                self.streams[e].append((wl, None, None, 0))

    def emit(self, nc, sems, final_waits):
        with nc.Block() as block:
            def mk(engname):
                def body(e):
                    for (wl, fn, sk, inc) in self.streams[engname]:
                        for (k, v) in wl:
                            e.wait_ge(sems[k], v)
                        if fn is None:
                            continue
                        ins = fn(e)
                        if sk is not None:
                            ins.then_inc(sems[sk], inc)
                    for (k, v) in final_waits.get(engname, []):
                        e.wait_ge(sems[k], v)
                return body
            block.tensor(mk("tensor"))
            block.vector(mk("vector"))
            block.scalar(mk("scalar"))
            block.gpsimd(mk("gpsimd"))
            block.sync(mk("sync"))


class StopBuild(Exception):
    pass


def build(T, nseq, layers, dbg=False, kstop=0):
    def chk(n):
        if kstop == n:
            raise StopBuild()
    nc = bass.Bass("TRN2", target_bir_lowering=False)
    P = Plan()
    NL = len(layers)
    dr = {}

    def din(name, shape, dt=F32):
        dr[name] = nc.dram_tensor(name, list(shape), dt, kind="ExternalInput").ap()
        return dr[name]

    x_in = din("x", [nseq, T, D])
    ident_d = din("ident", [128, 128], BF16)
    tri_d = din("tri3", [128, 3, 128])
    mskL_d = din("mskL", [128, 512])
    mskT_d = din("mskT", [128, 512])
    id4_d = din("ident4", [128, 512])
    WN = {}
    for l in layers:
        WN[l] = dict(
            tok=din(f"wtok{l}", [128, 8, 3072]), fm=din(f"wfm{l}", [128, 32, 8, 128]),
            l1=din(f"wl1{l}", [128, 8, 288]), l2=din(f"wl2{l}", [128, 4, 1024]),
            ro=din(f"wro{l}", [128, 8, 8, 128]), co=din(f"wco{l}", [128, 8, 8, 128]),
            wo=din(f"wo{l}", [128, 8, 1024]), g=din(f"wg{l}", [128, 8, NFF * 128]),
            u=din(f"wu{l}", [128, 8, NFF * 128]), d=din(f"wd{l}", [128, NFF, 1024]),
            vec=din(f"vec{l}", [NVEC, D]), vecT=din(f"vecT{l}", [128, 8, NVEC]), dw=din(f"dwT{l}", [128, 8, 31]))
    out = nc.dram_tensor("out", [nseq, T, D], F32, kind="ExternalOutput").ap()
    if 0 in layers and NL == 1:
        vf_d = nc.dram_tensor("vf", [nseq, T, D], F32, kind="ExternalOutput").ap()
    elif 0 in layers:
        vf_d = nc.dram_tensor("vf", [nseq, T, D], F32).ap()
    else:
        vf_d = din("vf", [nseq, T, D])
    SC = {}
    for l in layers:
        SC[l] = dict(
            tok=nc.dram_tensor(f"s_tok{l}", [128, 12, 16, 256], BF16).ap(),
            fm=nc.dram_tensor(f"s_fm{l}", [128, 32, 8, 128], BF16).ap(),
            l1=nc.dram_tensor(f"s_l1{l}", [128, 8, 2, 288], BF16).ap(),
            l2=nc.dram_tensor(f"s_l2{l}", [128, 4, 1024], BF16).ap(),
            ro=nc.dram_tensor(f"s_ro{l}", [128, 8, 8, 128], BF16).ap(),
            co=nc.dram_tensor(f"s_co{l}", [128, 8, 8, 128], BF16).ap(),
            wo=nc.dram_tensor(f"s_wo{l}", [128, 8, 1024], BF16).ap(),
            g=nc.dram_tensor(f"s_g{l}", [128, 8, NFF * 128], BF16).ap(),
            u=nc.dram_tensor(f"s_u{l}", [128, 8, NFF * 128], BF16).ap(),
            d=nc.dram_tensor(f"s_d{l}", [128, NFF, 1024], BF16).ap())

    with ExitStack() as st:
        def sb(name, shape, dt=F32):
            return st.enter_context(nc.sbuf_tensor(name, list(shape), dt))
        B = {}

        def nb(name):
            B[name] = Buf(name)
            return B[name]

        def act(out_, in_, func, R, W, **kw):
            P.op("scalar", lambda e: e.activation(out=out_, in_=in_, func=func, **kw), R, W)

        def ts(eng, out_, in0, s1, s2, op0, op1, R, W):
            if s2 is None:
                P.op(eng, lambda e: e.tensor_scalar(out=out_, in0=in0, scalar1=s1, scalar2=None, op0=op0), R, W)
            else:
                P.op(eng, lambda e: e.tensor_scalar(out=out_, in0=in0, scalar1=s1, scalar2=s2, op0=op0, op1=op1), R, W)

        def tt(eng, out_, in0, in1, op, R, W):
            P.op(eng, lambda e: e.tensor_tensor(out=out_, in0=in0, in1=in1, op=op), R, W)

        def stt(out_, in0, scalar, in1, op0, op1, R, W, noself=False):
            P.op("vector", lambda e: e.scalar_tensor_tensor(out=out_, in0=in0, scalar=scalar, in1=in1, op0=op0, op1=op1), R, W, noself=noself)

        def cp(eng, out_, in_, R, W):
            P.op(eng, lambda e: e.tensor_copy(out=out_, in_=in_), R, W)

        def mset(eng, ap, val, W):
            P.op(eng, lambda e: e.memset(ap, val), (), W)

        def mm(out_, lhsT, rhs, start, stop, R, W, sig):
            P.op("tensor", lambda e: e.matmul(out_, lhsT, rhs, start=start, stop=stop), R, W, sig=sig)

        def tr(out_, in_, idn, R, W, sig):
            P.op("tensor", lambda e: e.transpose(out_, in_, idn), R, W, sig=sig)

        dma_rr = [0]

        def dma(out_, in_, R, W, sem, eng="sync"):
            P.op(eng, lambda e: e.dma_start(out=out_, in_=in_), R, W, dma_sem=sem)

        dumps = []

        def dump(name, ap_, shape, dt_, bufs):
            if not dbg:
                return
            t_ = nc.dram_tensor(name, list(shape), dt_, kind="ExternalOutput").ap()
            dma(t_, ap_, bufs, (), "dbg_" + name)
            dumps.append("dbg_" + name)

        IDENT = sb("IDENT", [128, 128], BF16); nb("IDENT")
        TRI = sb("TRI", [128, 3, 128]); nb("TRI")
        MSKL = sb("MSKL", [128, 512]); nb("MSKL")
        MSKT = sb("MSKT", [128, 512]); nb("MSKT")
        ID4 = sb("ID4", [128, 512]); nb("ID4")
        ONES32 = sb("ONES32", [128, 128]); nb("ONES32")
        NH = sb("NH", [128, NT]); nb("NH")
        dma(IDENT[:], ident_d, (), [B["IDENT"]], "c0")
        dma(TRI[:], tri_d, (), [B["TRI"]], "c1")
        dma(MSKL[:], mskL_d, (), [B["MSKL"]], "c2")
        dma(MSKT[:], mskT_d, (), [B["MSKT"]], "c3")
        dma(ID4[:], id4_d, (), [B["ID4"]], "c4")
        mset("gpsimd", ONES32[:], 1.0, [B["ONES32"]])
        mset("gpsimd", NH[:], -0.5, [B["NH"]])

        with ExitStack() as pst:
            def psb(name, shape, dt=F32):
                return pst.enter_context(nc.sbuf_tensor(name, list(shape), dt))
            STG = [psb(f"STG{i}", [128, 4096]) for i in range(2)]
            STB = [psb(f"STB{i}", [128, 4608], BF16) for i in range(2)]
            for i in range(2):
                nb(f"STG{i}"); nb(f"STB{i}")
            VB = psb("VB", [128, 3, 1024]); nb("VB")
            VB1 = psb("VB1", [128, 3, 1024]); nb("VB1")
            VT = psb("VT", [128, 8, NVEC]); nb("VT")
            VT1 = psb("VT1", [128, 8, 4]); nb("VT1")
            pk = [0]

            def plain(dst2d, src2d, n):
                for c0 in range(0, n, 4096):
                    c1 = min(n, c0 + 4096)
                    i = pk[0] % 2; pk[0] += 1
                    dma(STG[i][:, 0:c1 - c0], src2d[:, c0:c1], (), [B[f"STG{i}"]], f"pi{i}")
                    if i == 0:
                        cp("vector", STB[i][:, 0:c1 - c0], STG[i][:, 0:c1 - c0], [B[f"STG{i}"]], [B[f"STB{i}"]])
                    else:
                        act(STB[i][:, 0:c1 - c0], STG[i][:, 0:c1 - c0], AF.Copy, [B[f"STG{i}"]], [B[f"STB{i}"]])
                    dma(dst2d[:, c0:c1], STB[i][:, 0:c1 - c0], [B[f"STB{i}"]], (), f"po{i}", eng="gpsimd")

            for l in layers:
                W_, S_ = WN[l], SC[l]
                for j in range(3):
                    dma(VB[:, j, :], W_["vec"][V_MUR + j, :].partition_broadcast(128), (), [B["VB"]], "pv0")
                dma(VT[:], W_["vecT"], (), [B["VT"]], "pv1")
                ts("vector", VB1[:], VB[:], -1.0, 1.0, ALU.mult, ALU.add, [B["VB"]], [B["VB1"]])
                ts("vector", VT1[:], VT[:, :, V_MUW:V_MUW + 4], -1.0, 1.0, ALU.mult, ALU.add, [B["VT"]], [B["VT1"]])
                for j in range(12):
                    i = pk[0] % 2; pk[0] += 1
                    wi = j // 4
                    dma(STG[i][:, 0:2048].rearrange("p (k c) -> p k c", k=8), W_["tok"][:, :, j * 256:(j + 1) * 256],
                        (), [B[f"STG{i}"]], f"pi{i}")
                    for s in range(2):
                        vb = (VB1 if s == 0 else VB)
                        for kc in range(8):
                            tt("vector" if kc % 2 == 0 else "gpsimd",
                               STB[i][:, (kc * 2 + s) * 256:(kc * 2 + s + 1) * 256],
                               STG[i][:, kc * 256:(kc + 1) * 256], vb[:, wi, (j % 4) * 256:(j % 4 + 1) * 256], ALU.mult,
                               [B[f"STG{i}"], B["VB"], B["VB1"]], [B[f"STB{i}"]])
                    dma(S_["tok"][:, j, :, :], STB[i][:, 0:4096].rearrange("p (k c) -> p k c", k=16),
                        [B[f"STB{i}"]], (), f"po{i}", eng="gpsimd")
                i = pk[0] % 2; pk[0] += 1
                dma(STG[i][:, 0:2304].rearrange("p (k c) -> p k c", k=8), W_["l1"], (), [B[f"STG{i}"]], f"pi{i}")
                for (c0, c1, mi) in ((0, 64, 0), (64, 128, 1), (128, 256, 2), (256, 288, 3)):
                    for kc in range(8):
                        for s in range(2):
                            sc_ = (VT1[:, kc, mi:mi + 1] if s == 0 else VT[:, kc, V_MUW + mi:V_MUW + mi + 1])
                            ts("vector", STB[i][:, (kc * 2 + s) * 288 + c0:(kc * 2 + s) * 288 + c1],
                               STG[i][:, kc * 288 + c0:kc * 288 + c1], sc_, None, ALU.mult, None,
                               [B[f"STG{i}"], B["VT"], B["VT1"]], [B[f"STB{i}"]])
                dma(S_["l1"].rearrange("p k s c -> p (k s c)"), STB[i][:, 0:4608],
                    [B[f"STB{i}"]], (), f"po{i}", eng="gpsimd")
                plain(S_["l2"].rearrange("p a c -> p (a c)"), W_["l2"].rearrange("p a c -> p (a c)"), 4096)
                plain(S_["fm"].rearrange("p a k c -> p (a k c)"), W_["fm"].rearrange("p a k c -> p (a k c)"), 32768)
                plain(S_["ro"].rearrange("p a k c -> p (a k c)"), W_["ro"].rearrange("p a k c -> p (a k c)"), 8192)
                plain(S_["co"].rearrange("p a k c -> p (a k c)"), W_["co"].rearrange("p a k c -> p (a k c)"), 8192)
                plain(S_["wo"].rearrange("p k c -> p (k c)"), W_["wo"].rearrange("p k c -> p (k c)"), 8192)
                plain(S_["g"].rearrange("p k c -> p (k c)"), W_["g"].rearrange("p k c -> p (k c)"), NFF * 1024)
                plain(S_["u"].rearrange("p k c -> p (k c)"), W_["u"].rearrange("p k c -> p (k c)"), NFF * 1024)
                plain(S_["d"].rearrange("p k c -> p (k c)"), W_["d"].rearrange("p k c -> p (k c)"), NFF * 1024)
            P.barrier()
        RING = [sb(f"RING{i}", [128, 4608], BF16) for i in range(3)]
        for i in range(3):
            nb(f"RING{i}")
        rk = [0]

        def slab(src2d, n):
            i = rk[0] % 3; rk[0] += 1
            dma(RING[i][:, 0:n], src2d, (), [B[f"RING{i}"]], f"rg{i}")
            return RING[i], B[f"RING{i}"]

        PS = [st.enter_context(nc.psum_tensor(f"PSB{i}", [128, 512], F32)) for i in range(7)]
        PT = st.enter_context(nc.psum_tensor("PTB", [128, 1024], BF16)); nb("PT")
        for i in range(7):
            nb(f"PS{i}")
        pk2 = [0]

        def bank():
            i = pk2[0] % 5; pk2[0] += 1
            return PS[i], B[f"PS{i}"]

        def T_(name, shape, dt=F32):
            nb(name)
            return sb(name, shape, dt)
        X = T_("X", [128, NS, D])
        XNT = T_("XNT", [128, 8, NT + 1], BF16)
        CARRY = T_("CARRY", [128, 8, 1], BF16)
        L2BUF = T_("L2BUF", [128, 4096], BF16)
        CB = T_("CB", [128, 8, NT + 30])
        ACC = T_("ACC", [128, 8, NT])
        CBo = [Buf() for _ in range(8)]
        CTb = None
        ACCo = [Buf() for _ in range(8)]
        CCT = T_("CCT", [128, 8, NT], BF16)
        YT = T_("YT", [128, 8, NT], BF16)
        MRG = T_("MRG", [128, 8, NT], BF16)
        SS = T_("SS", [128, 4]); MS = T_("MS", [128, 4]); RSTD = T_("RSTD", [128, 4])
        LW1 = T_("LW1", [65, NT], BF16); LA1 = T_("LA1", [65, NT], BF16); LV1 = T_("LV1", [33, NT], BF16)
        LG1 = T_("LG1", [128, NT], BF16)
        TMPF = [T_(f"TMPF{i}", [128, NT]) for i in range(2)]
        MEAN = T_("MEAN", [128, NT]); VAR = T_("VAR", [128, NT]); RS = T_("RS", [128, NT])
        PV = T_("PV", [128, 8, NVEC])
        HLN = T_("HLN", [128, 8, 2])
        DW = T_("DW", [128, 8, 31])
        GPOST = T_("GPOST", [128, D]); GFPOST = T_("GFPOST", [128, D])
        KKT = T_("KKT", [128, D]); KAT = T_("KAT", [128, D]); RKT = T_("RKT", [128, D])
        GNW = T_("GNW", [128, D]); GNB = T_("GNB", [128, D])
        A = [T_(f"A{i}", [128, D]) for i in range(7)]
        EB = [T_(f"EB{i}", [128, D], BF16) for i in range(4)]
        OB = [T_(f"OB{i}", [128, D], BF16) for i in range(2)]
        JUNK = OB[1]; B["JUNK"] = B["OB1"]
        XNB = OB[0]; B["XNB"] = B["OB0"]
        KH = T_("KH", [128, D], BF16); BH = T_("BH", [128, D], BF16); VBF = T_("VBF", [128, D], BF16)
        TAR = T_("TAR", [128, 8, 2, 128], BF16)
        TBT = T_("TBT", [128, 8, 128], BF16); TKT = T_("TKT", [128, 8, 128], BF16)
        SM = T_("SM", [128, 16, 4])
        S32 = T_("S32", [128, 8, 64]); SBF = T_("SBF", [128, 8, 64], BF16); PC = T_("PC", [128, 8])
        XB = T_("XB", [128, 16, 64], BF16); UB = T_("UB", [128, 16, 64], BF16)
        R0 = [T_(f"R0_{g}", [128, 512], BF16) for g in range(2)]
        QRG = [[T_(f"QRG{g}_{i}", [128, 3, 512], BF16) for i in range(2)] for g in range(2)]
        MB = T_("MB", [128, 16, 2, 128], BF16)
        MK = T_("MK", [128, 16, 2, 128], BF16)
        G7 = T_("G7", [128, 16, 128], BF16)
        HT = MB[:].rearrange("p h a t -> p (h a t)")[:, 0:NFF * NT].rearrange("p (k t) -> p k t", k=NFF)
        B["HT"] = B["MB"]
        HTOK = MK[:].rearrange("p h a t -> p (h a t)")[:, 0:NFF * 128]
        TARm = [T_(f"TARm{i}", [128, 8, 2, 128], BF16) for i in range(2)]
        TBTm = [T_(f"TBTm{i}", [128, 8, 128], BF16) for i in range(2)]
        SBFm = [T_(f"SBFm{i}", [128, 8, 64], BF16) for i in range(2)]
        for i_ in range(2):
            for (tn, tl) in (("TARm", TARm), ("TBTm", TBTm), ("SBFm", SBFm)):
                mset("vector", tl[i_][:], 0.0, [B[f"{tn}{i_}"]])

        mset("vector", LW1[64:65, :], 1.0, [B["LW1"]])
        mset("vector", LA1[64:65, :], 1.0, [B["LA1"]])
        mset("vector", LV1[32:33, :], 1.0, [B["LV1"]])

        def rmsnorm_to(dst, coff, gcol):
            for s_ in range(NS):
                act(JUNK[:], X[:, s_, :], AF.Square, [B["X"]], [B["JUNK"], B["SS"]], accum_out=SS[:, 0:1])
                ts("vector", MS[:, 0:1], SS[:, 0:1], 1.0 / D, 1e-6, ALU.mult, ALU.add, [B["SS"]], [B["MS"]])
                tt("gpsimd", RSTD[:, 0:1], MS[:, 0:1], NH[:, 0:1], ALU.pow, [B["MS"], B["NH"]], [B["RSTD"]])
                act(XNB[:], X[:, s_, :], AF.Copy, [B["X"], B["RSTD"]], [B["XNB"]], scale=RSTD[:, 0:1])
                for kc in range(8):
                    tr(PT[:, kc * 128:(kc + 1) * 128], XNB[:, kc * 128:(kc + 1) * 128], IDENT[:],
                       [B["XNB"], B["IDENT"]], [B["PT"]], sig=(kc == 7))
                for kc in range(8):
                    ts("vector" if kc % 2 else "gpsimd" if False else "vector",
                       dst[:, kc, coff + s_ * 128:coff + (s_ + 1) * 128], PT[:, kc * 128:(kc + 1) * 128],
                       PV[:, kc, gcol:gcol + 1], None, ALU.mult, None, [B["PT"], B["PV"]], [B[dst_name[id(dst)]]])

        dst_name = {id(XNT): "XNT"}

        def sigm_from_tanh(eng, ap, R, W):
            act(ap, ap, AF.Identity, R, W, scale=0.5, bias=0.5)

        OUTB = {}
        VFB = {}
        try:
            chk(1)
            for l in layers:
                W_, S_ = WN[l], SC[l]
                has_v = (l != 0)
                dma(PV[:], W_["vecT"], (), [B["PV"]], "lp0")
                dma(DW[:], W_["dw"], (), [B["DW"]], "lp1")
                for (tile_, bn, row) in ((GPOST, "GPOST", V_GPOST), (GFPOST, "GFPOST", V_GFPOST), (KKT, "KKT", V_KK),
                                         (KAT, "KAT", V_KA), (RKT, "RKT", V_RK), (GNW, "GNW", V_GNW), (GNB, "GNB", V_GNB)):
                    dma(tile_[:], W_["vec"][row, :].partition_broadcast(128), (), [B[bn]], "lp_" + bn)
                ts("vector", HLN[:], PV[:, :, V_LNW:V_LNW + 2], 0.5, None, ALU.mult, None, [B["PV"]], [B["HLN"]])
                dma(L2BUF[:], S_["l2"].rearrange("p a c -> p (a c)"), (), [B["L2BUF"]], "lp2")
                for seq in range(nseq):
                    mset("vector", S32[:], 0.0, [B["S32"]])
                    mset("vector", SBF[:], 0.0, [B["SBF"]])
                    mset("gpsimd", CB[:, :, 0:30], 0.0, CBo)
                    for ti in range(T // NT):
                        t0 = ti * NT
                        xsrc = (x_in if l == layers[0] else out)[seq, t0:t0 + NT, :].rearrange("(s p) d -> p s d", p=128)
                        ob_ = OUTB.setdefault((seq, ti), Buf())
                        vb_ = VFB.setdefault((seq, ti), Buf())
                        dma(X[:], xsrc, ([] if l == layers[0] else [ob_]), [B["X"]], "dx")
                        if ti == 0:
                            mset("vector", XNT[:, :, 0:1], 0.0, [B["XNT"]])
                        else:
                            cp("vector", XNT[:, :, 0:1], CARRY[:], [B["CARRY"]], [B["XNT"]])
                        rmsnorm_to(XNT, 1, V_GPRE)
                        cp("vector", CARRY[:], XNT[:, :, NT:NT + 1], [B["XNT"]], [B["CARRY"]])
                        chk(2)
                        L1, BL1 = slab(S_["l1"].rearrange("p k s c -> p (k s c)"), 4608)
                        L1v = L1[:, 0:4608].rearrange("p (k s c) -> p k s c", k=8, s=2)
                        for (c0, c1, dstt, dn, fn_) in ((0, 64, LW1, "LW1", AF.Tanh), (64, 128, LA1, "LA1", AF.Copy),
                                                       (128, 256, LG1, "LG1", AF.Tanh), (256, 288, LV1, "LV1", AF.Copy)):
                            if c0 == 256 and not has_v:
                                continue
                            M_ = c1 - c0
                            pb, bpb = bank()
                            for kc in range(8):
                                for s2 in range(2):
                                    mm(pb[0:M_, 0:NT], L1v[:, kc, s2, c0:c1], XNT[:, kc, 1 - s2:1 - s2 + NT],
                                       kc == 0 and s2 == 0, kc == 7 and s2 == 1, [BL1, B["XNT"]], [bpb], sig=(kc == 7 and s2 == 1))
                            if dn == "LG1":
                                act(LG1[:], pb[0:128, 0:NT], AF.Tanh, [bpb], [B["LG1"]], scale=0.5)
                                sigm_from_tanh("gpsimd", LG1[:], [B["LG1"]], [B["LG1"]])
                            else:
                                act(dstt[0:M_, :], pb[0:M_, 0:NT], fn_, [bpb], [B[dn]])
                        BL2 = B["L2BUF"]
                        L2v = L2BUF[:, 0:4096].rearrange("p (a c) -> p a c", a=4)
                        chk(3)
                        for q in range(2):
                            FU, BFU = slab(S_["fm"][:, q * 4:(q + 1) * 4].rearrange("p a k c -> p (a k c)"), 4096)
                            FG, BFG = slab(S_["fm"][:, 8 + q * 4:8 + (q + 1) * 4].rearrange("p a k c -> p (a k c)"), 4096)
                            FUv = FU[:, 0:4096].rearrange("p (a k c) -> p a k c", a=4, k=8)
                            FGv = FG[:, 0:4096].rearrange("p (a k c) -> p a k c", a=4, k=8)
                            for o4 in range(4):
                                oc = q * 4 + o4
                                bu, bbu = bank(); bg, bbg = bank()
                                for kc in range(8):
                                    mm(bu[:, 0:NT], FUv[:, o4, kc, :], XNT[:, kc, 1:1 + NT], kc == 0, kc == 7, [BFU, B["XNT"]], [bbu], sig=(kc == 7))
                                for kc in range(8):
                                    mm(bg[:, 0:NT], FGv[:, o4, kc, :], XNT[:, kc, 1:1 + NT], kc == 0, kc == 7, [BFG, B["XNT"]], [bbg], sig=(kc == 7))
                                tf = TMPF[oc % 2]; btf = B[f"TMPF{oc % 2}"]
                                act(tf[:], bg[:, 0:NT], AF.Tanh, [bbg], [btf], scale=0.5)
                                sigm_from_tanh("gpsimd", tf[:], [btf], [btf])
                                tt("vector", CB[:, oc, 30:30 + NT], bu[:, 0:NT], tf[:], ALU.mult, [bbu, btf], [CBo[oc]])
                        def conv_tail():
                            for oc in range(8):
                                ts("vector", ACC[:, oc, :], CB[:, oc, 0:NT], DW[:, oc, 0:1], PV[:, oc, V_CB:V_CB + 1], ALU.mult, ALU.add,
                                   [CBo[oc], B["DW"], B["PV"]], [ACCo[oc]])
                                yield
                            for j in range(1, 31):
                                for oc in range(8):
                                    stt(ACC[:, oc, :], CB[:, oc, j:j + NT], DW[:, oc, j:j + 1], ACC[:, oc, :], ALU.mult, ALU.add,
                                        [CBo[oc], B["DW"], ACCo[oc]], [ACCo[oc]])
                                    yield
                            cp("gpsimd", CB[:, :, 0:30], CB[:, :, NT:NT + 30], CBo, CBo)
                            yield
                            s1, bs1 = PS[5], B["PS5"]; s2b, bs2 = PS[6], B["PS6"]
                            for oc in range(8):
                                mm(s1[:, 0:NT], ONES32[:], ACC[:, oc, :], oc == 0, oc == 7, [B["ONES32"], ACCo[oc]], [bs1], sig=(oc == 7))
                                yield
                            for oc in range(8):
                                tf = TMPF[oc % 2]; btf = B[f"TMPF{oc % 2}"]
                                act(tf[:], ACC[:, oc, :], AF.Square, [ACCo[oc]], [btf])
                                yield
                                mm(s2b[:, 0:NT], ONES32[:], tf[:], oc == 0, oc == 7, [B["ONES32"], btf], [bs2], sig=True)
                                yield
                            act(MEAN[:], s1[:, 0:NT], AF.Copy, [bs1], [B["MEAN"]], scale=1.0 / D)
                            yield
                            act(VAR[:], s2b[:, 0:NT], AF.Copy, [bs2], [B["VAR"]], scale=1.0 / D)
                            yield
                            tt("gpsimd", RS[:], MEAN[:], MEAN[:], ALU.mult, [B["MEAN"]], [B["RS"]])
                            yield
                            tt("gpsimd", VAR[:], VAR[:], RS[:], ALU.subtract, [B["VAR"], B["RS"]], [B["VAR"]])
                            yield
                            ts("gpsimd", VAR[:], VAR[:], 1e-5, None, ALU.add, None, [B["VAR"]], [B["VAR"]])
                            yield
                            tt("gpsimd", RS[:], VAR[:], NH[:], ALU.pow, [B["VAR"], B["NH"]], [B["RS"]])
                            yield
                            for oc in range(8):
                                t1 = TMPF[0]; t2 = TMPF[1]
                                tt("vector", t1[:], ACC[:, oc, :], MEAN[:], ALU.subtract, [ACCo[oc], B["MEAN"]], [B["TMPF0"]])
                                yield
                                tt("gpsimd", t1[:], t1[:], RS[:], ALU.mult, [B["TMPF0"], B["RS"]], [B["TMPF0"]])
                                yield
                                act(t2[:], t1[:], AF.Tanh, [B["TMPF0"], B["HLN"]], [B["TMPF1"]], scale=HLN[:, oc, 0:1], bias=HLN[:, oc, 1:2])
                                yield
                                ts("vector", t1[:], t1[:], PV[:, oc, V_LNW:V_LNW + 1], PV[:, oc, V_LNB:V_LNB + 1], ALU.mult, ALU.add,
                                   [B["TMPF0"], B["PV"]], [B["TMPF0"]])
                                yield
                                sigm_from_tanh("gpsimd", t2[:], [B["TMPF1"]], [B["TMPF1"]])
                                yield
                                tt("vector", CCT[:, oc, :], t1[:], t2[:], ALU.mult, [B["TMPF0"], B["TMPF1"]], [B["CCT"]])
                                yield


                        P.hook = conv_tail(); P.hook_n = 0
                        chk(4)
                        r32, k32, v32, asg, lw, t1, t2 = A
                        bR, bK, bV, bAS, bLW, bT1, bT2 = [B[f"A{i}"] for i in range(7)]
                        for j in range(12):
                            TK, BTK = slab(S_["tok"][:, j].rearrange("p k c -> p (k c)"), 4096)
                            TKv = TK[:, 0:4096].rearrange("p (k c) -> p k c", k=16)
                            pb, bpb = bank()
                            for kc in range(8):
                                for s2 in range(2):
                                    mm(pb[:, 0:256], XNT[:, kc, 1 - s2:1 - s2 + NT], TKv[:, kc * 2 + s2, :],
                                       kc == 0 and s2 == 0, kc == 7 and s2 == 1, [BTK, B["XNT"]], [bpb], sig=(kc == 7 and s2 == 1))
                            dstA = A[j // 4]
                            act(dstA[:, (j % 4) * 256:(j % 4 + 1) * 256], pb[:, 0:256], AF.Copy, [bpb], [B[f"A{j // 4}"]])
                        def lora2(src, K_, a_idx):
                            res = []
                            for hf in range(2):
                                pb, bpb = bank()
                                mm(pb[:, 0:512], src[0:K_, :], L2v[0:K_, a_idx, hf * 512:(hf + 1) * 512], True, True,
                                   [B["LW1"], B["LA1"], B["LV1"], B["LG1"], BL2], [bpb], sig=True)
                                res.append((pb, bpb))
                            return res
                        if l == 0:
                            dma(vf_d[seq, t0:t0 + NT, :], v32[:], [bV], [vb_], "vfo")
                        else:
                            VF = t2
                            dma(VF[:], vf_d[seq, t0:t0 + NT, :], [vb_], [bT2], "vfi")
                            rr = lora2(LV1, 33, 2)
                            for hf, (pb, bpb) in enumerate(rr):
                                act(t1[:, hf * 512:(hf + 1) * 512], pb[:, 0:512], AF.Tanh, [bpb], [bT1], scale=0.5)
                            sigm_from_tanh("gpsimd", t1[:], [bT1], [bT1])
                            tt("gpsimd", VF[:], VF[:], v32[:], ALU.subtract, [bT2, bV], [bT2])
                            tt("gpsimd", VF[:], VF[:], t1[:], ALU.mult, [bT2, bT1], [bT2])
                            tt("gpsimd", v32[:], v32[:], VF[:], ALU.add, [bV, bT2], [bV])
                        rr = lora2(LA1, 65, 1)
                        for hf, (pb, bpb) in enumerate(rr):
                            act(asg[:, hf * 512:(hf + 1) * 512], pb[:, 0:512], AF.Tanh, [bpb], [bAS], scale=0.5)
                        sigm_from_tanh("gpsimd", asg[:], [bAS], [bAS])
                        rr = lora2(LW1, 65, 0)
                        for hf, (pb, bpb) in enumerate(rr):
                            act(lw[:, hf * 512:(hf + 1) * 512], pb[:, 0:512], AF.Tanh, [bpb], [bLW], scale=0.5)
                        ts("gpsimd", lw[:], lw[:], -0.30326533, -0.30326533, ALU.mult, ALU.add, [bLW], [bLW])
                        chk(5)
                        for (ti_, dsts) in ((0, ((EB[0], "EB0", 1.0), (EB[1], "EB1", -1.0))), (1, ((EB[2], "EB2", 1.0),)), (2, ((EB[3], "EB3", 1.0),))):
                            for hf in range(2):
                                pb, bpb = bank()
                                mm(pb[:, 0:512], TRI[:, ti_, :], lw[:, hf * 512:(hf + 1) * 512], True, True, [B["TRI"], bLW], [bpb], sig=True)
                                for (dd, dn, sc_) in dsts:
                                    act(dd[:, hf * 512:(hf + 1) * 512], pb[:, 0:512], AF.Exp, [bpb], [B[dn]], scale=sc_)
                        pb, bpb = bank()
                        for h in range(H):
                            po = (h % 2) * 64
                            mm(pb[po:po + 64, h // 2:h // 2 + 1], lw[:, h * 64:(h + 1) * 64], ONES32[:, 0:1], True, True,
                               [bLW, B["ONES32"]], [bpb], sig=(h == H - 1))
                        act(PC[:], pb[:, 0:8], AF.Exp, [bpb], [B["PC"]])
                        tt("gpsimd", t1[:], k32[:], KKT[:], ALU.mult, [bK, B["KKT"]], [bT1])
                        tt("gpsimd", t2[:], t1[:], t1[:], ALU.mult, [bT1], [bT2])
                        P.op("vector", lambda e: e.tensor_reduce(out=SM[:, :, 0], in_=t2[:].rearrange("p (h c) -> p h c", h=H), axis=AX.X, op=ALU.add), [bT2], [B["SM"]])
                        ts("vector", SM[:, :, 0], SM[:, :, 0], 1e-24, None, ALU.max, None, [B["SM"]], [B["SM"]])
                        tt("gpsimd", SM[:, :, 1], SM[:, :, 0], NH[:, 0:H], ALU.pow, [B["SM"], B["NH"]], [B["SM"]])
                        for h in range(H):
                            ts("vector", t1[:, h * 64:(h + 1) * 64], t1[:, h * 64:(h + 1) * 64], SM[:, h, 1:2], None, ALU.mult, None, [bT1, B["SM"]], [bT1])
                        kk = t1
                        stt(t2[:], asg[:], -1.0, KAT[:], ALU.add, ALU.mult, [bAS, B["KAT"]], [bT2])
                        stt(k32[:], t2[:], 1.0, k32[:], ALU.add, ALU.mult, [bT2, bK], [bK])
                        tt("gpsimd", t2[:], r32[:], k32[:], ALU.mult, [bR, bK], [bT2])
                        tt("gpsimd", t2[:], t2[:], RKT[:], ALU.mult, [bT2, B["RKT"]], [bT2])
                        P.op("vector", lambda e: e.tensor_reduce(out=SM[:, :, 2], in_=t2[:].rearrange("p (h c) -> p h c", h=H), axis=AX.X, op=ALU.add), [bT2], [B["SM"]])
                        tt("gpsimd", asg[:], asg[:], kk[:], ALU.mult, [bAS, bT1], [bAS])
                        bvec = asg
                        tt("vector", KH[:], k32[:], EB[3][:], ALU.mult, [bK, B["EB3"]], [B["KH"]])
                        tt("gpsimd", BH[:], bvec[:], EB[3][:], ALU.mult, [bAS, B["EB3"]], [B["BH"]])
                        cp("gpsimd", VBF[:], v32[:], [bV], [B["VBF"]])
                        def trans_to(srcf, bsrc, eb, ebn, neg, dst_fn, dname, oi):
                            ob = OB[oi]; bob = B[f"OB{oi}"]
                            if neg:
                                stt(ob[:], srcf[:], -1.0, eb[:], ALU.mult, ALU.mult, [bsrc, B[ebn]], [bob])
                            else:
                                tt("vector", ob[:], srcf[:], eb[:], ALU.mult, [bsrc, B[ebn]], [bob])
                            for kc in range(8):
                                tr(PT[:, kc * 128:(kc + 1) * 128], ob[:, kc * 128:(kc + 1) * 128], IDENT[:], [bob, B["IDENT"]], [B["PT"]], sig=(kc == 7))
                            cp("vector", dst_fn, PT[:].rearrange("p (k t) -> p k t", k=8), [B["PT"]], [B[dname]])
                        trans_to(r32, bR, EB[0], "EB0", False, TAR[:, :, 1, :], "TAR", 0)
                        trans_to(kk, bT1, EB[2], "EB2", True, TAR[:, :, 0, :], "TAR", 1)
                        trans_to(bvec, bAS, EB[1], "EB1", False, TBT[:], "TBT", 0)
                        trans_to(k32, bK, EB[1], "EB1", False, TKT[:], "TKT", 1)
                        for i_ in range(2):
                            ps_ = slice(i_ * 64, (i_ + 1) * 64)
                            cp("vector", TARm[i_][ps_], TAR[ps_], [B["TAR"]], [B[f"TARm{i_}"]])
                            cp("gpsimd", TBTm[i_][ps_], TBT[ps_], [B["TBT"]], [B[f"TBTm{i_}"]])
                            cp("gpsimd", SBFm[i_][ps_], SBF[ps_], [B["SBF"]], [B[f"SBFm{i_}"]])
                        chk(6)
                        for pr_ in range(2):
                            st_ = {}
                            for g4 in (2 * pr_, 2 * pr_ + 1):
                                gi = g4 % 2
                                pl, bpl = bank()
                                for hh in range(4):
                                    h = g4 * 4 + hh; kc = h // 2
                                    mm(pl[:, hh * 128:(hh + 1) * 128], TAR[:, kc, 0, :], TBTm[h % 2][:, kc, :], True, True,
                                       [B["TAR"], B[f"TBTm{h % 2}"]], [bpl], sig=(hh == 3))
                                tt("vector", R0[gi][:], pl[:, 0:512], MSKL[:], ALU.mult, [bpl, B["MSKL"]], [B[f"R0_{gi}"]])
                                for (lt, ltn, dstm, dmn) in ((TBT, "TBT", MB, "MB"), (TKT, "TKT", MK, "MK")):
                                    for h2 in range(2):
                                        pb, bpb = bank()
                                        for hh in range(2):
                                            h = g4 * 4 + h2 * 2 + hh; kc = h // 2
                                            mm(pb[:, hh * 256:(hh + 1) * 256], lt[:, kc, :], TARm[h % 2][:, kc, :, :].rearrange("p a t -> p (a t)"),
                                               True, True, [B[ltn], B[f"TARm{h % 2}"]], [bpb], sig=(hh == 1))
                                        h0 = g4 * 4 + h2 * 2
                                        tt("vector", dstm[:, h0:h0 + 2, :, :].rearrange("p h a t -> p (h a t)"), pb[:, 0:512], MSKT[:], ALU.mult,
                                           [bpb, B["MSKT"]], [B[dmn]])
                                cur = QRG[gi][0]; nxt = QRG[gi][1]; bcur = B[f"QRG{gi}_0"]; bnxt = B[f"QRG{gi}_1"]
                                cp("vector", cur[:, 0, :].rearrange("p (h t) -> p h t", h=4), MB[:, g4 * 4:g4 * 4 + 4, 0, :], [B["MB"]], [bcur])
                                cp("gpsimd", cur[:, 1, :], R0[gi][:], [B[f"R0_{gi}"]], [bcur])
                                tt("gpsimd", cur[:, 2, :], cur[:, 0, :], ID4[:], ALU.add, [bcur, B["ID4"]], [bcur])
                                st_[g4] = [cur, nxt, bcur, bnxt]
                            for jj in range(1, 7):
                                pbk = {}
                                for g4 in (2 * pr_, 2 * pr_ + 1):
                                    cur, nxt, bcur, bnxt = st_[g4]
                                    pq, bpq = bank() if jj < 6 else (None, None)
                                    pr, bpr = bank()
                                    for hh in range(4):
                                        cs_ = slice(hh * 128, (hh + 1) * 128)
                                        if jj < 6:
                                            mm(pq[:, cs_], cur[:, 1, cs_], cur[:, 0, cs_], True, True, [bcur], [bpq], sig=(hh == 3))
                                    for hh in range(4):
                                        cs_ = slice(hh * 128, (hh + 1) * 128)
                                        mm(pr[:, cs_], cur[:, 0, cs_], cur[:, 1, cs_], True, True, [bcur], [bpr], sig=(hh == 3))
                                    pbk[g4] = (pq, bpq, pr, bpr)
                                for g4 in (2 * pr_, 2 * pr_ + 1):
                                    cur, nxt, bcur, bnxt = st_[g4]
                                    pq, bpq, pr, bpr = pbk[g4]
                                    if jj < 6:
                                        act(nxt[:, 0, :], pq[:, 0:512], AF.Copy, [bpq], [bnxt])
                                    cp("vector", nxt[:, 1, :], pr[:, 0:512], [bpr], [bnxt])
                                pgk = {}
                                for g4 in (2 * pr_, 2 * pr_ + 1):
                                    cur, nxt, bcur, bnxt = st_[g4]
                                    pg, bpg = bank()
                                    for hh in range(4):
                                        cs_ = slice(hh * 128, (hh + 1) * 128)
                                        mm(pg[:, cs_], nxt[:, 1, cs_], cur[:, 2, cs_], True, True, [bnxt, bcur], [bpg], sig=(hh == 3))
                                    pgk[g4] = (pg, bpg)
                                for g4 in (2 * pr_, 2 * pr_ + 1):
                                    cur, nxt, bcur, bnxt = st_[g4]
                                    pg, bpg = pgk[g4]
                                    if jj < 6:
                                        tt("vector", nxt[:, 2, :], pg[:, 0:512], cur[:, 2, :], ALU.add, [bpg, bcur], [bnxt])
                                    else:
                                        tt("vector", G7[:, g4 * 4:g4 * 4 + 4, :].rearrange("p h t -> p (h t)"), pg[:, 0:512], cur[:, 2, :], ALU.add,
                                           [bpg, bcur], [B["G7"]])
                                    st_[g4] = [nxt, cur, bnxt, bcur]
                        chk(7)
                        for h8 in range(2):
                            px, bpx = bank()
                            for hh in range(8):
                                h = h8 * 8 + hh; kc = h // 2; po = (h % 2) * 64
                                mm(px[:, hh * 64:(hh + 1) * 64], TAR[:, kc, 0, :], SBFm[h % 2][:, kc, :], True, False, [B["TAR"], B[f"SBFm{h % 2}"]], [bpx], sig=False)
                                mm(px[:, hh * 64:(hh + 1) * 64], MK[:, h, 0, :], VBF[:, h * 64:(h + 1) * 64], False, True, [B["MK"], B["VBF"]], [bpx], sig=(hh == 7))
                            cp("vector", XB[:, h8 * 8:(h8 + 1) * 8, :].rearrange("p h c -> p (h c)"), px[:, 0:512], [bpx], [B["XB"]])
                        for h8 in range(2):
                            pu, bpu = bank()
                            for hh in range(8):
                                h = h8 * 8 + hh
                                mm(pu[:, hh * 64:(hh + 1) * 64], G7[:, h, :], XB[:, h, :], True, True, [B["G7"], B["XB"]], [bpu], sig=(hh == 7))
                            cp("vector", UB[:, h8 * 8:(h8 + 1) * 8, :].rearrange("p h c -> p (h c)"), pu[:, 0:512], [bpu], [B["UB"]])
                        ybanks = []
                        for h8 in range(2):
                            py, bpy = bank()
                            for hh in range(8):
                                h = h8 * 8 + hh; kc = h // 2; po = (h % 2) * 64
                                o_ = py[:, hh * 64:(hh + 1) * 64]
                                mm(o_, TAR[:, kc, 1, :], SBFm[h % 2][:, kc, :], True, False, [B["TAR"], B[f"SBFm{h % 2}"]], [bpy], sig=False)
                                mm(o_, MB[:, h, 1, :], UB[:, h, :], False, False, [B["MB"], B["UB"]], [bpy], sig=False)
                                mm(o_, MK[:, h, 1, :], VBF[:, h * 64:(h + 1) * 64], False, True, [B["MK"], B["VBF"]], [bpy], sig=(hh == 7))
                            ybanks.append((py, bpy))
                        pss, bpss = bank()
                        for h in range(H):
                            kc = h // 2; po = (h % 2) * 64
                            o_ = pss[po:po + 64, kc * 64:(kc + 1) * 64]
                            mm(o_, BH[:, h * 64:(h + 1) * 64], UB[:, h, :], True, False, [B["BH"], B["UB"]], [bpss], sig=False)
                            mm(o_, KH[:, h * 64:(h + 1) * 64], VBF[:, h * 64:(h + 1) * 64], False, True, [B["KH"], B["VBF"]], [bpss], sig=(h == H - 1))
                        for kc in range(8):
                            stt(S32[:, kc, :], S32[:, kc, :], PC[:, kc:kc + 1], pss[:, kc * 64:(kc + 1) * 64], ALU.mult, ALU.add,
                                [B["S32"], B["PC"], bpss], [B["S32"]])
                        y32 = r32
                        for h8, (py, bpy) in enumerate(ybanks):
                            act(y32[:, h8 * 512:(h8 + 1) * 512], py[:, 0:512], AF.Copy, [bpy], [bR])
                        cp("gpsimd", SBF[:], S32[:], [B["S32"]], [B["SBF"]])
                        P.op("vector", lambda e: e.tensor_reduce(out=SM[:, :, 0], in_=y32[:].rearrange("p (h c) -> p h c", h=H), axis=AX.X, op=ALU.add), [bR], [B["SM"]])
                        tt("gpsimd", t2[:], y32[:], y32[:], ALU.mult, [bR], [bT2])
                        P.op("vector", lambda e: e.tensor_reduce(out=SM[:, :, 1], in_=t2[:].rearrange("p (h c) -> p h c", h=H), axis=AX.X, op=ALU.add), [bT2], [B["SM"]])
                        ts("vector", SM[:, :, 0], SM[:, :, 0], 1.0 / 64, None, ALU.mult, None, [B["SM"]], [B["SM"]])
                        tt("vector", SM[:, :, 3], SM[:, :, 0], SM[:, :, 0], ALU.mult, [B["SM"]], [B["SM"]])
                        stt(SM[:, :, 1], SM[:, :, 1], 1.0 / 64, SM[:, :, 3], ALU.mult, ALU.subtract, [B["SM"]], [B["SM"]])
                        ts("vector", SM[:, :, 1], SM[:, :, 1], 64e-5, None, ALU.add, None, [B["SM"]], [B["SM"]])
                        tt("gpsimd", SM[:, :, 3], SM[:, :, 1], NH[:, 0:H], ALU.pow, [B["SM"], B["NH"]], [B["SM"]])
                        for h in range(H):
                            hs = slice(h * 64, (h + 1) * 64)
                            ts("vector", y32[:, hs], y32[:, hs], SM[:, h, 0:1], SM[:, h, 3:4], ALU.subtract, ALU.mult, [bR, B["SM"]], [bR])
                        tt("gpsimd", y32[:], y32[:], GNW[:], ALU.mult, [bR, B["GNW"]], [bR])
                        tt("gpsimd", y32[:], y32[:], GNB[:], ALU.add, [bR, B["GNB"]], [bR])
                        for h in range(H):
                            hs = slice(h * 64, (h + 1) * 64)
                            stt(y32[:, hs], v32[:, hs], SM[:, h, 2:3], y32[:, hs], ALU.mult, ALU.add, [bV, B["SM"], bR], [bR])
                        rr = lora2(LG1, 128, 3)
                        for hf, (pb, bpb) in enumerate(rr):
                            tt("vector", OB[0][:, hf * 512:(hf + 1) * 512], pb[:, 0:512], y32[:, hf * 512:(hf + 1) * 512], ALU.mult, [bpb, bR], [B["OB0"]])
                        for kc in range(8):
                            tr(PT[:, kc * 128:(kc + 1) * 128], OB[0][:, kc * 128:(kc + 1) * 128], IDENT[:], [B["OB0"], B["IDENT"]], [B["PT"]], sig=(kc == 7))
                        cp("vector", YT[:], PT[:].rearrange("p (k t) -> p k t", k=8), [B["PT"]], [B["YT"]])

                        if P.hook is not None:
                            P.in_hook = True
                            for _ in P.hook:
                                pass
                            P.in_hook = False
                            P.hook = None
                        chk(8)
                        def fm_proj(slab_src_fn, rhs_t, brhs, oc):
                            pass
                        for q in range(2):
                            RO_, BRO = slab(S_["ro"][:, q * 4:(q + 1) * 4].rearrange("p a k c -> p (a k c)"), 4096)
                            ZR_, BZR = slab(S_["fm"][:, 16 + q * 4:16 + (q + 1) * 4].rearrange("p a k c -> p (a k c)"), 4096)
                            for o4 in range(4):
                                oc = q * 4 + o4
                                py_, bpy_ = bank(); pz_, bpz_ = bank()
                                ROv = RO_[:, 0:4096].rearrange("p (a k c) -> p a k c", a=4, k=8)
                                ZRv = ZR_[:, 0:4096].rearrange("p (a k c) -> p a k c", a=4, k=8)
                                for kc in range(8):
                                    mm(py_[:, 0:NT], ROv[:, o4, kc, :], YT[:, kc, :], kc == 0, kc == 7, [BRO, B["YT"]], [bpy_], sig=(kc == 7))
                                for kc in range(8):
                                    mm(pz_[:, 0:NT], ZRv[:, o4, kc, :], XNT[:, kc, 1:1 + NT], kc == 0, kc == 7, [BZR, B["XNT"]], [bpz_], sig=(kc == 7))
                                tf = TMPF[0]
                                act(tf[:], pz_[:, 0:NT], AF.Tanh, [bpz_], [B["TMPF0"]], scale=0.5)
                                sigm_from_tanh("gpsimd", tf[:], [B["TMPF0"]], [B["TMPF0"]])
                                tt("vector", ACC[:, oc, :], py_[:, 0:NT], tf[:], ALU.mult, [bpy_, B["TMPF0"]], [ACCo[oc]])
                        for q in range(2):
                            CO_, BCO = slab(S_["co"][:, q * 4:(q + 1) * 4].rearrange("p a k c -> p (a k c)"), 4096)
                            ZC_, BZC = slab(S_["fm"][:, 24 + q * 4:24 + (q + 1) * 4].rearrange("p a k c -> p (a k c)"), 4096)
                            for o4 in range(4):
                                oc = q * 4 + o4
                                py_, bpy_ = bank(); pz_, bpz_ = bank()
                                COv = CO_[:, 0:4096].rearrange("p (a k c) -> p a k c", a=4, k=8)
                                ZCv = ZC_[:, 0:4096].rearrange("p (a k c) -> p a k c", a=4, k=8)
                                for kc in range(8):
                                    mm(py_[:, 0:NT], COv[:, o4, kc, :], CCT[:, kc, :], kc == 0, kc == 7, [BCO, B["CCT"]], [bpy_], sig=(kc == 7))
                                for kc in range(8):
                                    mm(pz_[:, 0:NT], ZCv[:, o4, kc, :], XNT[:, kc, 1:1 + NT], kc == 0, kc == 7, [BZC, B["XNT"]], [bpz_], sig=(kc == 7))
                                tf = TMPF[1]
                                act(tf[:], pz_[:, 0:NT], AF.Tanh, [bpz_], [B["TMPF1"]], scale=0.5)
                                sigm_from_tanh("gpsimd", tf[:], [B["TMPF1"]], [B["TMPF1"]])
                                tt("vector", tf[:], py_[:, 0:NT], tf[:], ALU.mult, [bpy_, B["TMPF1"]], [B["TMPF1"]])
                                tt("gpsimd", MRG[:, oc, :], tf[:], ACC[:, oc, :], ALU.add, [B["TMPF1"], ACCo[oc]], [B["MRG"]])

                        def out_norm_resid(pbs, gtile, gname):
                            for hf, (pb, bpb) in enumerate(pbs):
                                act(JUNK[:, 0:512], pb[:, 0:512], AF.Square, [bpb], [B["JUNK"], B["SS"]], accum_out=SS[:, hf:hf + 1])
                            tt("vector", MS[:, 0:1], SS[:, 0:1], SS[:, 1:2], ALU.add, [B["SS"]], [B["MS"]])
                            ts("vector", MS[:, 0:1], MS[:, 0:1], 1.0 / D, 1e-6, ALU.mult, ALU.add, [B["MS"]], [B["MS"]])
                            tt("gpsimd", RSTD[:, 0:1], MS[:, 0:1], NH[:, 0:1], ALU.pow, [B["MS"], B["NH"]], [B["RSTD"]])
                            for hf, (pb, bpb) in enumerate(pbs):
                                hs = slice(hf * 512, (hf + 1) * 512)
                                stt(A[5][:, hs], pb[:, 0:512], RSTD[:, 0:1], gtile[:, hs], ALU.mult, ALU.mult, [bpb, B["RSTD"], B[gname]], [B["A5"]])
                            tt("gpsimd", X[:, 0, :], X[:, 0, :], A[5][:], ALU.add, [B["X"], B["A5"]], [B["X"]])

                        WO_, BWO = [], []
                        pbs = []
                        for hf in range(2):
                            w_, bw_ = slab(S_["wo"][:, :, hf * 512:(hf + 1) * 512], 4096)
                            wv = w_[:, 0:4096].rearrange("p (k c) -> p k c", k=8)
                            pb, bpb = bank()
                            for kc in range(8):
                                mm(pb[:, 0:512], MRG[:, kc, :], wv[:, kc, :], kc == 0, kc == 7, [B["MRG"], bw_], [bpb], sig=(kc == 7))
                            pbs.append((pb, bpb))
                        out_norm_resid(pbs, GPOST, "GPOST")
                        if ti == 0 and seq == 0 and l == layers[0]:
                            dump("d_x1", X[:, 0, :], [128, D], F32, [B["X"]])
                            dump("d_yt", YT[:], [128, 8, NT], BF16, [B["YT"]])
                            dump("d_cct", CCT[:], [128, 8, NT], BF16, [B["CCT"]])
                            dump("d_mrg", MRG[:], [128, 8, NT], BF16, [B["MRG"]])
                            dump("d_xnt", XNT[:], [128, 8, NT + 1], BF16, [B["XNT"]])
                            dump("d_v", A[2][:], [128, D], F32, [B["A2"]])
                            dump("d_k", A[1][:], [128, D], F32, [B["A1"]])
                            dump("d_y", A[0][:], [128, D], F32, [B["A0"]])
                            dump("d_b", A[3][:], [128, D], F32, [B["A3"]])
                            dump("d_lw", A[4][:], [128, D], F32, [B["A4"]])
                        rmsnorm_to(XNT, 1, V_GFPRE)
                        for cb in range(6):
                            c0 = cb * 512; cw = 512 if cb < 5 else 256
                            G_, BG_ = slab(S_["g"][:, :, c0:c0 + cw], 8 * cw)
                            U_, BU_ = slab(S_["u"][:, :, c0:c0 + cw], 8 * cw)
                            Gv = G_[:, 0:8 * cw].rearrange("p (k c) -> p k c", k=8)
                            Uv = U_[:, 0:8 * cw].rearrange("p (k c) -> p k c", k=8)
                            pg_, bpg_ = bank(); pu_, bpu_ = bank()
                            for kc in range(8):
                                mm(pg_[:, 0:cw], XNT[:, kc, 1:1 + NT], Gv[:, kc, :], kc == 0, kc == 7, [BG_, B["XNT"]], [bpg_], sig=(kc == 7))
                            for kc in range(8):
                                mm(pu_[:, 0:cw], XNT[:, kc, 1:1 + NT], Uv[:, kc, :], kc == 0, kc == 7, [BU_, B["XNT"]], [bpu_], sig=(kc == 7))
                            ft = A[6][:, (cb % 2) * 512:(cb % 2) * 512 + cw]
                            act(ft, pg_[:, 0:cw], AF.Tanh, [bpg_], [B["A6"]], scale=0.5)
                            act(ft, ft, AF.Identity, [B["A6"]], [B["A6"]], scale=0.5, bias=0.5)
                            tt("vector", ft, pg_[:, 0:cw], ft, ALU.mult, [bpg_, B["A6"]], [B["A6"]])
                            tt("vector", HTOK[:, c0:c0 + cw], pu_[:, 0:cw], ft, ALU.mult, [bpu_, B["A6"]], [B["MK"]])
                        for g8 in range(3):
                            n8 = 8 if g8 < 2 else 6
                            for k8 in range(n8):
                                kc = g8 * 8 + k8
                                tr(PT[:, k8 * 128:(k8 + 1) * 128], HTOK[:, kc * 128:(kc + 1) * 128], IDENT[:], [B["MK"], B["IDENT"]], [B["PT"]], sig=(k8 == n8 - 1))
                            cp("vector" if g8 != 1 else "scalar_", HT[:, g8 * 8:g8 * 8 + n8, :], PT[:, 0:n8 * 128].rearrange("p (k t) -> p k t", k=n8), [B["PT"]], [B["HT"]]) if g8 != 1 else act(HT[:, g8 * 8:g8 * 8 + n8, :], PT[:, 0:n8 * 128].rearrange("p (k t) -> p k t", k=n8), AF.Copy, [B["PT"]], [B["HT"]])
                        pbs = []
                        for hf in range(2):
                            pb, bpb = bank()
                            for q in range(3):
                                n8 = 8 if q < 2 else 6
                                w_, bw_ = slab(S_["d"][:, q * 8:q * 8 + n8, hf * 512:(hf + 1) * 512], n8 * 512)
                                wv = w_[:, 0:n8 * 512].rearrange("p (k c) -> p k c", k=n8)
                                for k8 in range(n8):
                                    kc = q * 8 + k8
                                    mm(pb[:, 0:512], HT[:, kc, :], wv[:, k8, :], kc == 0, kc == NFF - 1, [B["HT"], bw_], [bpb], sig=(k8 == n8 - 1))
                            pbs.append((pb, bpb))
                        out_norm_resid(pbs, GFPOST, "GFPOST")
                        dma(out[seq, t0:t0 + NT, :], X[:, 0, :], [B["X"]], [ob_], "dxo")
        except StopBuild:
            pass
        sems = {}
        for k in list(Plan.ENGS) + list(P.dma_cnt.keys()):
            sems[k] = st.enter_context(nc.semaphore("zq_" + k + "_sm"))
        P.emit(nc, sems, {"sync": ([("dxo", P.dma_cnt["dxo"])] if "dxo" in P.dma_cnt else []) + ([("vfo", P.dma_cnt["vfo"])] if "vfo" in P.dma_cnt else []) + [(d_, 16) for d_ in dumps]})
    return nc, P


def _fm_layout(W):
    n = W.shape[1] // 128
    return np.ascontiguousarray(W.reshape(8, 128, n, 128).transpose(1, 2, 0, 3))


def _tok_layout(W):
    K = W.shape[0] // 128
    return np.ascontiguousarray(W.reshape(K, 128, W.shape[1]).transpose(1, 0, 2))


def host_consts():
    s = np.arange(128)[:, None]
    t = np.arange(128)[None, :]
    c = {}
    c["ident"] = np.eye(128).astype(ml_dtypes.bfloat16)
    c["tri3"] = np.ascontiguousarray(np.stack([(s <= t), (s < t), (s > t)], axis=1).astype(np.float32))
    c["mskL"] = np.ascontiguousarray(np.tile((s > t).astype(np.float32), (1, 4)))
    mT = np.concatenate([(t > s), (t >= s)], axis=1).astype(np.float32)
    c["mskT"] = np.ascontiguousarray(np.tile(mT, (1, 2)))
    c["ident4"] = np.ascontiguousarray(np.tile(np.eye(128, dtype=np.float32), (1, 4)))
    return c


def host_layer(inp, l):
    f = lambda a: np.asarray(a, dtype=np.float32)
    w_in = f(inp["w_in"][l])
    d = {}
    d[f"wtok{l}"] = _tok_layout(w_in[:, 0:3072])
    d[f"wfm{l}"] = _fm_layout(w_in[:, 3072:7168])
    v1 = f(inp["vres_1"][l - 1]) if l > 0 else np.zeros((D, 32), np.float32)
    d[f"wl1{l}"] = _tok_layout(np.concatenate([f(inp["decay_w1"][l]), f(inp["a_1"][l]), f(inp["g_1"][l]), v1], axis=1))
    l2 = np.zeros((128, 4, D), np.float32)
    l2[0:64, 0] = f(inp["decay_w2"][l]); l2[64, 0] = f(inp["decay_w0"][l])
    l2[0:64, 1] = f(inp["a_2"][l]); l2[64, 1] = f(inp["a_0"][l])
    if l > 0:
        l2[0:32, 2] = f(inp["vres_2"][l - 1]); l2[32, 2] = f(inp["vres_0"][l - 1])
    l2[0:128, 3] = f(inp["g_2"][l])
    d[f"wl2{l}"] = l2
    d[f"wro{l}"] = _fm_layout(f(inp["w_rwkv_out"][l]))
    d[f"wco{l}"] = _fm_layout(f(inp["w_conv_out"][l]))
    d[f"wo{l}"] = _tok_layout(f(inp["w_out"][l]))
    d[f"wg{l}"] = _tok_layout(f(inp["ffn_w_gate"][l]))
    d[f"wu{l}"] = _tok_layout(f(inp["ffn_w_up"][l]))
    d[f"wd{l}"] = _tok_layout(f(inp["ffn_w_down"][l]))
    vres_mu = f(inp["vres_mu"][l - 1]) if l > 0 else np.zeros(D, np.float32)
    rows = [inp["pre_mix_norm"][l], inp["post_mix_norm"][l], inp["pre_ffn_norm"][l], inp["post_ffn_norm"][l],
            inp["mu_rkv"][l][0], inp["mu_rkv"][l][1], inp["mu_rkv"][l][2],
            inp["mu_wag"][l][0], inp["mu_wag"][l][1], inp["mu_wag"][l][2], vres_mu,
            inp["k_k"][l], inp["k_a"][l], np.asarray(inp["r_k"][l]).reshape(-1), inp["gn_w"][l], inp["gn_b"][l],
            inp["conv_b"][l], inp["conv_ln_w"][l], inp["conv_ln_b"][l]]
    d[f"vec{l}"] = np.ascontiguousarray(np.stack([f(r) for r in rows], axis=0))
    d[f"vecT{l}"] = np.ascontiguousarray(d[f"vec{l}"].reshape(NVEC, 8, 128).transpose(2, 1, 0))
    d[f"dwT{l}"] = np.ascontiguousarray(f(inp["conv_dw"][l]).reshape(31, 8, 128).transpose(2, 1, 0))
    return d


_PROG = {}


def kernel(**inputs):
    x = np.asarray(inputs["x"], dtype=np.float32)
    Bt, T, _ = x.shape
    nseq = Bt // 8
    key = (T, nseq)
    if key not in _PROG:
        _PROG[key] = build(T, nseq, [0, 1, 2, 3])[0]
    nc = _PROG[key]
    base = host_consts()
    for l in range(4):
        base.update(host_layer(inputs, l))
    in_maps = []
    for c in range(8):
        m = {"x": np.ascontiguousarray(x[c * nseq:(c + 1) * nseq])}
        m.update(base)
        in_maps.append(m)
    res = run_bass_kernel_spmd(nc, in_maps, core_ids=list(range(8)))
    return np.concatenate([res.results[c]["out"] for c in range(8)], axis=0).astype(np.float32)
```

```python
import numpy as np
import ml_dtypes
from contextlib import ExitStack
import concourse.bass as bass
import concourse.mybir as mybir
from concourse.bass_utils import run_bass_kernel_spmd

F32 = mybir.dt.float32
BF16 = mybir.dt.bfloat16
AF = mybir.ActivationFunctionType
ALU = mybir.AluOpType
AX = mybir.AxisListType

D = 1024
H = 16
NFF = 22
NT = 128
NS = NT // 128
NVEC = 19
(V_GPRE, V_GPOST, V_GFPRE, V_GFPOST, V_MUR, V_MUK, V_MUV, V_MUW, V_MUA, V_MUG, V_MUVR, V_KK, V_KA, V_RK,
 V_GNW, V_GNB, V_CB, V_LNW, V_LNB) = range(NVEC)


class Buf:
    __slots__ = ("name", "w", "r")

    def __init__(self, name=""):
        self.name = name
        self.w = None
        self.r = []


class Plan:
    ENGS = ("tensor", "vector", "scalar", "gpsimd", "sync")

    def __init__(self):
        self.streams = {e: [] for e in self.ENGS}
        self.cnt = {e: 0 for e in self.ENGS}
        self.waited = {e: {} for e in self.ENGS}
        self.dma_cnt = {}
        self.hook = None
        self.in_hook = False
        self.hook_n = 0

    def _need(self, eng, ev, waits):
        if ev is None:
            return
        k, v = ev
        if k == eng and v > self.cnt[eng]:
            return
        if self.waited[eng].get(k, 0) >= v:
            return
        if waits.get(k, 0) < v:
            waits[k] = v

    def op(self, eng, fn, reads=(), writes=(), sig=True, dma_sem=None, noself=False):
        waits = {}
        for b in reads:
            self._need(eng, b.w, waits)
        for b in writes:
            self._need(eng, b.w, waits)
            for ev in b.r:
                self._need(eng, ev, waits)
        wl = []
        for k, v in waits.items():
            if noself and k == eng:
                continue
            self.waited[eng][k] = v
            wl.append((k, v))
        if dma_sem is not None:
            self.dma_cnt[dma_sem] = self.dma_cnt.get(dma_sem, 0) + 16
            ev = (dma_sem, self.dma_cnt[dma_sem])
            self.streams[eng].append((wl, fn, dma_sem, 16))
        elif sig:
            self.cnt[eng] += 1
            ev = (eng, self.cnt[eng])
            self.streams[eng].append((wl, fn, eng, 1))
        else:
            self.streams[eng].append((wl, fn, None, 0))
            ev = (eng, self.cnt[eng] + 1)
        for b in reads:
            if len(b.r) > 8:
                b.r = [e for e in b.r if not (e[0] == ev[0] and e[1] <= ev[1])]
            b.r.append(ev)
        for b in writes:
            b.w = ev
            b.r = []
        if self.hook is not None and not self.in_hook:
            self.hook_n += 1
            if self.hook_n % 3 == 0:
                self.in_hook = True
                try:
                    next(self.hook)
                except StopIteration:
                    self.hook = None
                self.in_hook = False

    def barrier(self):
        evs = [(e, self.cnt[e]) for e in self.ENGS if self.cnt[e] > 0]
        evs += [(k, v) for k, v in self.dma_cnt.items()]
        for e in self.ENGS:
            wl = []
            for (k, v) in evs:
                if k == e:
                    continue
                if self.waited[e].get(k, 0) < v:
                    self.waited[e][k] = v
                    wl.append((k, v))
            if wl:
                self.streams[e].append((wl, None, None, 0))

    def emit(self, nc, sems, final_waits):
        with nc.Block() as block:
            def mk(engname):
                def body(e):
                    for (wl, fn, sk, inc) in self.streams[engname]:
                        for (k, v) in wl:
                            e.wait_ge(sems[k], v)
                        if fn is None:
                            continue
                        ins = fn(e)
                        if sk is not None:
                            ins.then_inc(sems[sk], inc)
                    for (k, v) in final_waits.get(engname, []):
                        e.wait_ge(sems[k], v)
                return body
            block.tensor(mk("tensor"))
            block.vector(mk("vector"))
            block.scalar(mk("scalar"))
            block.gpsimd(mk("gpsimd"))
            block.sync(mk("sync"))


class StopBuild(Exception):
    pass


def build(T, nseq, layers, dbg=False, kstop=0):
    def chk(n):
        if kstop == n:
            raise StopBuild()
    nc = bass.Bass("TRN2", target_bir_lowering=False)
    P = Plan()
    NL = len(layers)
    dr = {}

    def din(name, shape, dt=F32):
        dr[name] = nc.dram_tensor(name, list(shape), dt, kind="ExternalInput").ap()
        return dr[name]

    x_in = din("x", [nseq, T, D])
    ident_d = din("ident", [128, 128], BF16)
    tri_d = din("tri3", [128, 3, 128])
    mskL_d = din("mskL", [128, 512])
    mskT_d = din("mskT", [128, 512])
    id4_d = din("ident4", [128, 512])
    WN = {}
    for l in layers:
        WN[l] = dict(
            tok=din(f"wtok{l}", [128, 8, 3072]), fm=din(f"wfm{l}", [128, 32, 8, 128]),
            l1=din(f"wl1{l}", [128, 8, 288]), l2=din(f"wl2{l}", [128, 4, 1024]),
            ro=din(f"wro{l}", [128, 8, 8, 128]), co=din(f"wco{l}", [128, 8, 8, 128]),
            wo=din(f"wo{l}", [128, 8192]), g=din(f"wg{l}", [128, NFF * 1024]),
            u=din(f"wu{l}", [128, NFF * 1024]), d=din(f"wd{l}", [128, NFF * 1024]),
            vec=din(f"vec{l}", [NVEC, D]), vecT=din(f"vecT{l}", [128, 8, NVEC]), dw=din(f"dwT{l}", [128, 8, 31]))
    out = nc.dram_tensor("out", [nseq, T, D], F32, kind="ExternalOutput").ap()
    if 0 in layers and NL == 1:
        vf_d = nc.dram_tensor("vf", [nseq, T, D], F32, kind="ExternalOutput").ap()
    elif 0 in layers:
        vf_d = nc.dram_tensor("vf", [nseq, T, D], F32).ap()
    else:
        vf_d = din("vf", [nseq, T, D])
    SC = {}
    for l in layers:
        SC[l] = dict(
            tok=nc.dram_tensor(f"s_tok{l}", [128, 12, 16, 256], BF16).ap(),
            fm=nc.dram_tensor(f"s_fm{l}", [128, 32, 8, 128], BF16).ap(),
            l1=nc.dram_tensor(f"s_l1{l}", [128, 8, 2, 288], BF16).ap(),
            l2=nc.dram_tensor(f"s_l2{l}", [128, 4, 1024], BF16).ap(),
            ro=nc.dram_tensor(f"s_ro{l}", [128, 8, 8, 128], BF16).ap(),
            co=nc.dram_tensor(f"s_co{l}", [128, 8, 8, 128], BF16).ap(),
            wo=nc.dram_tensor(f"s_wo{l}", [128, 8192], BF16).ap(),
            g=nc.dram_tensor(f"s_g{l}", [128, NFF * 1024], BF16).ap(),
            u=nc.dram_tensor(f"s_u{l}", [128, NFF * 1024], BF16).ap(),
            d=nc.dram_tensor(f"s_d{l}", [128, NFF * 1024], BF16).ap())

    with ExitStack() as st:
        def sb(name, shape, dt=F32):
            return st.enter_context(nc.sbuf_tensor(name, list(shape), dt))
        B = {}

        def nb(name):
            B[name] = Buf(name)
            return B[name]

        def act(out_, in_, func, R, W, **kw):
            P.op("scalar", lambda e: e.activation(out=out_, in_=in_, func=func, **kw), R, W)

        def ts(eng, out_, in0, s1, s2, op0, op1, R, W):
            if s2 is None:
                P.op(eng, lambda e: e.tensor_scalar(out=out_, in0=in0, scalar1=s1, scalar2=None, op0=op0), R, W)
            else:
                P.op(eng, lambda e: e.tensor_scalar(out=out_, in0=in0, scalar1=s1, scalar2=s2, op0=op0, op1=op1), R, W)

        def tt(eng, out_, in0, in1, op, R, W):
            P.op(eng, lambda e: e.tensor_tensor(out=out_, in0=in0, in1=in1, op=op), R, W)

        def stt(out_, in0, scalar, in1, op0, op1, R, W, noself=False):
            P.op("vector", lambda e: e.scalar_tensor_tensor(out=out_, in0=in0, scalar=scalar, in1=in1, op0=op0, op1=op1), R, W, noself=noself)

        def cp(eng, out_, in_, R, W):
            P.op(eng, lambda e: e.tensor_copy(out=out_, in_=in_), R, W)

        def mset(eng, ap, val, W):
            P.op(eng, lambda e: e.memset(ap, val), (), W)

        def mm(out_, lhsT, rhs, start, stop, R, W, sig):
            P.op("tensor", lambda e: e.matmul(out_, lhsT, rhs, start=start, stop=stop), R, W, sig=sig)

        def tr(out_, in_, idn, R, W, sig):
            P.op("tensor", lambda e: e.transpose(out_, in_, idn), R, W, sig=sig)

        dma_rr = [0]

        def dma(out_, in_, R, W, sem, eng="sync"):
            P.op(eng, lambda e: e.dma_start(out=out_, in_=in_), R, W, dma_sem=sem)

        dumps = []

        def dump(name, ap_, shape, dt_, bufs):
            if not dbg:
                return
            t_ = nc.dram_tensor(name, list(shape), dt_, kind="ExternalOutput").ap()
            dma(t_, ap_, bufs, (), "dbg_" + name)
            dumps.append("dbg_" + name)

        IDENT = sb("IDENT", [128, 128], BF16); nb("IDENT")
        TRI = sb("TRI", [128, 3, 128]); nb("TRI")
        MSKL = sb("MSKL", [128, 512]); nb("MSKL")
        MSKT = sb("MSKT", [128, 512]); nb("MSKT")
        ID4 = sb("ID4", [128, 512]); nb("ID4")
        ONES32 = sb("ONES32", [128, 128]); nb("ONES32")
        NH = sb("NH", [128, NT]); nb("NH")
        dma(IDENT[:], ident_d, (), [B["IDENT"]], "c0")
        dma(TRI[:], tri_d, (), [B["TRI"]], "c1")
        dma(MSKL[:], mskL_d, (), [B["MSKL"]], "c2")
        dma(MSKT[:], mskT_d, (), [B["MSKT"]], "c3")
        dma(ID4[:], id4_d, (), [B["ID4"]], "c4")
        mset("gpsimd", ONES32[:], 1.0, [B["ONES32"]])
        mset("gpsimd", NH[:], -0.5, [B["NH"]])

        with ExitStack() as pst:
            def psb(name, shape, dt=F32):
                return pst.enter_context(nc.sbuf_tensor(name, list(shape), dt))
            STG = [psb(f"STG{i}", [128, 4096]) for i in range(2)]
            STB = [psb(f"STB{i}", [128, 4608], BF16) for i in range(2)]
            for i in range(2):
                nb(f"STG{i}"); nb(f"STB{i}")
            VB = psb("VB", [128, 3, 1024]); nb("VB")
            VB1 = psb("VB1", [128, 3, 1024]); nb("VB1")
            VT = psb("VT", [128, 8, NVEC]); nb("VT")
            VT1 = psb("VT1", [128, 8, 4]); nb("VT1")
            pk = [0]

            def plain(dst2d, src2d, n):
                for c0 in range(0, n, 4096):
                    c1 = min(n, c0 + 4096)
                    i = pk[0] % 2; pk[0] += 1
                    dma(STG[i][:, 0:c1 - c0], src2d[:, c0:c1], (), [B[f"STG{i}"]], f"pi{i}")
                    if i == 0:
                        cp("vector", STB[i][:, 0:c1 - c0], STG[i][:, 0:c1 - c0], [B[f"STG{i}"]], [B[f"STB{i}"]])
                    else:
                        act(STB[i][:, 0:c1 - c0], STG[i][:, 0:c1 - c0], AF.Copy, [B[f"STG{i}"]], [B[f"STB{i}"]])
                    dma(dst2d[:, c0:c1], STB[i][:, 0:c1 - c0], [B[f"STB{i}"]], (), f"po{i}", eng="gpsimd")

            for l in layers:
                W_, S_ = WN[l], SC[l]
                for j in range(3):
                    dma(VB[:, j, :], W_["vec"][V_MUR + j, :].partition_broadcast(128), (), [B["VB"]], "pv0")
                dma(VT[:], W_["vecT"], (), [B["VT"]], "pv1")
                ts("vector", VB1[:], VB[:], -1.0, 1.0, ALU.mult, ALU.add, [B["VB"]], [B["VB1"]])
                ts("vector", VT1[:], VT[:, :, V_MUW:V_MUW + 4], -1.0, 1.0, ALU.mult, ALU.add, [B["VT"]], [B["VT1"]])
                for j in range(12):
                    i = pk[0] % 2; pk[0] += 1
                    wi = j // 4
                    dma(STG[i][:, 0:2048].rearrange("p (k c) -> p k c", k=8), W_["tok"][:, :, j * 256:(j + 1) * 256],
                        (), [B[f"STG{i}"]], f"pi{i}")
                    for s in range(2):
                        vb = (VB1 if s == 0 else VB)
                        for kc in range(8):
                            tt("vector" if kc % 2 == 0 else "gpsimd",
                               STB[i][:, (kc * 2 + s) * 256:(kc * 2 + s + 1) * 256],
                               STG[i][:, kc * 256:(kc + 1) * 256], vb[:, wi, (j % 4) * 256:(j % 4 + 1) * 256], ALU.mult,
                               [B[f"STG{i}"], B["VB"], B["VB1"]], [B[f"STB{i}"]])
                    dma(S_["tok"][:, j, :, :], STB[i][:, 0:4096].rearrange("p (k c) -> p k c", k=16),
                        [B[f"STB{i}"]], (), f"po{i}", eng="gpsimd")
                i = pk[0] % 2; pk[0] += 1
                dma(STG[i][:, 0:2304].rearrange("p (k c) -> p k c", k=8), W_["l1"], (), [B[f"STG{i}"]], f"pi{i}")
                for (c0, c1, mi) in ((0, 64, 0), (64, 128, 1), (128, 256, 2), (256, 288, 3)):
                    for kc in range(8):
                        for s in range(2):
                            sc_ = (VT1[:, kc, mi:mi + 1] if s == 0 else VT[:, kc, V_MUW + mi:V_MUW + mi + 1])
                            ts("vector", STB[i][:, (kc * 2 + s) * 288 + c0:(kc * 2 + s) * 288 + c1],
                               STG[i][:, kc * 288 + c0:kc * 288 + c1], sc_, None, ALU.mult, None,
                               [B[f"STG{i}"], B["VT"], B["VT1"]], [B[f"STB{i}"]])
                dma(S_["l1"].rearrange("p k s c -> p (k s c)"), STB[i][:, 0:4608],
                    [B[f"STB{i}"]], (), f"po{i}", eng="gpsimd")
                plain(S_["l2"].rearrange("p a c -> p (a c)"), W_["l2"].rearrange("p a c -> p (a c)"), 4096)
                plain(S_["fm"].rearrange("p a k c -> p (a k c)"), W_["fm"].rearrange("p a k c -> p (a k c)"), 32768)
                plain(S_["ro"].rearrange("p a k c -> p (a k c)"), W_["ro"].rearrange("p a k c -> p (a k c)"), 8192)
                plain(S_["co"].rearrange("p a k c -> p (a k c)"), W_["co"].rearrange("p a k c -> p (a k c)"), 8192)
                plain(S_["wo"], W_["wo"], 8192)
                plain(S_["g"], W_["g"], NFF * 1024)
                plain(S_["u"], W_["u"], NFF * 1024)
                plain(S_["d"], W_["d"], NFF * 1024)
            P.barrier()
        RING = [sb(f"RING{i}", [128, 4608], BF16) for i in range(3)]
        for i in range(3):
            nb(f"RING{i}")
        rk = [0]

        def slab(src2d, n):
            i = rk[0] % 3; rk[0] += 1
            dma(RING[i][:, 0:n], src2d, (), [B[f"RING{i}"]], f"rg{i}")
            return RING[i], B[f"RING{i}"]

        PS = [st.enter_context(nc.psum_tensor(f"PSB{i}", [128, 512], F32)) for i in range(7)]
        PT = st.enter_context(nc.psum_tensor("PTB", [128, 1024], BF16)); nb("PT")
        for i in range(7):
            nb(f"PS{i}")
        pk2 = [0]

        def bank():
            i = pk2[0] % 5; pk2[0] += 1
            return PS[i], B[f"PS{i}"]

        def T_(name, shape, dt=F32):
            nb(name)
            return sb(name, shape, dt)
        X = T_("X", [128, NS, D])
        XNT = T_("XNT", [128, 8, NT + 1], BF16)
        CARRY = T_("CARRY", [128, 8, 1], BF16)
        L2BUF = T_("L2BUF", [128, 4096], BF16)
        CB = T_("CB", [128, 8, NT + 30])
        ACC = T_("ACC", [128, 8, NT])
        CBo = [Buf() for _ in range(8)]
        CTb = None
        ACCo = [Buf() for _ in range(8)]
        CCT = T_("CCT", [128, 8, NT], BF16)
        YT = T_("YT", [128, 8, NT], BF16)
        MRG = T_("MRG", [128, 8, NT], BF16)
        SS = T_("SS", [128, 4]); MS = T_("MS", [128, 4]); RSTD = T_("RSTD", [128, 4])
        LW1 = T_("LW1", [65, NT], BF16); LA1 = T_("LA1", [65, NT], BF16); LV1 = T_("LV1", [33, NT], BF16)
        LG1 = T_("LG1", [128, NT], BF16)
        TMPF = [T_(f"TMPF{i}", [128, NT]) for i in range(2)]
        MEAN = T_("MEAN", [128, NT]); VAR = T_("VAR", [128, NT]); RS = T_("RS", [128, NT])
        PV = T_("PV", [128, 8, NVEC])
        HLN = T_("HLN", [128, 8, 2])
        DW = T_("DW", [128, 8, 31])
        GPOST = T_("GPOST", [128, D]); GFPOST = T_("GFPOST", [128, D])
        KKT = T_("KKT", [128, D]); KAT = T_("KAT", [128, D]); RKT = T_("RKT", [128, D])
        GNW = T_("GNW", [128, D]); GNB = T_("GNB", [128, D])
        A = [T_(f"A{i}", [128, D]) for i in range(7)]
        EB = [T_(f"EB{i}", [128, D], BF16) for i in range(4)]
        OB = [T_(f"OB{i}", [128, D], BF16) for i in range(2)]
        JUNK = OB[1]; B["JUNK"] = B["OB1"]
        XNB = OB[0]; B["XNB"] = B["OB0"]
        KH = T_("KH", [128, D], BF16); BH = T_("BH", [128, D], BF16); VBF = T_("VBF", [128, D], BF16)
        TAR = T_("TAR", [128, 8, 2, 128], BF16)
        TBT = T_("TBT", [128, 8, 128], BF16); TKT = T_("TKT", [128, 8, 128], BF16)
        SM = T_("SM", [128, 16, 4])
        S32 = T_("S32", [128, 8, 64]); SBF = T_("SBF", [128, 8, 64], BF16); PC = T_("PC", [128, 8])
        XB = T_("XB", [128, 16, 64], BF16); UB = T_("UB", [128, 16, 64], BF16)
        R0 = [T_(f"R0_{g}", [128, 512], BF16) for g in range(2)]
        QRG = [[T_(f"QRG{g}_{i}", [128, 3, 512], BF16) for i in range(2)] for g in range(2)]
        MB = T_("MB", [128, 16, 2, 128], BF16)
        MK = T_("MK", [128, 16, 2, 128], BF16)
        G7 = T_("G7", [128, 16, 128], BF16)
        HT = MB[:].rearrange("p h a t -> p (h a t)")[:, 0:NFF * NT].rearrange("p (k t) -> p k t", k=NFF)
        B["HT"] = B["MB"]
        HTOK = MK[:].rearrange("p h a t -> p (h a t)")[:, 0:NFF * 128]
        TARm = [T_(f"TARm{i}", [128, 8, 2, 128], BF16) for i in range(2)]
        TBTm = [T_(f"TBTm{i}", [128, 8, 128], BF16) for i in range(2)]
        SBFm = [T_(f"SBFm{i}", [128, 8, 64], BF16) for i in range(2)]
        for i_ in range(2):
            for (tn, tl) in (("TARm", TARm), ("TBTm", TBTm), ("SBFm", SBFm)):
                mset("vector", tl[i_][:], 0.0, [B[f"{tn}{i_}"]])

        mset("vector", LW1[64:65, :], 1.0, [B["LW1"]])
        mset("vector", LA1[64:65, :], 1.0, [B["LA1"]])
        mset("vector", LV1[32:33, :], 1.0, [B["LV1"]])

        def rmsnorm_to(dst, coff, gcol):
            for s_ in range(NS):
                act(JUNK[:], X[:, s_, :], AF.Square, [B["X"]], [B["JUNK"], B["SS"]], accum_out=SS[:, 0:1])
                ts("vector", MS[:, 0:1], SS[:, 0:1], 1.0 / D, 1e-6, ALU.mult, ALU.add, [B["SS"]], [B["MS"]])
                tt("gpsimd", RSTD[:, 0:1], MS[:, 0:1], NH[:, 0:1], ALU.pow, [B["MS"], B["NH"]], [B["RSTD"]])
                act(XNB[:], X[:, s_, :], AF.Copy, [B["X"], B["RSTD"]], [B["XNB"]], scale=RSTD[:, 0:1])
                for kc in range(8):
                    tr(PT[:, kc * 128:(kc + 1) * 128], XNB[:, kc * 128:(kc + 1) * 128], IDENT[:],
                       [B["XNB"], B["IDENT"]], [B["PT"]], sig=(kc == 7))
                for kc in range(8):
                    ts("vector" if kc % 2 else "gpsimd" if False else "vector",
                       dst[:, kc, coff + s_ * 128:coff + (s_ + 1) * 128], PT[:, kc * 128:(kc + 1) * 128],
                       PV[:, kc, gcol:gcol + 1], None, ALU.mult, None, [B["PT"], B["PV"]], [B[dst_name[id(dst)]]])

        dst_name = {id(XNT): "XNT"}

        def sigm_from_tanh(eng, ap, R, W):
            act(ap, ap, AF.Identity, R, W, scale=0.5, bias=0.5)

        OUTB = {}
        VFB = {}
        try:
            chk(1)
            for l in layers:
                W_, S_ = WN[l], SC[l]
                has_v = (l != 0)
                dma(PV[:], W_["vecT"], (), [B["PV"]], "lp0")
                dma(DW[:], W_["dw"], (), [B["DW"]], "lp1")
                for (tile_, bn, row) in ((GPOST, "GPOST", V_GPOST), (GFPOST, "GFPOST", V_GFPOST), (KKT, "KKT", V_KK),
                                         (KAT, "KAT", V_KA), (RKT, "RKT", V_RK), (GNW, "GNW", V_GNW), (GNB, "GNB", V_GNB)):
                    dma(tile_[:], W_["vec"][row, :].partition_broadcast(128), (), [B[bn]], "lp_" + bn)
                ts("vector", HLN[:], PV[:, :, V_LNW:V_LNW + 2], 0.5, None, ALU.mult, None, [B["PV"]], [B["HLN"]])
                dma(L2BUF[:], S_["l2"].rearrange("p a c -> p (a c)"), (), [B["L2BUF"]], "lp2")
                for seq in range(nseq):
                    mset("vector", S32[:], 0.0, [B["S32"]])
                    mset("vector", SBF[:], 0.0, [B["SBF"]])
                    mset("gpsimd", CB[:, :, 0:30], 0.0, CBo)
                    for ti in range(T // NT):
                        t0 = ti * NT
                        xsrc = (x_in if l == layers[0] else out)[seq, t0:t0 + NT, :].rearrange("(s p) d -> p s d", p=128)
                        ob_ = OUTB.setdefault((seq, ti), Buf())
                        vb_ = VFB.setdefault((seq, ti), Buf())
                        dma(X[:], xsrc, ([] if l == layers[0] else [ob_]), [B["X"]], "dx")
                        if ti == 0:
                            mset("vector", XNT[:, :, 0:1], 0.0, [B["XNT"]])
                        else:
                            cp("vector", XNT[:, :, 0:1], CARRY[:], [B["CARRY"]], [B["XNT"]])
                        rmsnorm_to(XNT, 1, V_GPRE)
                        cp("vector", CARRY[:], XNT[:, :, NT:NT + 1], [B["XNT"]], [B["CARRY"]])
                        chk(2)
                        L1, BL1 = slab(S_["l1"].rearrange("p k s c -> p (k s c)"), 4608)
                        L1v = L1[:, 0:4608].rearrange("p (k s c) -> p k s c", k=8, s=2)
                        for (c0, c1, dstt, dn, fn_) in ((0, 64, LW1, "LW1", AF.Tanh), (64, 128, LA1, "LA1", AF.Copy),
                                                       (128, 256, LG1, "LG1", AF.Tanh), (256, 288, LV1, "LV1", AF.Copy)):
                            if c0 == 256 and not has_v:
                                continue
                            M_ = c1 - c0
                            pb, bpb = bank()
                            for kc in range(8):
                                for s2 in range(2):
                                    mm(pb[0:M_, 0:NT], L1v[:, kc, s2, c0:c1], XNT[:, kc, 1 - s2:1 - s2 + NT],
                                       kc == 0 and s2 == 0, kc == 7 and s2 == 1, [BL1, B["XNT"]], [bpb], sig=(kc == 7 and s2 == 1))
                            if dn == "LG1":
                                act(LG1[:], pb[0:128, 0:NT], AF.Tanh, [bpb], [B["LG1"]], scale=0.5)
                                sigm_from_tanh("gpsimd", LG1[:], [B["LG1"]], [B["LG1"]])
                            else:
                                act(dstt[0:M_, :], pb[0:M_, 0:NT], fn_, [bpb], [B[dn]])
                        BL2 = B["L2BUF"]
                        L2v = L2BUF[:, 0:4096].rearrange("p (a c) -> p a c", a=4)
                        chk(3)
                        for q in range(2):
                            FU, BFU = slab(S_["fm"][:, q * 4:(q + 1) * 4].rearrange("p a k c -> p (a k c)"), 4096)
                            FG, BFG = slab(S_["fm"][:, 8 + q * 4:8 + (q + 1) * 4].rearrange("p a k c -> p (a k c)"), 4096)
                            FUv = FU[:, 0:4096].rearrange("p (a k c) -> p a k c", a=4, k=8)
                            FGv = FG[:, 0:4096].rearrange("p (a k c) -> p a k c", a=4, k=8)
                            for o4 in range(4):
                                oc = q * 4 + o4
                                bu, bbu = bank(); bg, bbg = bank()
                                for kc in range(8):
                                    mm(bu[:, 0:NT], FUv[:, o4, kc, :], XNT[:, kc, 1:1 + NT], kc == 0, kc == 7, [BFU, B["XNT"]], [bbu], sig=(kc == 7))
                                for kc in range(8):
                                    mm(bg[:, 0:NT], FGv[:, o4, kc, :], XNT[:, kc, 1:1 + NT], kc == 0, kc == 7, [BFG, B["XNT"]], [bbg], sig=(kc == 7))
                                tf = TMPF[oc % 2]; btf = B[f"TMPF{oc % 2}"]
                                act(tf[:], bg[:, 0:NT], AF.Tanh, [bbg], [btf], scale=0.5)
                                sigm_from_tanh("gpsimd", tf[:], [btf], [btf])
                                tt("vector", CB[:, oc, 30:30 + NT], bu[:, 0:NT], tf[:], ALU.mult, [bbu, btf], [CBo[oc]])
                        def conv_tail():
                            for oc in range(8):
                                ts("vector", ACC[:, oc, :], CB[:, oc, 0:NT], DW[:, oc, 0:1], PV[:, oc, V_CB:V_CB + 1], ALU.mult, ALU.add,
                                   [CBo[oc], B["DW"], B["PV"]], [ACCo[oc]])
                                yield
                            for j in range(1, 31):
                                for oc in range(8):
                                    stt(ACC[:, oc, :], CB[:, oc, j:j + NT], DW[:, oc, j:j + 1], ACC[:, oc, :], ALU.mult, ALU.add,
                                        [CBo[oc], B["DW"], ACCo[oc]], [ACCo[oc]])
                                    yield
                            cp("gpsimd", CB[:, :, 0:30], CB[:, :, NT:NT + 30], CBo, CBo)
                            yield
                            s1, bs1 = PS[5], B["PS5"]; s2b, bs2 = PS[6], B["PS6"]
                            for oc in range(8):
                                mm(s1[:, 0:NT], ONES32[:], ACC[:, oc, :], oc == 0, oc == 7, [B["ONES32"], ACCo[oc]], [bs1], sig=(oc == 7))
                                yield
                            for oc in range(8):
                                tf = TMPF[oc % 2]; btf = B[f"TMPF{oc % 2}"]
                                act(tf[:], ACC[:, oc, :], AF.Square, [ACCo[oc]], [btf])
                                yield
                                mm(s2b[:, 0:NT], ONES32[:], tf[:], oc == 0, oc == 7, [B["ONES32"], btf], [bs2], sig=True)
                                yield
                            act(MEAN[:], s1[:, 0:NT], AF.Copy, [bs1], [B["MEAN"]], scale=1.0 / D)
                            yield
                            act(VAR[:], s2b[:, 0:NT], AF.Copy, [bs2], [B["VAR"]], scale=1.0 / D)
                            yield
                            tt("gpsimd", RS[:], MEAN[:], MEAN[:], ALU.mult, [B["MEAN"]], [B["RS"]])
                            yield
                            tt("gpsimd", VAR[:], VAR[:], RS[:], ALU.subtract, [B["VAR"], B["RS"]], [B["VAR"]])
                            yield
                            ts("gpsimd", VAR[:], VAR[:], 1e-5, None, ALU.add, None, [B["VAR"]], [B["VAR"]])
                            yield
                            tt("gpsimd", RS[:], VAR[:], NH[:], ALU.pow, [B["VAR"], B["NH"]], [B["RS"]])
                            yield
                            for oc in range(8):
                                t1 = TMPF[0]; t2 = TMPF[1]
                                tt("vector", t1[:], ACC[:, oc, :], MEAN[:], ALU.subtract, [ACCo[oc], B["MEAN"]], [B["TMPF0"]])
                                yield
                                tt("gpsimd", t1[:], t1[:], RS[:], ALU.mult, [B["TMPF0"], B["RS"]], [B["TMPF0"]])
                                yield
                                act(t2[:], t1[:], AF.Tanh, [B["TMPF0"], B["HLN"]], [B["TMPF1"]], scale=HLN[:, oc, 0:1], bias=HLN[:, oc, 1:2])
                                yield
                                ts("vector", t1[:], t1[:], PV[:, oc, V_LNW:V_LNW + 1], PV[:, oc, V_LNB:V_LNB + 1], ALU.mult, ALU.add,
                                   [B["TMPF0"], B["PV"]], [B["TMPF0"]])
                                yield
                                sigm_from_tanh("gpsimd", t2[:], [B["TMPF1"]], [B["TMPF1"]])
                                yield
                                tt("vector", CCT[:, oc, :], t1[:], t2[:], ALU.mult, [B["TMPF0"], B["TMPF1"]], [B["CCT"]])
                                yield


                        P.hook = conv_tail(); P.hook_n = 0
                        chk(4)
                        r32, k32, v32, asg, lw, t1, t2 = A
                        bR, bK, bV, bAS, bLW, bT1, bT2 = [B[f"A{i}"] for i in range(7)]
                        for j in range(12):
                            TK, BTK = slab(S_["tok"][:, j].rearrange("p k c -> p (k c)"), 4096)
                            TKv = TK[:, 0:4096].rearrange("p (k c) -> p k c", k=16)
                            pb, bpb = bank()
                            for kc in range(8):
                                for s2 in range(2):
                                    mm(pb[:, 0:256], XNT[:, kc, 1 - s2:1 - s2 + NT], TKv[:, kc * 2 + s2, :],
                                       kc == 0 and s2 == 0, kc == 7 and s2 == 1, [BTK, B["XNT"]], [bpb], sig=(kc == 7 and s2 == 1))
                            dstA = A[j // 4]
                            act(dstA[:, (j % 4) * 256:(j % 4 + 1) * 256], pb[:, 0:256], AF.Copy, [bpb], [B[f"A{j // 4}"]])
                        def lora2(src, K_, a_idx):
                            res = []
                            for hf in range(2):
                                pb, bpb = bank()
                                mm(pb[:, 0:512], src[0:K_, :], L2v[0:K_, a_idx, hf * 512:(hf + 1) * 512], True, True,
                                   [B["LW1"], B["LA1"], B["LV1"], B["LG1"], BL2], [bpb], sig=True)
                                res.append((pb, bpb))
                            return res
                        if l == 0:
                            dma(vf_d[seq, t0:t0 + NT, :], v32[:], [bV], [vb_], "vfo")
                        else:
                            VF = t2
                            dma(VF[:], vf_d[seq, t0:t0 + NT, :], [vb_], [bT2], "vfi")
                            rr = lora2(LV1, 33, 2)
                            for hf, (pb, bpb) in enumerate(rr):
                                act(t1[:, hf * 512:(hf + 1) * 512], pb[:, 0:512], AF.Tanh, [bpb], [bT1], scale=0.5)
                            sigm_from_tanh("gpsimd", t1[:], [bT1], [bT1])
                            tt("gpsimd", VF[:], VF[:], v32[:], ALU.subtract, [bT2, bV], [bT2])
                            tt("gpsimd", VF[:], VF[:], t1[:], ALU.mult, [bT2, bT1], [bT2])
                            tt("gpsimd", v32[:], v32[:], VF[:], ALU.add, [bV, bT2], [bV])
                        rr = lora2(LA1, 65, 1)
                        for hf, (pb, bpb) in enumerate(rr):
                            act(asg[:, hf * 512:(hf + 1) * 512], pb[:, 0:512], AF.Tanh, [bpb], [bAS], scale=0.5)
                        sigm_from_tanh("gpsimd", asg[:], [bAS], [bAS])
                        rr = lora2(LW1, 65, 0)
                        for hf, (pb, bpb) in enumerate(rr):
                            act(lw[:, hf * 512:(hf + 1) * 512], pb[:, 0:512], AF.Tanh, [bpb], [bLW], scale=0.5)
                        act(lw[:], lw[:], AF.Identity, [bLW], [bLW], scale=-0.30326533, bias=-0.30326533)
                        chk(5)
                        for (ti_, dsts) in ((0, ((EB[0], "EB0", 1.0), (EB[1], "EB1", -1.0))), (1, ((EB[2], "EB2", 1.0),)), (2, ((EB[3], "EB3", 1.0),))):
                            for hf in range(2):
                                pb, bpb = bank()
                                mm(pb[:, 0:512], TRI[:, ti_, :], lw[:, hf * 512:(hf + 1) * 512], True, True, [B["TRI"], bLW], [bpb], sig=True)
                                for (dd, dn, sc_) in dsts:
                                    act(dd[:, hf * 512:(hf + 1) * 512], pb[:, 0:512], AF.Exp, [bpb], [B[dn]], scale=sc_)
                        pb, bpb = bank()
                        for h in range(H):
                            po = (h % 2) * 64
                            mm(pb[po:po + 64, h // 2:h // 2 + 1], lw[:, h * 64:(h + 1) * 64], ONES32[:, 0:1], True, True,
                               [bLW, B["ONES32"]], [bpb], sig=(h == H - 1))
                        act(PC[:], pb[:, 0:8], AF.Exp, [bpb], [B["PC"]])
                        tt("gpsimd", t1[:], k32[:], KKT[:], ALU.mult, [bK, B["KKT"]], [bT1])
                        act(t2[:], t1[:], AF.Square, [bT1], [bT2])
                        P.op("vector", lambda e: e.tensor_reduce(out=SM[:, :, 0], in_=t2[:].rearrange("p (h c) -> p h c", h=H), axis=AX.X, op=ALU.add), [bT2], [B["SM"]])
                        ts("vector", SM[:, :, 0], SM[:, :, 0], 1e-24, None, ALU.max, None, [B["SM"]], [B["SM"]])
                        tt("gpsimd", SM[:, :, 1], SM[:, :, 0], NH[:, 0:H], ALU.pow, [B["SM"], B["NH"]], [B["SM"]])
                        t1v = t1[:].rearrange("p (h c) -> p h c", h=H)
                        tt("vector", t1v, t1v, SM[:, :, 1:2].broadcast_to([128, H, 64]), ALU.mult, [bT1, B["SM"]], [bT1])
                        kk = t1
                        stt(t2[:], asg[:], -1.0, KAT[:], ALU.add, ALU.mult, [bAS, B["KAT"]], [bT2])
                        stt(k32[:], t2[:], 1.0, k32[:], ALU.add, ALU.mult, [bT2, bK], [bK])
                        tt("gpsimd", t2[:], r32[:], k32[:], ALU.mult, [bR, bK], [bT2])
                        tt("gpsimd", t2[:], t2[:], RKT[:], ALU.mult, [bT2, B["RKT"]], [bT2])
                        P.op("vector", lambda e: e.tensor_reduce(out=SM[:, :, 2], in_=t2[:].rearrange("p (h c) -> p h c", h=H), axis=AX.X, op=ALU.add), [bT2], [B["SM"]])
                        tt("gpsimd", asg[:], asg[:], kk[:], ALU.mult, [bAS, bT1], [bAS])
                        bvec = asg
                        tt("vector", KH[:], k32[:], EB[3][:], ALU.mult, [bK, B["EB3"]], [B["KH"]])
                        tt("gpsimd", BH[:], bvec[:], EB[3][:], ALU.mult, [bAS, B["EB3"]], [B["BH"]])
                        act(VBF[:], v32[:], AF.Copy, [bV], [B["VBF"]])
                        def trans_to(srcf, bsrc, eb, ebn, neg, dst_fn, dname, oi):
                            ob = OB[oi]; bob = B[f"OB{oi}"]
                            if neg:
                                stt(ob[:], srcf[:], -1.0, eb[:], ALU.mult, ALU.mult, [bsrc, B[ebn]], [bob])
                            else:
                                tt("vector", ob[:], srcf[:], eb[:], ALU.mult, [bsrc, B[ebn]], [bob])
                            for kc in range(8):
                                tr(PT[:, kc * 128:(kc + 1) * 128], ob[:, kc * 128:(kc + 1) * 128], IDENT[:], [bob, B["IDENT"]], [B["PT"]], sig=(kc == 7))
                            cp("vector", dst_fn, PT[:].rearrange("p (k t) -> p k t", k=8), [B["PT"]], [B[dname]])
                        trans_to(r32, bR, EB[0], "EB0", False, TAR[:, :, 1, :], "TAR", 0)
                        trans_to(kk, bT1, EB[2], "EB2", True, TAR[:, :, 0, :], "TAR", 1)
                        trans_to(bvec, bAS, EB[1], "EB1", False, TBT[:], "TBT", 0)
                        trans_to(k32, bK, EB[1], "EB1", False, TKT[:], "TKT", 1)
                        for i_ in range(2):
                            ps_ = slice(i_ * 64, (i_ + 1) * 64)
                            cp("vector", TARm[i_][ps_], TAR[ps_], [B["TAR"]], [B[f"TARm{i_}"]])
                            cp("gpsimd", TBTm[i_][ps_], TBT[ps_], [B["TBT"]], [B[f"TBTm{i_}"]])
                            cp("gpsimd", SBFm[i_][ps_], SBF[ps_], [B["SBF"]], [B[f"SBFm{i_}"]])
                        chk(6)
                        for pr_ in range(2):
                            st_ = {}
                            for g4 in (2 * pr_, 2 * pr_ + 1):
                                gi = g4 % 2
                                pl, bpl = bank()
                                for hh in range(4):
                                    h = g4 * 4 + hh; kc = h // 2
                                    mm(pl[:, hh * 128:(hh + 1) * 128], TAR[:, kc, 0, :], TBTm[h % 2][:, kc, :], True, True,
                                       [B["TAR"], B[f"TBTm{h % 2}"]], [bpl], sig=(hh == 3))
                                tt("vector", R0[gi][:], pl[:, 0:512], MSKL[:], ALU.mult, [bpl, B["MSKL"]], [B[f"R0_{gi}"]])
                                for (lt, ltn, dstm, dmn) in ((TBT, "TBT", MB, "MB"), (TKT, "TKT", MK, "MK")):
                                    for h2 in range(2):
                                        pb, bpb = bank()
                                        for hh in range(2):
                                            h = g4 * 4 + h2 * 2 + hh; kc = h // 2
                                            mm(pb[:, hh * 256:(hh + 1) * 256], lt[:, kc, :], TARm[h % 2][:, kc, :, :].rearrange("p a t -> p (a t)"),
                                               True, True, [B[ltn], B[f"TARm{h % 2}"]], [bpb], sig=(hh == 1))
                                        h0 = g4 * 4 + h2 * 2
                                        tt("vector", dstm[:, h0:h0 + 2, :, :].rearrange("p h a t -> p (h a t)"), pb[:, 0:512], MSKT[:], ALU.mult,
                                           [bpb, B["MSKT"]], [B[dmn]])
                                cur = QRG[gi][0]; nxt = QRG[gi][1]; bcur = B[f"QRG{gi}_0"]; bnxt = B[f"QRG{gi}_1"]
                                cp("vector", cur[:, 0, :].rearrange("p (h t) -> p h t", h=4), MB[:, g4 * 4:g4 * 4 + 4, 0, :], [B["MB"]], [bcur])
                                cp("gpsimd", cur[:, 1, :], R0[gi][:], [B[f"R0_{gi}"]], [bcur])
                                tt("gpsimd", cur[:, 2, :], cur[:, 0, :], ID4[:], ALU.add, [bcur, B["ID4"]], [bcur])
                                st_[g4] = [cur, nxt, bcur, bnxt]
                            for jj in range(1, 7):
                                pbk = {}
                                for g4 in (2 * pr_, 2 * pr_ + 1):
                                    cur, nxt, bcur, bnxt = st_[g4]
                                    pq, bpq = bank() if jj < 6 else (None, None)
                                    pr, bpr = bank()
                                    for hh in range(4):
                                        cs_ = slice(hh * 128, (hh + 1) * 128)
                                        if jj < 6:
                                            mm(pq[:, cs_], cur[:, 1, cs_], cur[:, 0, cs_], True, True, [bcur], [bpq], sig=(hh == 3))
                                    for hh in range(4):
                                        cs_ = slice(hh * 128, (hh + 1) * 128)
                                        mm(pr[:, cs_], cur[:, 0, cs_], cur[:, 1, cs_], True, True, [bcur], [bpr], sig=(hh == 3))
                                    pbk[g4] = (pq, bpq, pr, bpr)
                                for g4 in (2 * pr_, 2 * pr_ + 1):
                                    cur, nxt, bcur, bnxt = st_[g4]
                                    pq, bpq, pr, bpr = pbk[g4]
                                    if jj < 6:
                                        act(nxt[:, 0, :], pq[:, 0:512], AF.Copy, [bpq], [bnxt])
                                    cp("vector", nxt[:, 1, :], pr[:, 0:512], [bpr], [bnxt])
                                pgk = {}
                                for g4 in (2 * pr_, 2 * pr_ + 1):
                                    cur, nxt, bcur, bnxt = st_[g4]
                                    pg, bpg = bank()
                                    for hh in range(4):
                                        cs_ = slice(hh * 128, (hh + 1) * 128)
                                        mm(pg[:, cs_], nxt[:, 1, cs_], cur[:, 2, cs_], True, True, [bnxt, bcur], [bpg], sig=(hh == 3))
                                    pgk[g4] = (pg, bpg)
                                for g4 in (2 * pr_, 2 * pr_ + 1):
                                    cur, nxt, bcur, bnxt = st_[g4]
                                    pg, bpg = pgk[g4]
                                    if jj < 6:
                                        tt("vector", nxt[:, 2, :], pg[:, 0:512], cur[:, 2, :], ALU.add, [bpg, bcur], [bnxt])
                                    else:
                                        tt("vector", G7[:, g4 * 4:g4 * 4 + 4, :].rearrange("p h t -> p (h t)"), pg[:, 0:512], cur[:, 2, :], ALU.add,
                                           [bpg, bcur], [B["G7"]])
                                    st_[g4] = [nxt, cur, bnxt, bcur]
                        chk(7)
                        for h8 in range(2):
                            px, bpx = bank()
                            for hh in range(8):
                                h = h8 * 8 + hh; kc = h // 2; po = (h % 2) * 64
                                mm(px[:, hh * 64:(hh + 1) * 64], TAR[:, kc, 0, :], SBFm[h % 2][:, kc, :], True, False, [B["TAR"], B[f"SBFm{h % 2}"]], [bpx], sig=False)
                                mm(px[:, hh * 64:(hh + 1) * 64], MK[:, h, 0, :], VBF[:, h * 64:(h + 1) * 64], False, True, [B["MK"], B["VBF"]], [bpx], sig=(hh == 7))
                            cp("vector", XB[:, h8 * 8:(h8 + 1) * 8, :].rearrange("p h c -> p (h c)"), px[:, 0:512], [bpx], [B["XB"]])
                        for h8 in range(2):
                            pu, bpu = bank()
                            for hh in range(8):
                                h = h8 * 8 + hh
                                mm(pu[:, hh * 64:(hh + 1) * 64], G7[:, h, :], XB[:, h, :], True, True, [B["G7"], B["XB"]], [bpu], sig=(hh == 7))
                            cp("vector", UB[:, h8 * 8:(h8 + 1) * 8, :].rearrange("p h c -> p (h c)"), pu[:, 0:512], [bpu], [B["UB"]])
                        ybanks = []
                        for h8 in range(2):
                            py, bpy = bank()
                            for hh in range(8):
                                h = h8 * 8 + hh; kc = h // 2; po = (h % 2) * 64
                                o_ = py[:, hh * 64:(hh + 1) * 64]
                                mm(o_, TAR[:, kc, 1, :], SBFm[h % 2][:, kc, :], True, False, [B["TAR"], B[f"SBFm{h % 2}"]], [bpy], sig=False)
                                mm(o_, MB[:, h, 1, :], UB[:, h, :], False, False, [B["MB"], B["UB"]], [bpy], sig=False)
                                mm(o_, MK[:, h, 1, :], VBF[:, h * 64:(h + 1) * 64], False, True, [B["MK"], B["VBF"]], [bpy], sig=(hh == 7))
                            ybanks.append((py, bpy))
                        pss, bpss = bank()
                        for h in range(H):
                            kc = h // 2; po = (h % 2) * 64
                            o_ = pss[po:po + 64, kc * 64:(kc + 1) * 64]
                            mm(o_, BH[:, h * 64:(h + 1) * 64], UB[:, h, :], True, False, [B["BH"], B["UB"]], [bpss], sig=False)
                            mm(o_, KH[:, h * 64:(h + 1) * 64], VBF[:, h * 64:(h + 1) * 64], False, True, [B["KH"], B["VBF"]], [bpss], sig=(h == H - 1))
                        for kc in range(8):
                            stt(S32[:, kc, :], S32[:, kc, :], PC[:, kc:kc + 1], pss[:, kc * 64:(kc + 1) * 64], ALU.mult, ALU.add,
                                [B["S32"], B["PC"], bpss], [B["S32"]])
                        y32 = r32
                        for h8, (py, bpy) in enumerate(ybanks):
                            act(y32[:, h8 * 512:(h8 + 1) * 512], py[:, 0:512], AF.Copy, [bpy], [bR])
                        act(SBF[:], S32[:], AF.Copy, [B["S32"]], [B["SBF"]])
                        P.op("vector", lambda e: e.tensor_reduce(out=SM[:, :, 0], in_=y32[:].rearrange("p (h c) -> p h c", h=H), axis=AX.X, op=ALU.add), [bR], [B["SM"]])
                        act(t2[:], y32[:], AF.Square, [bR], [bT2])
                        P.op("vector", lambda e: e.tensor_reduce(out=SM[:, :, 1], in_=t2[:].rearrange("p (h c) -> p h c", h=H), axis=AX.X, op=ALU.add), [bT2], [B["SM"]])
                        ts("vector", SM[:, :, 0], SM[:, :, 0], 1.0 / 64, None, ALU.mult, None, [B["SM"]], [B["SM"]])
                        tt("vector", SM[:, :, 3], SM[:, :, 0], SM[:, :, 0], ALU.mult, [B["SM"]], [B["SM"]])
                        stt(SM[:, :, 1], SM[:, :, 1], 1.0 / 64, SM[:, :, 3], ALU.mult, ALU.subtract, [B["SM"]], [B["SM"]])
                        ts("vector", SM[:, :, 1], SM[:, :, 1], 64e-5, None, ALU.add, None, [B["SM"]], [B["SM"]])
                        tt("gpsimd", SM[:, :, 3], SM[:, :, 1], NH[:, 0:H], ALU.pow, [B["SM"], B["NH"]], [B["SM"]])
                        yv = y32[:].rearrange("p (h c) -> p h c", h=H)
                        tt("vector", yv, yv, SM[:, :, 0:1].broadcast_to([128, H, 64]), ALU.subtract, [bR, B["SM"]], [bR])
                        tt("vector", yv, yv, SM[:, :, 3:4].broadcast_to([128, H, 64]), ALU.mult, [bR, B["SM"]], [bR])
                        tt("gpsimd", y32[:], y32[:], GNW[:], ALU.mult, [bR, B["GNW"]], [bR])
                        tt("gpsimd", y32[:], y32[:], GNB[:], ALU.add, [bR, B["GNB"]], [bR])
                        tt("vector", t2[:].rearrange("p (h c) -> p h c", h=H), v32[:].rearrange("p (h c) -> p h c", h=H),
                           SM[:, :, 2:3].broadcast_to([128, H, 64]), ALU.mult, [bV, B["SM"]], [bT2])
                        tt("vector", y32[:], y32[:], t2[:], ALU.add, [bR, bT2], [bR])
                        rr = lora2(LG1, 128, 3)
                        for hf, (pb, bpb) in enumerate(rr):
                            tt("vector", OB[0][:, hf * 512:(hf + 1) * 512], pb[:, 0:512], y32[:, hf * 512:(hf + 1) * 512], ALU.mult, [bpb, bR], [B["OB0"]])
                        for kc in range(8):
                            tr(PT[:, kc * 128:(kc + 1) * 128], OB[0][:, kc * 128:(kc + 1) * 128], IDENT[:], [B["OB0"], B["IDENT"]], [B["PT"]], sig=(kc == 7))
                        cp("vector", YT[:], PT[:].rearrange("p (k t) -> p k t", k=8), [B["PT"]], [B["YT"]])

                        if P.hook is not None:
                            P.in_hook = True
                            for _ in P.hook:
                                pass
                            P.in_hook = False
                            P.hook = None
                        chk(8)
                        def fm_proj(slab_src_fn, rhs_t, brhs, oc):
                            pass
                        for q in range(2):
                            RO_, BRO = slab(S_["ro"][:, q * 4:(q + 1) * 4].rearrange("p a k c -> p (a k c)"), 4096)
                            ZR_, BZR = slab(S_["fm"][:, 16 + q * 4:16 + (q + 1) * 4].rearrange("p a k c -> p (a k c)"), 4096)
                            for o4 in range(4):
                                oc = q * 4 + o4
                                py_, bpy_ = bank(); pz_, bpz_ = bank()
                                ROv = RO_[:, 0:4096].rearrange("p (a k c) -> p a k c", a=4, k=8)
                                ZRv = ZR_[:, 0:4096].rearrange("p (a k c) -> p a k c", a=4, k=8)
                                for kc in range(8):
                                    mm(py_[:, 0:NT], ROv[:, o4, kc, :], YT[:, kc, :], kc == 0, kc == 7, [BRO, B["YT"]], [bpy_], sig=(kc == 7))
                                for kc in range(8):
                                    mm(pz_[:, 0:NT], ZRv[:, o4, kc, :], XNT[:, kc, 1:1 + NT], kc == 0, kc == 7, [BZR, B["XNT"]], [bpz_], sig=(kc == 7))
                                tf = TMPF[0]
                                act(tf[:], pz_[:, 0:NT], AF.Tanh, [bpz_], [B["TMPF0"]], scale=0.5)
                                sigm_from_tanh("gpsimd", tf[:], [B["TMPF0"]], [B["TMPF0"]])
                                tt("vector", ACC[:, oc, :], py_[:, 0:NT], tf[:], ALU.mult, [bpy_, B["TMPF0"]], [ACCo[oc]])
                        for q in range(2):
                            CO_, BCO = slab(S_["co"][:, q * 4:(q + 1) * 4].rearrange("p a k c -> p (a k c)"), 4096)
                            ZC_, BZC = slab(S_["fm"][:, 24 + q * 4:24 + (q + 1) * 4].rearrange("p a k c -> p (a k c)"), 4096)
                            for o4 in range(4):
                                oc = q * 4 + o4
                                py_, bpy_ = bank(); pz_, bpz_ = bank()
                                COv = CO_[:, 0:4096].rearrange("p (a k c) -> p a k c", a=4, k=8)
                                ZCv = ZC_[:, 0:4096].rearrange("p (a k c) -> p a k c", a=4, k=8)
                                for kc in range(8):
                                    mm(py_[:, 0:NT], COv[:, o4, kc, :], CCT[:, kc, :], kc == 0, kc == 7, [BCO, B["CCT"]], [bpy_], sig=(kc == 7))
                                for kc in range(8):
                                    mm(pz_[:, 0:NT], ZCv[:, o4, kc, :], XNT[:, kc, 1:1 + NT], kc == 0, kc == 7, [BZC, B["XNT"]], [bpz_], sig=(kc == 7))
                                tf = TMPF[1]
                                act(tf[:], pz_[:, 0:NT], AF.Tanh, [bpz_], [B["TMPF1"]], scale=0.5)
                                sigm_from_tanh("gpsimd", tf[:], [B["TMPF1"]], [B["TMPF1"]])
                                tt("vector", tf[:], py_[:, 0:NT], tf[:], ALU.mult, [bpy_, B["TMPF1"]], [B["TMPF1"]])
                                tt("gpsimd", MRG[:, oc, :], tf[:], ACC[:, oc, :], ALU.add, [B["TMPF1"], ACCo[oc]], [B["MRG"]])

                        def out_norm_resid(pbs, gtile, gname):
                            for hf, (pb, bpb) in enumerate(pbs):
                                act(JUNK[:, 0:512], pb[:, 0:512], AF.Square, [bpb], [B["JUNK"], B["SS"]], accum_out=SS[:, hf:hf + 1])
                            tt("vector", MS[:, 0:1], SS[:, 0:1], SS[:, 1:2], ALU.add, [B["SS"]], [B["MS"]])
                            ts("vector", MS[:, 0:1], MS[:, 0:1], 1.0 / D, 1e-6, ALU.mult, ALU.add, [B["MS"]], [B["MS"]])
                            tt("gpsimd", RSTD[:, 0:1], MS[:, 0:1], NH[:, 0:1], ALU.pow, [B["MS"], B["NH"]], [B["RSTD"]])
                            for hf, (pb, bpb) in enumerate(pbs):
                                hs = slice(hf * 512, (hf + 1) * 512)
                                stt(A[5][:, hs], pb[:, 0:512], RSTD[:, 0:1], gtile[:, hs], ALU.mult, ALU.mult, [bpb, B["RSTD"], B[gname]], [B["A5"]])
                            tt("gpsimd", X[:, 0, :], X[:, 0, :], A[5][:], ALU.add, [B["X"], B["A5"]], [B["X"]])

                        WO_, BWO = [], []
                        pbs = []
                        for hf in range(2):
                            w_, bw_ = slab(S_["wo"][:, hf * 4096:(hf + 1) * 4096], 4096)
                            wv = w_[:, 0:4096].rearrange("p (k c) -> p k c", k=8)
                            pb, bpb = bank()
                            for kc in range(8):
                                mm(pb[:, 0:512], MRG[:, kc, :], wv[:, kc, :], kc == 0, kc == 7, [B["MRG"], bw_], [bpb], sig=(kc == 7))
                            pbs.append((pb, bpb))
                        out_norm_resid(pbs, GPOST, "GPOST")
                        if ti == 0 and seq == 0 and l == layers[0]:
                            dump("d_x1", X[:, 0, :], [128, D], F32, [B["X"]])
                            dump("d_yt", YT[:], [128, 8, NT], BF16, [B["YT"]])
                            dump("d_cct", CCT[:], [128, 8, NT], BF16, [B["CCT"]])
                            dump("d_mrg", MRG[:], [128, 8, NT], BF16, [B["MRG"]])
                            dump("d_xnt", XNT[:], [128, 8, NT + 1], BF16, [B["XNT"]])
                            dump("d_v", A[2][:], [128, D], F32, [B["A2"]])
                            dump("d_k", A[1][:], [128, D], F32, [B["A1"]])
                            dump("d_y", A[0][:], [128, D], F32, [B["A0"]])
                            dump("d_b", A[3][:], [128, D], F32, [B["A3"]])
                            dump("d_lw", A[4][:], [128, D], F32, [B["A4"]])
                        rmsnorm_to(XNT, 1, V_GFPRE)
                        for cb in range(6):
                            c0 = cb * 512; cw = 512 if cb < 5 else 256
                            G_, BG_ = slab(S_["g"][:, cb * 4096:cb * 4096 + 8 * cw], 8 * cw)
                            U_, BU_ = slab(S_["u"][:, cb * 4096:cb * 4096 + 8 * cw], 8 * cw)
                            Gv = G_[:, 0:8 * cw].rearrange("p (k c) -> p k c", k=8)
                            Uv = U_[:, 0:8 * cw].rearrange("p (k c) -> p k c", k=8)
                            pg_, bpg_ = bank(); pu_, bpu_ = bank()
                            for kc in range(8):
                                mm(pg_[:, 0:cw], XNT[:, kc, 1:1 + NT], Gv[:, kc, :], kc == 0, kc == 7, [BG_, B["XNT"]], [bpg_], sig=(kc == 7))
                            for kc in range(8):
                                mm(pu_[:, 0:cw], XNT[:, kc, 1:1 + NT], Uv[:, kc, :], kc == 0, kc == 7, [BU_, B["XNT"]], [bpu_], sig=(kc == 7))
                            ft = A[6][:, (cb % 2) * 512:(cb % 2) * 512 + cw]
                            act(ft, pg_[:, 0:cw], AF.Tanh, [bpg_], [B["A6"]], scale=0.5)
                            act(ft, ft, AF.Identity, [B["A6"]], [B["A6"]], scale=0.5, bias=0.5)
                            tt("vector", ft, pg_[:, 0:cw], ft, ALU.mult, [bpg_, B["A6"]], [B["A6"]])
                            tt("vector", HTOK[:, c0:c0 + cw], pu_[:, 0:cw], ft, ALU.mult, [bpu_, B["A6"]], [B["MK"]])
                        for g8 in range(3):
                            n8 = 8 if g8 < 2 else 6
                            for k8 in range(n8):
                                kc = g8 * 8 + k8
                                tr(PT[:, k8 * 128:(k8 + 1) * 128], HTOK[:, kc * 128:(kc + 1) * 128], IDENT[:], [B["MK"], B["IDENT"]], [B["PT"]], sig=(k8 == n8 - 1))
                            cp("vector" if g8 != 1 else "scalar_", HT[:, g8 * 8:g8 * 8 + n8, :], PT[:, 0:n8 * 128].rearrange("p (k t) -> p k t", k=n8), [B["PT"]], [B["HT"]]) if g8 != 1 else act(HT[:, g8 * 8:g8 * 8 + n8, :], PT[:, 0:n8 * 128].rearrange("p (k t) -> p k t", k=n8), AF.Copy, [B["PT"]], [B["HT"]])
                        pbs = []
                        for hf in range(2):
                            pb, bpb = bank()
                            for q in range(3):
                                n8 = 8 if q < 2 else 6
                                w_, bw_ = slab(S_["d"][:, hf * 11264 + q * 4096:hf * 11264 + q * 4096 + n8 * 512], n8 * 512)
                                wv = w_[:, 0:n8 * 512].rearrange("p (k c) -> p k c", k=n8)
                                for k8 in range(n8):
                                    kc = q * 8 + k8
                                    mm(pb[:, 0:512], HT[:, kc, :], wv[:, k8, :], kc == 0, kc == NFF - 1, [B["HT"], bw_], [bpb], sig=(k8 == n8 - 1))
                            pbs.append((pb, bpb))
                        out_norm_resid(pbs, GFPOST, "GFPOST")
                        dma(out[seq, t0:t0 + NT, :], X[:, 0, :], [B["X"]], [ob_], "dxo")
        except StopBuild:
            pass
        sems = {}
        for k in list(Plan.ENGS) + list(P.dma_cnt.keys()):
            sems[k] = st.enter_context(nc.semaphore("zq_" + k + "_sm"))
        P.emit(nc, sems, {"sync": ([("dxo", P.dma_cnt["dxo"])] if "dxo" in P.dma_cnt else []) + ([("vfo", P.dma_cnt["vfo"])] if "vfo" in P.dma_cnt else []) + [(d_, 16) for d_ in dumps]})
    return nc, P


def _fm_layout(W):
    n = W.shape[1] // 128
    return np.ascontiguousarray(W.reshape(8, 128, n, 128).transpose(1, 2, 0, 3))


def _tok_layout(W):
    K = W.shape[0] // 128
    return np.ascontiguousarray(W.reshape(K, 128, W.shape[1]).transpose(1, 0, 2))


def host_consts():
    s = np.arange(128)[:, None]
    t = np.arange(128)[None, :]
    c = {}
    c["ident"] = np.eye(128).astype(ml_dtypes.bfloat16)
    c["tri3"] = np.ascontiguousarray(np.stack([(s <= t), (s < t), (s > t)], axis=1).astype(np.float32))
    c["mskL"] = np.ascontiguousarray(np.tile((s > t).astype(np.float32), (1, 4)))
    mT = np.concatenate([(t > s), (t >= s)], axis=1).astype(np.float32)
    c["mskT"] = np.ascontiguousarray(np.tile(mT, (1, 2)))
    c["ident4"] = np.ascontiguousarray(np.tile(np.eye(128, dtype=np.float32), (1, 4)))
    return c


def host_layer(inp, l):
    f = lambda a: np.asarray(a, dtype=np.float32)
    w_in = f(inp["w_in"][l])
    d = {}
    d[f"wtok{l}"] = _tok_layout(w_in[:, 0:3072])
    d[f"wfm{l}"] = _fm_layout(w_in[:, 3072:7168])
    v1 = f(inp["vres_1"][l - 1]) if l > 0 else np.zeros((D, 32), np.float32)
    d[f"wl1{l}"] = _tok_layout(np.concatenate([f(inp["decay_w1"][l]), f(inp["a_1"][l]), f(inp["g_1"][l]), v1], axis=1))
    l2 = np.zeros((128, 4, D), np.float32)
    l2[0:64, 0] = f(inp["decay_w2"][l]); l2[64, 0] = f(inp["decay_w0"][l])
    l2[0:64, 1] = f(inp["a_2"][l]); l2[64, 1] = f(inp["a_0"][l])
    if l > 0:
        l2[0:32, 2] = f(inp["vres_2"][l - 1]); l2[32, 2] = f(inp["vres_0"][l - 1])
    l2[0:128, 3] = f(inp["g_2"][l])
    d[f"wl2{l}"] = l2
    d[f"wro{l}"] = _fm_layout(f(inp["w_rwkv_out"][l]))
    d[f"wco{l}"] = _fm_layout(f(inp["w_conv_out"][l]))
    wo_t = _tok_layout(f(inp["w_out"][l]))
    d[f"wo{l}"] = np.ascontiguousarray(np.concatenate([wo_t[:, :, h_ * 512:(h_ + 1) * 512].reshape(128, -1) for h_ in range(2)], axis=1))
    wg_t = _tok_layout(f(inp["ffn_w_gate"][l]))
    d[f"wg{l}"] = np.ascontiguousarray(np.concatenate([wg_t[:, :, c * 512:min(c * 512 + 512, 2816)].reshape(128, -1) for c in range(6)], axis=1))
    wu_t = _tok_layout(f(inp["ffn_w_up"][l]))
    d[f"wu{l}"] = np.ascontiguousarray(np.concatenate([wu_t[:, :, c * 512:min(c * 512 + 512, 2816)].reshape(128, -1) for c in range(6)], axis=1))
    wd_t = _tok_layout(f(inp["ffn_w_down"][l]))
    d[f"wd{l}"] = np.ascontiguousarray(np.concatenate([wd_t[:, q * 8:min(q * 8 + 8, 22), h_ * 512:(h_ + 1) * 512].reshape(128, -1) for h_ in range(2) for q in range(3)], axis=1))
    vres_mu = f(inp["vres_mu"][l - 1]) if l > 0 else np.zeros(D, np.float32)
    rows = [inp["pre_mix_norm"][l], inp["post_mix_norm"][l], inp["pre_ffn_norm"][l], inp["post_ffn_norm"][l],
            inp["mu_rkv"][l][0], inp["mu_rkv"][l][1], inp["mu_rkv"][l][2],
            inp["mu_wag"][l][0], inp["mu_wag"][l][1], inp["mu_wag"][l][2], vres_mu,
            inp["k_k"][l], inp["k_a"][l], np.asarray(inp["r_k"][l]).reshape(-1), inp["gn_w"][l], inp["gn_b"][l],
            inp["conv_b"][l], inp["conv_ln_w"][l], inp["conv_ln_b"][l]]
    d[f"vec{l}"] = np.ascontiguousarray(np.stack([f(r) for r in rows], axis=0))
    d[f"vecT{l}"] = np.ascontiguousarray(d[f"vec{l}"].reshape(NVEC, 8, 128).transpose(2, 1, 0))
    d[f"dwT{l}"] = np.ascontiguousarray(f(inp["conv_dw"][l]).reshape(31, 8, 128).transpose(2, 1, 0))
    return d


_PROG = {}


def kernel(**inputs):
    x = np.asarray(inputs["x"], dtype=np.float32)
    Bt, T, _ = x.shape
    nseq = Bt // 8
    key = (T, nseq)
    if key not in _PROG:
        _PROG[key] = build(T, nseq, [0, 1, 2, 3])[0]
    nc = _PROG[key]
    base = host_consts()
    for l in range(4):
        base.update(host_layer(inputs, l))
    in_maps = []
    for c in range(8):
        m = {"x": np.ascontiguousarray(x[c * nseq:(c + 1) * nseq])}
        m.update(base)
        in_maps.append(m)
    res = run_bass_kernel_spmd(nc, in_maps, core_ids=list(range(8)))
    return np.concatenate([res.results[c]["out"] for c in range(8)], axis=0).astype(np.float32)
```
